# Optimizing a Trainium2 kernel written in Bass

```python
import jax
import jax.numpy as jnp
from jax import lax
import numpy as np

D_MODEL = 1024
BATCH = 8
SEQ = 4096
DEPTH = 2

GRID_W = 64
CTX_LEN = 256
D_FF = 4 * D_MODEL
D_MIX = D_MODEL
CHUNK = 64
Q_BLOCK = 128
EPS = 1e-6
ROPE_THETA = 10000.0

GLA_H = 8
GLA_DK = D_MIX // (4 * GLA_H)
GLA_DV = D_MIX // (2 * GLA_H)
GLA_LR = 16
GLA_TAU = 16.0
ATT_H = 8
ATT_KV = 2
ATT_HD = D_MIX // (2 * ATT_H)
ML_H = 8
ML_HD = D_MIX // (2 * ML_H)
ML_D = ML_H * ML_HD
CONV_W = 3
RET_H = 8
RET_HD = D_MIX // (2 * RET_H)
RET_D = RET_H * RET_HD

AB_SIZES = (GLA_H * GLA_DK, GLA_H * GLA_DK, GLA_H * GLA_DV, GLA_H * GLA_DV, GLA_LR, GLA_LR, ATT_H * ATT_HD, ATT_KV * ATT_HD, ATT_KV * ATT_HD)
AB_IN = sum(AB_SIZES)
CD_SIZES = (ML_D, ML_D, RET_D, RET_D, RET_D, RET_D)
CD_IN = sum(CD_SIZES)
N_EVEN = (DEPTH + 1) // 2
N_ODD = DEPTH // 2

kernel_name = 'hybrid_gla_gqa_mlstm_retention_dit'


def rmsnorm(x, g):
    xf = x.astype(jnp.float32)
    y = xf * lax.rsqrt(jnp.mean(xf * xf, axis=-1, keepdims=True) + EPS)
    return (y * g.astype(jnp.float32)).astype(x.dtype)


def modulate(x, g, shift, scale):
    return rmsnorm(x, g) * (1.0 + scale) + shift


def sq_relu_mlp(h, w1, w2):
    return jnp.square(jax.nn.relu(h @ w1)) @ w2


def heads(t, n_heads):
    b, l, _ = t.shape
    return t.reshape(b, l, n_heads, -1).transpose(0, 2, 1, 3)


def merge(t):
    b, h, l, d = t.shape
    return t.transpose(0, 2, 1, 3).reshape(b, l, h * d)


def split_cols(u, sizes):
    return jnp.split(u, np.cumsum(sizes)[:-1].tolist(), axis=-1)


def flip_seq(t):
    return jnp.flip(t, axis=2)


def grid_angles(n_tokens, head_dim):
    n_rows = n_tokens // GRID_W
    rows, cols = jnp.meshgrid(jnp.arange(n_rows), jnp.arange(GRID_W), indexing='ij')
    axis_dim = head_dim // 2
    inv_freq = ROPE_THETA ** (-jnp.arange(0, axis_dim, 2, dtype=jnp.float32) / axis_dim)
    ang_r = rows.reshape(-1, 1).astype(jnp.float32) * inv_freq
    ang_c = cols.reshape(-1, 1).astype(jnp.float32) * inv_freq
    return ang_r, ang_c


def rope_axis(x, ang):
    x1, x2 = jnp.split(x, 2, axis=-1)
    cos, sin = jnp.cos(ang), jnp.sin(ang)
    return jnp.concatenate([x1 * cos - x2 * sin, x1 * sin + x2 * cos], axis=-1)


def axial_rope(x, ang_r, ang_c):
    xf = x.astype(jnp.float32)
    half = x.shape[-1] // 2
    y = jnp.concatenate([rope_axis(xf[..., :half], ang_r), rope_axis(xf[..., half:], ang_c)], axis=-1)
    return y.astype(x.dtype)


def short_conv(x, w, b):
    y = lax.conv_general_dilated(x, w[:, None, :], window_strides=(1,), padding=[(CONV_W // 2, CONV_W // 2)],
                                 dimension_numbers=('NWC', 'WIO', 'NWC'), feature_group_count=x.shape[-1])
    return y + b


def gla_dir(q, k, v, log_g, state0, want_out):
    bsz, nh, length, dk = q.shape
    dv = v.shape[-1]
    n = length // CHUNK
    q, k, v, lg = (t.reshape(bsz, nh, n, CHUNK, t.shape[-1]) for t in (q, k, v, log_g))
    b = jnp.cumsum(lg.astype(jnp.float32), axis=3)
    b_end = b[:, :, :, -1:, :]
    u = jnp.einsum('bhncd,bhnce->bhnde', k * jnp.exp(b_end - b), v)
    decay = jnp.exp(b_end[:, :, :, 0, :])

    def step(s, inp):
        dec, uc = inp
        return dec[..., None] * s + uc, s

    s_final, s_start = lax.scan(step, state0, (jnp.moveaxis(decay, 2, 0), jnp.moveaxis(u, 2, 0)))
    if not want_out:
        return None, s_final
    s_start = jnp.moveaxis(s_start, 0, 2)
    qd = q * jnp.exp(b)
    ki = k * jnp.exp(-b)
    tril = jnp.tril(jnp.ones((CHUNK, CHUNK), dtype=bool))
    a = jnp.where(tril, jnp.einsum('bhnid,bhnjd->bhnij', qd, ki), 0.0)
    o = jnp.einsum('bhnij,bhnje->bhnie', a, v) + jnp.einsum('bhnid,bhnde->bhnie', qd, s_start)
    return o.reshape(bsz, nh, length, dv).astype(v.dtype), s_final


def mlstm_dir(q, k, v, i_pre, logf, state0, want_out):
    bsz, nh, length, d = q.shape
    n = length // CHUNK
    q, k, v = (t.reshape(bsz, nh, n, CHUNK, d) for t in (q, k, v))
    ig = i_pre.reshape(bsz, nh, n, CHUNK).astype(jnp.float32)
    b = jnp.cumsum(logf.reshape(bsz, nh, n, CHUNK).astype(jnp.float32), axis=-1)
    b_end = b[..., -1]
    a = b_end[..., None] - b + ig
    m_loc = jnp.max(a, axis=-1)
    w = jnp.exp(a - m_loc[..., None])
    u_c = jnp.einsum('bhnc,bhncd,bhnce->bhnde', w, k, v)
    u_n = jnp.einsum('bhnc,bhncd->bhnd', w, k)

    def step(carry, inp):
        c_s, n_s, m_s = carry
        be, ml, uc, un = inp
        m_new = jnp.maximum(be + m_s, ml)
        s_old = jnp.exp(be + m_s - m_new)
        s_new = jnp.exp(ml - m_new)
        new = (s_old[..., None, None] * c_s + s_new[..., None, None] * uc,
               s_old[..., None] * n_s + s_new[..., None] * un, m_new)
        return new, carry

    final, starts = lax.scan(step, state0, tuple(jnp.moveaxis(t, 2, 0) for t in (b_end, m_loc, u_c, u_n)))
    if not want_out:
        return None, final
    c_st, n_st, m_st = (jnp.moveaxis(t, 0, 2) for t in starts)
    tril = jnp.tril(jnp.ones((CHUNK, CHUNK), dtype=bool))
    dmat = jnp.where(tril, b[..., :, None] - b[..., None, :] + ig[..., None, :], -jnp.inf)
    m_inter = b + m_st[..., None]
    m_i = jnp.maximum(m_inter, jnp.max(dmat, axis=-1))
    qk = jnp.einsum('bhnid,bhnjd->bhnij', q, k) * jnp.exp(dmat - m_i[..., None])
    s_inter = jnp.exp(m_inter - m_i)
    num = jnp.einsum('bhnij,bhnje->bhnie', qk, v) + s_inter[..., None] * jnp.einsum('bhnid,bhnde->bhnie', q, c_st)
    den = jnp.sum(qk, axis=-1) + s_inter * jnp.einsum('bhnid,bhnd->bhni', q, n_st)
    h = num / jnp.maximum(jnp.abs(den), jnp.exp(-m_i))[..., None]
    return h.reshape(bsz, nh, length, d).astype(v.dtype), final


def bidir_two_stream(scan_dir, lat_fwd, lat_bwd, ctx_fwd, ctx_bwd, state0, ctx_out):
    ctx_of, st_f = scan_dir(*ctx_fwd, state0, ctx_out)
    lat_of, _ = scan_dir(*lat_fwd, st_f, True)
    ctx_ob, st_b = scan_dir(*[flip_seq(t) for t in ctx_bwd], state0, ctx_out)
    lat_ob, _ = scan_dir(*[flip_seq(t) for t in lat_bwd], st_b, True)
    lat = lat_of + flip_seq(lat_ob)
    ctx = ctx_of + flip_seq(ctx_ob) if ctx_out else None
    return lat, ctx


def softmax_attend(q, k, v):
    s = jnp.einsum('bkgqd,bksd->bkgqs', q, k).astype(jnp.float32) * (q.shape[-1] ** -0.5)
    p = jax.nn.softmax(s, axis=-1).astype(v.dtype)
    return jnp.einsum('bkgqs,bksd->bkgqd', p, v)


def blocked_attend(q, k, v):
    bsz, nkv, g, length, d = q.shape
    nb = length // Q_BLOCK
    qb = jnp.moveaxis(q.reshape(bsz, nkv, g, nb, Q_BLOCK, d), 3, 0)
    ob = lax.map(lambda blk: softmax_attend(blk, k, v), qb)
    return jnp.moveaxis(ob, 0, 3).reshape(bsz, nkv, g, length, d)


def group_q(q):
    b, h, l, d = q.shape
    return q.reshape(b, ATT_KV, h // ATT_KV, l, d)


def mix_ab(ul, uc, w_gate, b_gate, gla_g, qk_g, ctx_out):
    lat, cx = split_cols(ul, AB_SIZES), split_cols(uc, AB_SIZES)
    n_lat = ul.shape[1]

    def gla_streams(p):
        q = heads(p[0], GLA_H) * GLA_DK ** -0.5
        k, v = heads(p[1], GLA_H), heads(p[2], GLA_H)
        lg_f, lg_b = (heads(jax.nn.log_sigmoid((lr @ w_gate[d] + b_gate[d]).astype(jnp.float32)) / GLA_TAU, GLA_H)
                      for d, lr in enumerate((p[4], p[5])))
        return (q, k, v, lg_f), (q, k, v, lg_b)

    lat_f, lat_b = gla_streams(lat)
    ctx_f, ctx_b = gla_streams(cx)
    s0 = jnp.zeros((ul.shape[0], GLA_H, GLA_DK, GLA_DV), jnp.float32)
    o_lat, o_ctx = bidir_two_stream(gla_dir, lat_f, lat_b, ctx_f, ctx_b, s0, ctx_out)

    def gla_out(o, g):
        return merge(rmsnorm(o, gla_g)) * jax.nn.silu(g)

    ang_r, ang_c = grid_angles(n_lat, ATT_HD)

    def q_of(p):
        return rmsnorm(heads(p[6], ATT_H), qk_g[0])

    def kv_of(p):
        return rmsnorm(heads(p[7], ATT_KV), qk_g[1]), heads(p[8], ATT_KV)

    k_l, v_l = kv_of(lat)
    k_c, v_c = kv_of(cx)
    q_l = axial_rope(q_of(lat), ang_r, ang_c)
    k_l = axial_rope(k_l, ang_r, ang_c)
    k_all = jnp.concatenate([k_c, k_l], axis=2)
    v_all = jnp.concatenate([v_c, v_l], axis=2)
    att_l = merge(blocked_attend(group_q(q_l), k_all, v_all).reshape(q_l.shape))
    y_lat = jnp.concatenate([gla_out(o_lat, lat[3]), att_l], axis=-1)
    if not ctx_out:
        return y_lat, None
    q_c = q_of(cx)
    att_c = merge(softmax_attend(group_q(q_c), k_c, v_c).reshape(q_c.shape))
    return y_lat, jnp.concatenate([gla_out(o_ctx, cx[3]), att_c], axis=-1)


def mix_cd(ul, uc, conv_w, conv_b, w_qkv, w_gate, b_gate, ml_g, skip, decay_logit, ret_g, ctx_out):
    lat, cx = split_cols(ul, CD_SIZES), split_cols(uc, CD_SIZES)
    bsz, n_lat = ul.shape[0], ul.shape[1]

    def ml_streams(p):
        xc = jax.nn.silu(short_conv(p[0], conv_w, conv_b))
        xch = heads(xc, ML_H)
        q = jnp.einsum('bhld,hde->bhle', xch, w_qkv[0])
        k = jnp.einsum('bhld,hde->bhle', xch, w_qkv[1])
        v = jnp.einsum('bhld,hde->bhle', heads(p[0], ML_H), w_qkv[2])
        gin = jnp.concatenate([merge(q), merge(k), merge(v)], axis=-1)
        dirs = []
        for d in range(2):
            z = jnp.swapaxes((gin @ w_gate[d] + b_gate[d]).astype(jnp.float32), 1, 2)
            dirs.append((q * ML_HD ** -0.5, k, v, z[:, :ML_H], jax.nn.log_sigmoid(z[:, ML_H:])))
        return xc, dirs[0], dirs[1]

    xc_l, lat_f, lat_b = ml_streams(lat)
    xc_c, ctx_f, ctx_b = ml_streams(cx)
    s0 = (jnp.zeros((bsz, ML_H, ML_HD, ML_HD), jnp.float32), jnp.zeros((bsz, ML_H, ML_HD), jnp.float32),
          jnp.zeros((bsz, ML_H), jnp.float32))
    h_lat, h_ctx = bidir_two_stream(mlstm_dir, lat_f, lat_b, ctx_f, ctx_b, s0, ctx_out)

    def ml_out(h, xc, z):
        return (merge(rmsnorm(h, ml_g)) + skip * xc) * jax.nn.silu(z)

    ang_r, ang_c = grid_angles(n_lat, RET_HD)
    log_decay = jax.nn.log_sigmoid(decay_logit.astype(jnp.float32))

    def ret_streams(p, rotate):
        q, k, v = heads(p[2], RET_H), heads(p[3], RET_H), heads(p[4], RET_H)
        if rotate:
            q, k = axial_rope(q, ang_r, ang_c), axial_rope(k, ang_r, ang_c)
        q = q * RET_HD ** -0.5
        lg = [jnp.broadcast_to(log_decay[d][None, :, None, None], k.shape) for d in range(2)]
        return (q, k, v, lg[0]), (q, k, v, lg[1])

    rl_f, rl_b = ret_streams(lat, True)
    rc_f, rc_b = ret_streams(cx, False)
    s0r = jnp.zeros((bsz, RET_H, RET_HD, RET_HD), jnp.float32)
    r_lat, r_ctx = bidir_two_stream(gla_dir, rl_f, rl_b, rc_f, rc_b, s0r, ctx_out)

    def ret_out(o, g):
        return merge(rmsnorm(o, ret_g)) * jax.nn.silu(g)

    y_lat = jnp.concatenate([ml_out(h_lat, xc_l, lat[1]), ret_out(r_lat, lat[5])], axis=-1)
    if not ctx_out:
        return y_lat, None
    return y_lat, jnp.concatenate([ml_out(h_ctx, xc_c, cx[1]), ret_out(r_ctx, cx[5])], axis=-1)


def setup_inputs(seed: int = 0) -> dict:
    key = jax.random.key(seed)
    ks = iter(jax.random.split(key, 32))

    def nrm(shape, s):
        return jax.random.normal(next(ks), shape, jnp.float32) * s

    f_bias = jnp.linspace(3.0, 6.0, ML_H, dtype=jnp.float32)
    ret_logit = jnp.log(2.0 ** (5.0 + jnp.arange(RET_H, dtype=jnp.float32)) - 1.0)
    return {
        'x': nrm((BATCH, SEQ, D_MODEL), 1.0),
        'c': nrm((BATCH, D_MODEL), 1.0),
        'ctx': nrm((BATCH, CTX_LEN, D_MODEL), 1.0),
        'c_ctx': nrm((D_MODEL,), 1.0),
        'ada_w': nrm((DEPTH, D_MODEL, 6 * D_MODEL), D_MODEL ** -0.5),
        'ada_b': nrm((DEPTH, 6 * D_MODEL), 0.02),
        'norm_g': 1.0 + nrm((DEPTH, 4, D_MODEL), 0.05),
        'w_out': nrm((DEPTH, D_MIX, D_MODEL), D_MIX ** -0.5),
        'mlp_w1': nrm((DEPTH, D_MODEL, D_FF), D_MODEL ** -0.5),
        'mlp_w2': nrm((DEPTH, D_FF, D_MODEL), D_FF ** -0.5),
        'ab_w_in': nrm((N_EVEN, D_MODEL, AB_IN), D_MODEL ** -0.5),
        'gla_w_gate': nrm((N_EVEN, 2, GLA_LR, GLA_H * GLA_DK), GLA_LR ** -0.5),
        'gla_b_gate': nrm((N_EVEN, 2, GLA_H * GLA_DK), 0.02),
        'gla_norm_g': 1.0 + nrm((N_EVEN, GLA_DV), 0.05),
        'att_qk_norm_g': 1.0 + nrm((N_EVEN, 2, ATT_HD), 0.05),
        'cd_w_in': nrm((N_ODD, D_MODEL, CD_IN), D_MODEL ** -0.5),
        'ml_conv_w': nrm((N_ODD, CONV_W, ML_D), CONV_W ** -0.5),
        'ml_conv_b': nrm((N_ODD, ML_D), 0.02),
        'ml_w_qkv': nrm((N_ODD, 3, ML_H, ML_HD, ML_HD), ML_HD ** -0.5),
        'ml_w_gate': nrm((N_ODD, 2, 3 * ML_D, 2 * ML_H), (3 * ML_D) ** -0.5),
        'ml_b_gate': jnp.concatenate([nrm((N_ODD, 2, ML_H), 0.1), f_bias + nrm((N_ODD, 2, ML_H), 0.1)], axis=-1),
        'ml_norm_g': 1.0 + nrm((N_ODD, ML_HD), 0.05),
        'ml_skip': 1.0 + nrm((N_ODD, ML_D), 0.05),
        'ret_decay_logit': ret_logit + nrm((N_ODD, 2, RET_H), 0.1),
        'ret_norm_g': 1.0 + nrm((N_ODD, RET_HD), 0.05),
    }


def reference(x, c, ctx, c_ctx, ada_w, ada_b, norm_g, w_out, mlp_w1, mlp_w2, ab_w_in, gla_w_gate, gla_b_gate,
              gla_norm_g, att_qk_norm_g, cd_w_in, ml_conv_w, ml_conv_b, ml_w_qkv, ml_w_gate, ml_b_gate, ml_norm_g,
              ml_skip, ret_decay_logit, ret_norm_g):
    lat, cx = x, ctx
    for layer in range(DEPTH):
        ctx_out = layer < DEPTH - 1
        g = norm_g[layer]
        mod_l = jnp.split((jax.nn.silu(c) @ ada_w[layer] + ada_b[layer])[:, None, :], 6, axis=-1)
        mod_c = jnp.split(jax.nn.silu(c_ctx) @ ada_w[layer] + ada_b[layer], 6, axis=-1)
        ul = modulate(lat, g[0], mod_l[0], mod_l[1])
        uc = modulate(cx, g[0], mod_c[0], mod_c[1])
        j = layer // 2
        if layer % 2 == 0:
            yl, yc = mix_ab(ul @ ab_w_in[j], uc @ ab_w_in[j], gla_w_gate[j], gla_b_gate[j], gla_norm_g[j],
                            att_qk_norm_g[j], ctx_out)
        else:
            yl, yc = mix_cd(ul @ cd_w_in[j], uc @ cd_w_in[j], ml_conv_w[j], ml_conv_b[j], ml_w_qkv[j],
                            ml_w_gate[j], ml_b_gate[j], ml_norm_g[j], ml_skip[j], ret_decay_logit[j],
                            ret_norm_g[j], ctx_out)
        lat = lat + mod_l[2] * rmsnorm(yl @ w_out[layer], g[1])
        lat = lat + mod_l[5] * rmsnorm(sq_relu_mlp(modulate(lat, g[2], mod_l[3], mod_l[4]), mlp_w1[layer], mlp_w2[layer]), g[3])
        if ctx_out:
            cx = cx + mod_c[2] * rmsnorm(yc @ w_out[layer], g[1])
            cx = cx + mod_c[5] * rmsnorm(sq_relu_mlp(modulate(cx, g[2], mod_c[3], mod_c[4]), mlp_w1[layer], mlp_w2[layer]), g[3])
    return lat
```

```python
import math
from contextlib import ExitStack

import numpy as np
import concourse.bass as bass
import concourse.mybir as mybir
from concourse.bass_utils import run_bass_kernel_spmd

F32 = mybir.dt.float32
BF16 = mybir.dt.bfloat16
ALU = mybir.AluOpType
AF = mybir.ActivationFunctionType
AX = mybir.AxisListType

D = 1024
NCTX = 256
NLAT = 4096
NT = NCTX + NLAT
NTILE = NT // 128
EPS = 1e-6
W0C = 2336 + 512 + 128
W1C = 3072 + 512 + 512


class Buf:
    __slots__ = ("w", "r")

    def __init__(self):
        self.w = None
        self.r = {}


class FW:
    NDMA = 24
    NPRE = 8

    def __init__(self, nc, stack):
        self.nc = nc
        self.eng = {"pe": nc.tensor, "act": nc.scalar, "dve": nc.vector, "pool": nc.gpsimd, "sp": nc.sync}
        self.sems = {}
        self.cnt = {}
        for k in ("pe", "act", "dve", "pool"):
            self.sems[k] = stack.enter_context(nc.semaphore("s_" + k))
            self.cnt[k] = 0
        for i in range(self.NDMA):
            k = "d%d" % i
            self.sems[k] = stack.enter_context(nc.semaphore("s_" + k))
            self.cnt[k] = 0
        for i in range(self.NPRE):
            k = "p%d" % i
            self.sems[k] = stack.enter_context(nc.semaphore("s_" + k))
            self.cnt[k] = 0
        self.rr = 0
        self.rrp = 0
        self.seen = {e: {} for e in self.eng}
        self.nins = 0

    def wait(self, e, ticket):
        if ticket is None:
            return
        k, v = ticket
        if self.seen[e].get(k, 0) >= v:
            return
        self.eng[e].wait_ge(self.sems[k], v)
        self.seen[e][k] = v

    def deps(self, e, reads, writes):
        for b in reads:
            self.wait(e, b.w)
        for b in writes:
            if b.w is not None and b.w[0] != e:
                self.wait(e, b.w)
            for k, v in b.r.items():
                if k != e:
                    self.wait(e, (k, v))

    def mark(self, ticket, reads, writes):
        k, v = ticket
        for b in reads:
            if b.r.get(k, 0) < v:
                b.r[k] = v
        for b in writes:
            b.w = ticket
            b.r = {}

    def op(self, e, fn, reads=(), writes=(), signal=True):
        self.deps(e, reads, writes)
        ins = fn(self.eng[e])
        self.nins += 1
        if signal:
            self.cnt[e] += 1
            ins.then_inc(self.sems[e], 1)
            t = (e, self.cnt[e])
        else:
            t = (e, self.cnt[e] + 1)
        self.mark(t, reads, writes)
        return t

    def dma(self, e, out, in_, reads=(), writes=(), pref=False, **kw):
        self.deps(e, reads, writes)
        if pref:
            k = "p%d" % self.rrp
            self.rrp = (self.rrp + 1) % self.NPRE
        else:
            k = "d%d" % self.rr
            self.rr = (self.rr + 1) % self.NDMA
        if self.cnt[k] > 0:
            self.wait(e, (k, self.cnt[k]))
        ins = self.eng[e].dma_start(out=out, in_=in_, **kw)
        self.cnt[k] += 16
        ins.then_inc(self.sems[k], 16)
        self.nins += 1
        t = (k, self.cnt[k])
        self.mark(t, reads, writes)
        return t

    def barrier(self, full=False):
        for e in self.eng:
            for k, v in self.cnt.items():
                if v > 0 and (full or not k.startswith("p")):
                    self.wait(e, (k, v))


def TP(hp):
    return {"tile_position": (96, 0)} if hp == 3 else {}


class Ring:
    def __init__(self, aps):
        self.aps = aps
        self.bufs = [Buf() for _ in aps]
        self.i = 0

    def next(self):
        j = self.i
        self.i = (self.i + 1) % len(self.aps)
        return self.aps[j], self.bufs[j]


class Prog:
    def __init__(self, dbg=False, upto=99):
        self.dbg = dbg
        self.upto = upto
        self.nc = bass.Bass("TRN2", target_bir_lowering=False)
        self.dbg_names = []

    def din(self, name, shape, dt=F32):
        return self.nc.dram_tensor(name, list(shape), dt, kind="ExternalInput").ap()

    def dscr(self, name, shape, dt=F32):
        if self.dbg:
            self.dbg_names.append(name)
            return self.nc.dram_tensor(name, list(shape), dt, kind="ExternalOutput").ap()
        return self.nc.dram_tensor(name, list(shape), dt).ap()

    def sb(self, st, name, shape, dt=F32):
        self._n = getattr(self, "_n", 0) + 1
        return st.enter_context(self.nc.sbuf_tensor("sb%d_%s" % (self._n, name), list(shape), dt)).ap()

    def ring(self, st, name, shape, dt, n):
        return Ring([self.sb(st, "%s%d" % (name, i), shape, dt) for i in range(n)])

    def build(self):
        nc = self.nc
        I = {}
        I["x"] = self.din("x", [NLAT, D])
        I["ctx"] = self.din("ctx", [NCTX, D])
        I["ccols"] = self.din("ccols", [128, 8, 2])
        I["ada_w"] = self.din("ada_w", [2, D, 6 * D])
        I["ada_b"] = self.din("ada_b", [2, 6 * D])
        I["norm_g"] = self.din("norm_g", [2, 4, D])
        I["w_out"] = self.din("w_out", [2, D, D])
        I["mlp_w1"] = self.din("mlp_w1", [2, D, 4 * D])
        I["mlp_w2"] = self.din("mlp_w2", [2, 4 * D, D])
        I["w_in0"] = self.din("w_in0", [D, W0C])
        I["w_in1"] = self.din("w_in1", [D, W1C])
        I["gla_wg"] = self.din("gla_wg", [2, 17, 256])
        I["gla_ng"] = self.din("gla_ng", [1, 64])
        I["att_g"] = self.din("att_g", [128, 4])
        I["ml_conv"] = self.din("ml_conv", [128, 4, 4])
        I["ml_bd"] = self.din("ml_bd", [3, 4, 128, 128])
        I["ml_wg"] = self.din("ml_wg", [12, 128, 32])
        I["ml_bg"] = self.din("ml_bg", [1, 32])
        I["ml_ng"] = self.din("ml_ng", [1, 64])
        I["ml_skip"] = self.din("ml_skip", [128, 4])
        I["ret_logit"] = self.din("ret_logit", [1, 16])
        I["ret_ng"] = self.din("ret_ng", [1, 64])
        I["cmat"] = self.din("cmat", [8, 128, 128])
        I["rope"] = self.din("rope", [2, 128, NT])
        I["jcol"] = self.din("jcol", [128, 2])
        self.I = I
        self.out = nc.dram_tensor("out", [NLAT, D], F32, kind="ExternalOutput").ap()

        with ExitStack() as gst:
            self.fw = FW(nc, gst)
            fw = self.fw
            self.psr = Ring([gst.enter_context(nc.psum_tensor("ps%d" % i, [128, 512], F32)).ap() for i in range(8)])
            self.cm = self.sb(gst, "cm", [128, 8, 128], F32)
            self.cmb = self.sb(gst, "cmb", [128, 8, 128], BF16)
            self.bcm = Buf()
            fw.dma("sp", self.cm, I["cmat"].rearrange("n p f -> p n f"), writes=[self.bcm])
            fw.dma("pool", self.cmb, I["cmat"].rearrange("n p f -> p n f"), writes=[self.bcm])
            self.psb = self.psr.aps
            self.S = {}
            self.S["yT0"] = self.dscr("yT0", [D, NT], BF16)
            self.S["yT1"] = self.dscr("yT1", [D, NT], BF16)
            self.res1 = self.dscr("res1", [NT, D])
            with ExitStack() as wst0:
                W0pre = self.inproj_weights(wst0, 0, pref=True)
                self.phase_ada()
                if self.upto >= 1:
                    self.phase_inproj(0, W0pre)
            for layer in range(2):
                u = self.upto - 10 * layer
                if u >= 1 and layer == 1:
                    self.phase_inproj(layer)
                if layer == 0:
                    if u >= 2:
                        self.phase_attn()
                    if u >= 3:
                        self.phase_gla()
                else:
                    if u >= 2:
                        self.phase_mlstm()
                    if u >= 3:
                        self.phase_ret()
                if u >= 4:
                    with ExitStack() as wst:
                        Wpre = self.mlp_weights(wst, layer) if u >= 5 else None
                        self.phase_outproj(layer)
                        if u >= 5:
                            self.phase_mlp(layer, Wpre)
            fw.barrier(full=True)
        return nc

    def psring(self, idx):
        return Ring([self.psb[i] for i in idx])

    def phase_attn(self):
        nc, fw, S = self.nc, self.fw, self.S
        yT = S["yT0"]
        with ExitStack() as st:
            kd = [self.sb(st, "kd%d" % g, [128, NT], BF16) for g in range(2)]
            bkd = [Buf(), Buf()]
            va = [self.sb(st, "va%d" % g, [128, NTILE, 128], BF16) for g in range(2)]
            bva = [Buf(), Buf()]
            for g in range(2):
                for hh in range(2):
                    fw.dma("sp", kd[g][hh * 64:(hh + 1) * 64, :], S["kBT"][g * 64:(g + 1) * 64, :], writes=[bkd[g]])
                fw.op("pool", lambda e: e.memset(va[g], 1.0), writes=[bva[g]])
                vsrc = S["vB"][:, g * 64:(g + 1) * 64].rearrange("(kb p) d -> p kb d", p=128)
                for q4 in range(0, NTILE, 8):
                    q5 = min(NTILE, q4 + 8)
                    fw.dma("sp", va[g][:, q4:q5, 0:64], vsrc[:, q4:q5, :], writes=[bva[g]])
            qr = self.ring(st, "qT", [128, 2, 256], BF16, 4)
            for qa, qb_ in zip(qr.aps, qr.bufs):
                fw.op("pool", lambda e: e.memset(qa, 0.0), writes=[qb_])
            pr = self.ring(st, "pT", [128, 512], BF16, 4)
            recr = self.ring(st, "rec", [64, 512], F32, 2)
            outr = self.ring(st, "ao", [64, 512], BF16, 3)
            accr = self.psring([0, 1])
            sr = self.psring([2, 3, 4, 5, 6, 7])
            work = [(t0, m) for t0 in range(0, NTILE, 2) for m in range(4)]

            def load_q(t0, m):
                qT, bq = qr.next()
                for hh in range(2):
                    fw.dma("sp", qT[hh * 64:(hh + 1) * 64, hh, :], S["qBT"][m * 128 + hh * 64:m * 128 + (hh + 1) * 64, t0 * 128:t0 * 128 + 256], writes=[bq])
                return qT, bq

            qnext = load_q(*work[0])
            for wi, (t0, m) in enumerate(work):
                if True:
                    c0 = t0 * 128
                    blocks = [0, 1] if t0 < 2 else list(range(NTILE))
                    g = m // 2
                    qT, bq = qnext
                    if wi + 1 < len(work):
                        qnext = load_q(*work[wi + 1])
                    acc, bacc = accr.next()
                    pend = []

                    def s_mm(kb):
                        s_, bs_ = sr.next()
                        fw.op("pe", lambda e: e.matmul(s_, lhsT=kd[g][:, kb * 128:(kb + 1) * 128], rhs=qT.rearrange("p h t -> p (h t)"), start=True, stop=True),
                              reads=[bkd[g], bq], writes=[bs_])
                        p_, bp_ = pr.next()
                        fw.op("act", lambda e: e.activation(p_, s_, AF.Exp, scale=0.125), reads=[bs_], writes=[bp_])
                        pend.append((kb, p_, bp_))

                    def pv_mm():
                        kb, p_, bp_ = pend.pop(0)
                        for hh in range(2):
                            fw.op("pe", lambda e: e.matmul(acc[:, hh * 256:(hh + 1) * 256], lhsT=va[g][:, kb, :], rhs=p_[:, hh * 256:(hh + 1) * 256], start=(kb == blocks[0] and hh == 0), stop=(kb == blocks[-1])),
                                  reads=[bva[g], bp_], writes=[bacc], signal=(hh == 1))

                    for kb in blocks:
                        s_mm(kb)
                        if len(pend) > 2:
                            pv_mm()
                    while pend:
                        pv_mm()
                    rec, brec = recr.next()
                    fw.op("dve", lambda e: e.reciprocal(rec, acc[64:128, :]), reads=[bacc], writes=[brec])
                    ao, bao = outr.next()
                    fw.op("dve", lambda e: e.tensor_tensor(ao, acc[0:64, :], rec, op=ALU.mult), reads=[bacc, brec], writes=[bao])
                    fw.dma("sp", yT[512 + m * 128:512 + (m + 1) * 128, c0:c0 + 256].rearrange("(hh d) t -> d hh t", hh=2), ao.rearrange("p (hh t) -> p hh t", hh=2), reads=[bao])
            fw.barrier()

    def scan_driver(self, orders, mk):
        n = len(orders[0])
        Ls = [dict(), dict()]
        As = [dict(), dict()]
        for step in range(-2, n):
            for d in range(2):
                if 0 <= step + 2 < n:
                    Ls[d][step + 2] = mk[d][0](orders[d][step + 2])
            for d in range(2):
                if 0 <= step + 1 < n:
                    As[d][step + 1] = mk[d][1](orders[d][step + 1], Ls[d].pop(step + 1))
            for d in range(2):
                if step >= 0:
                    mk[d][2](orders[d][step], As[d].pop(step))

    def phase_gla(self):
        nc, fw, S, cm, cmb = self.nc, self.fw, self.S, self.cm, self.cmb
        yT = S["yT0"]
        with ExitStack() as st:
            oacc = self.sb(st, "oacc", [128, NTILE, 512]); boacc = [Buf() for _ in range(NTILE)]
            Sf = [[self.sb(st, "Sf%d%d" % (d, i), [128, 64]) for i in range(2)] for d in range(2)]
            Sb = [[self.sb(st, "Sb%d%d" % (d, i), [128, 64], BF16) for i in range(2)] for d in range(2)]
            bS = [[Buf(), Buf()], [Buf(), Buf()]]
            hm = self.sb(st, "hm", [128, 4]); bhm = Buf()
            fw.op("dve", lambda e: e.tensor_copy(hm, cm[:, self.BLK32, 0:128:32]), reads=[self.bcm], writes=[bhm])
            RL, RW = 8, 6
            qTr = self.ring(st, "gq", [128, 2, 128], F32, RL)
            kTr = self.ring(st, "gk", [128, 2, 128], F32, RL)
            ktr = self.ring(st, "gkt", [128, 256], F32, RL)
            vr = self.ring(st, "gv", [128, 512], BF16, RL)
            lgr = self.ring(st, "glg", [128, 256], F32, RL)
            gr = self.ring(st, "gg", [128, 512], F32, 8)
            Eqr = self.ring(st, "Eq", [128, 2, 128], F32, RW)
            Ekr = self.ring(st, "Ek", [128, 2, 128], F32, 4)
            Err = self.ring(st, "Er", [128, 256], F32, 4)
            qddr = self.ring(st, "qdd", [128, 2, 128], F32, 4)
            qdr = self.ring(st, "qd", [128, 2, 4, 128], BF16, RW)
            kir = self.ring(st, "ki", [128, 2, 128], BF16, 4)
            kdr = self.ring(st, "kdd", [128, 256], BF16, RW)
            Amr = self.ring(st, "Am", [128, 8, 128], BF16, RW)
            t32r = self.ring(st, "gt32", [128, 512], F32, 3)
            ybr = self.ring(st, "gyb", [128, 512], BF16, 2)
            yTr = self.ring(st, "gyT", [128, 4, 128], BF16, 2)
            str_ = self.ring(st, "gst", [128, 24], F32, 2)
            psr = self.psr
            yv = yT[0:512, :].rearrange("(m p) t -> p m t", p=128)
            orders = [list(range(NTILE)), [1, 0] + list(range(NTILE - 1, 1, -1))]
            pos = [{c: i for i, c in enumerate(o)} for o in orders]
            last_dir = {c: (1 if pos[1][c] >= pos[0][c] else 0) for c in range(NTILE)}
            for d in range(2):
                for hc in range(2):
                    fw.op("dve", lambda e: e.memset(Sf[d][hc], 0.0), writes=[bS[d][hc]])
                    fw.op("dve", lambda e: e.memset(Sb[d][hc], 0.0), writes=[bS[d][hc]])

            def make_dir(d):
                TRI = self.UINC if d == 0 else self.LINC
                TRIS = self.LSTR if d == 0 else self.USTR
                ecol = 127 if d == 0 else 0

                def stageL(c):
                    r0 = c * 128
                    qT, bq = qTr.next(); kT, bk = kTr.next(); kt, bkt = ktr.next(); v, bv = vr.next(); lg, blg = lgr.next()
                    fw.dma("sp", qT, S["qAT"][:, r0:r0 + 128].rearrange("(hc p) t -> p hc t", p=128), writes=[bq])
                    fw.dma("sp", kT, S["kAT"][:, r0:r0 + 128].rearrange("(hc p) t -> p hc t", p=128), writes=[bk])
                    fw.dma("sp", kt, S["kA"][r0:r0 + 128, :], writes=[bkt])
                    fw.dma("sp", v, S["vA"][r0:r0 + 128, :], writes=[bv])
                    fw.dma("sp", lg, S["lgp"][r0:r0 + 128, d * 256:(d + 1) * 256], writes=[blg])
                    gt = bg = None
                    if last_dir[c] == d:
                        gt, bg = gr.next()
                        fw.dma("sp", gt, S["gA"][r0:r0 + 128, :], writes=[bg])
                    return locals()

                def stageA(c, L):
                    qT, bq, kT, bk, kt, bkt, v, bv, lg, blg = (L[k] for k in ("qT", "bq", "kT", "bk", "kt", "bkt", "v", "bv", "lg", "blg"))
                    gt, bg, r0 = L["gt"], L["bg"], L["r0"]
                    pc, bpc = psr.next()
                    for hc in range(2):
                        fw.op("pe", lambda e: e.matmul(pc[:, hc * 128:(hc + 1) * 128], lhsT=lg[:, hc * 128:(hc + 1) * 128], rhs=cm[:, TRI, :], start=True, stop=True),
                              reads=[blg, self.bcm], writes=[bpc], signal=False)
                    fw.op("pe", lambda e: e.matmul(pc[:, 256:512], lhsT=cm[:, TRIS, :], rhs=lg, start=True, stop=True), reads=[blg, self.bcm], writes=[bpc])
                    Eq, bEq = Eqr.next(); Ek, bEk = Ekr.next(); Er, bEr = Err.next()
                    fw.op("act", lambda e: e.activation(Eq.rearrange("p a b -> p (a b)"), pc[:, 0:256], AF.Exp, scale=-1.0 / 16), reads=[bpc], writes=[bEq])
                    fw.op("act", lambda e: e.activation(Ek.rearrange("p a b -> p (a b)"), pc[:, 0:256], AF.Exp, scale=1.0 / 16), reads=[bpc], writes=[bEk])
                    fw.op("act", lambda e: e.activation(Er, pc[:, 256:512], AF.Exp, scale=-1.0 / 16), reads=[bpc], writes=[bEr])
                    qdd, bqdd = qddr.next(); qd, bqd = qdr.next(); ki, bki = kir.next(); kdd, bkdd = kdr.next()
                    fw.op("dve", lambda e: e.tensor_tensor(qdd, qT, Eq, op=ALU.mult), reads=[bq, bEq], writes=[bqdd])
                    import os
                    if os.environ.get("GLA_Q4D", "1") == "1":
                        for hc in range(2):
                            fw.op("dve", lambda e: e.tensor_tensor(qd[:, hc, :, :], qdd[:, hc, :].unsqueeze(1).to_broadcast([128, 4, 128]),
                                                                    hm.unsqueeze(2).to_broadcast([128, 4, 128]), op=ALU.mult), reads=[bqdd, bhm], writes=[bqd])
                    else:
                        for hc in range(2):
                            for hp in range(4):
                                fw.op("dve", lambda e: e.tensor_scalar(qd[:, hc, hp, :], qdd[:, hc, :], hm[:, hp:hp + 1], None, op0=ALU.mult), reads=[bqdd, bhm], writes=[bqd])
                    fw.op("pool", lambda e: e.tensor_tensor(ki, kT, Ek, op=ALU.mult), reads=[bk, bEk], writes=[bki])
                    fw.op("pool", lambda e: e.tensor_tensor(kdd, kt, Er, op=ALU.mult), reads=[bkt, bEr], writes=[bkdd])
                    Am, bAm = Amr.next()
                    for hc in range(2):
                        pa, bpa = psr.next()
                        fw.op("pe", lambda e: e.matmul(pa, lhsT=ki[:, hc, :], rhs=qd[:, hc, :, :].rearrange("p h t -> p (h t)"), start=True, stop=True),
                              reads=[bki, bqd], writes=[bpa])
                        fw.op("dve", lambda e: e.tensor_tensor(Am[:, hc * 4:(hc + 1) * 4, :], pa.rearrange("p (h t) -> p h t", h=4),
                                                                cm[:, TRI, :].unsqueeze(1).to_broadcast([128, 4, 128]), op=ALU.mult), reads=[bpa, self.bcm], writes=[bAm])
                    return locals()

                def stageB(c, L):
                    r0, qd, bqd, v, bv, Am, bAm, kdd, bkdd, Eq, bEq = (L[k] for k in ("r0", "qd", "bqd", "v", "bv", "Am", "bAm", "kdd", "bkdd", "Eq", "bEq"))
                    gt, bg = L["gt"], L["bg"]
                    po, bpo = psr.next()
                    for h in range(8):
                        hc, hp = h // 4, h % 4
                        fw.op("pe", lambda e: e.matmul(po[:, h * 64:(h + 1) * 64], lhsT=Am[:, h, :], rhs=v[:, h * 64:(h + 1) * 64], start=True, stop=False),
                              reads=[bAm, bv], writes=[bpo], signal=False)
                        fw.op("pe", lambda e: e.matmul(po[:, h * 64:(h + 1) * 64], lhsT=qd[:, hc, hp, :], rhs=Sb[d][hc], start=False, stop=True),
                              reads=[bqd, bS[d][hc]], writes=[bpo], signal=(h == 7))
                    if last_dir[c] != d:
                        fw.op("act", lambda e: e.copy(oacc[:, c, :], po), reads=[bpo], writes=[boacc[c]])
                    else:
                        fw.op("dve", lambda e: e.tensor_tensor(oacc[:, c, :], oacc[:, c, :], po, op=ALU.add), reads=[bpo, boacc[c]], writes=[boacc[c]])
                    for hc in range(2):
                        pu, bpu = psr.next()
                        fw.op("pe", lambda e: e.matmul(pu, lhsT=kdd[:, hc * 128:(hc + 1) * 128], rhs=v, start=True, stop=True), reads=[bkdd, bv], writes=[bpu])
                        for hp in range(4):
                            h = hc * 4 + hp
                            sl = slice(hp * 32, (hp + 1) * 32)
                            fw.op("dve", lambda e: e.scalar_tensor_tensor(Sf[d][hc][sl, :], Sf[d][hc][sl, :], Eq[sl, hc, ecol:ecol + 1], pu[sl, h * 64:(h + 1) * 64], op0=ALU.mult, op1=ALU.add),
                                  reads=[bpu, bEq, bS[d][hc]], writes=[bS[d][hc]])
                        fw.op("act", lambda e: e.copy(Sb[d][hc], Sf[d][hc]), reads=[bS[d][hc]], writes=[bS[d][hc]])
                    if last_dir[c] == d:
                        self.finalize_tok(oacc[:, c, :], boacc[c], gt, bg, t32r, str_, ybr, yTr, yv[:, :, r0:r0 + 128])

                return stageL, stageA, stageB

            self.scan_driver(orders, [make_dir(0), make_dir(1)])
            fw.barrier()

    def finalize_tok(self, o, bo, gt, bg, t32r, str_, ybr, yTr, dst, post=None):
        fw, cmb = self.fw, self.cmb
        t1, b1 = t32r.next()
        fw.op("pool", lambda e: e.tensor_tensor(t1, o, o, op=ALU.mult), reads=[bo], writes=[b1])
        stt, bst = str_.next()
        fw.op("dve", lambda e: e.tensor_reduce(stt[:, 0:8], t1.rearrange("p (h d) -> p h d", d=64), axis=AX.X, op=ALU.add), reads=[b1], writes=[bst])
        self.rstd_col(stt[:, 0:8], bst, stt[:, 16:24], stt[:, 8:16], 8, 1.0 / 64)
        t2, b2 = t32r.next()
        fw.op("dve", lambda e: e.tensor_tensor(t2.rearrange("p (h d) -> p h d", d=64), o.rearrange("p (h d) -> p h d", d=64),
                                                stt[:, 16:24].unsqueeze(2).to_broadcast([128, 8, 64]), op=ALU.mult), reads=[bo, bst], writes=[b2])
        yb, byb = ybr.next()
        if len(gt.shape) == 3:
            fw.op("pool", lambda e: e.tensor_tensor(yb.rearrange("p (h d) -> p h d", d=64), t2.rearrange("p (h d) -> p h d", d=64), gt, op=ALU.mult), reads=[b2, bg], writes=[byb])
        else:
            fw.op("pool", lambda e: e.tensor_tensor(yb, t2, gt, op=ALU.mult), reads=[b2, bg], writes=[byb])
        ps, bp = self.psr.next()
        psb = ps.bitcast(BF16)
        for m in range(4):
            fw.op("pe", lambda e: e.transpose(psb[:, m * 128:(m + 1) * 128], yb[:, m * 128:(m + 1) * 128], cmb[:, self.IDENT, :]),
                  reads=[byb, self.bcm], writes=[bp], signal=(m == 3))
        if post is not None:
            post(psb, bp, dst)
            return
        yTt, byT = yTr.next()
        fw.op("act", lambda e: e.copy(yTt, psb[:, 0:512].rearrange("p (m t) -> p m t", m=4)), reads=[bp], writes=[byT])
        fw.dma("sp", dst, yTt, reads=[byT])

    def phase_outproj(self, layer):
        nc, fw, I, S, cmb = self.nc, self.fw, self.I, self.S, self.cmb
        yT = S["yT%d" % layer]
        with ExitStack() as st:
            Wo = self.sb(st, "Wo", [128, 8, D], BF16); bWo = Buf()
            fw.dma("pool", Wo, I["w_out"][layer].rearrange("(kc p) n -> p kc n", p=128), writes=[bWo])
            yr = self.ring(st, "oy", [128, 8, 128], BF16, 3)
            xr = self.ring(st, "ox", [128, D], F32, 3)
            tr = self.ring(st, "ot", [128, D], F32, 2)
            outr = self.ring(st, "oo", [128, D], F32, 2)
            sqj = self.sb(st, "osq", [128, 512]); bsqj = Buf()
            str_ = self.ring(st, "ost", [128, 8], F32, 4)
            cur_stream = None
            tiles = list(range(NTILE)) if layer == 0 else list(range(2, NTILE))
            for t in tiles:
                stream = 1 if t < 2 else 0
                if stream != cur_stream:
                    bc, bbc = self.load_bc(st, "obc%d" % stream, layer, stream, [2])
                    cur_stream = stream
                r0 = t * 128
                yt, by = yr.next()
                fw.dma("sp", yt, yT[:, r0:r0 + 128].rearrange("(kc p) t -> p kc t", p=128), writes=[by])
                xt, bx = xr.next()
                if layer == 0:
                    srcap = I["ctx"][r0:r0 + 128, :] if t < 2 else I["x"][r0 - 256:r0 - 128, :]
                else:
                    srcap = self.res1[r0:r0 + 128, :]
                fw.dma("sp", xt, srcap, writes=[bx])
                pss = []
                stt, bst = str_.next()
                for n in range(2):
                    ps, bp = self.psr.next()
                    for kc in range(8):
                        fw.op("pe", lambda e: e.matmul(ps, lhsT=yt[:, kc, :], rhs=Wo[:, kc, n * 512:(n + 1) * 512], start=(kc == 0), stop=(kc == 7)),
                              reads=[by, bWo], writes=[bp], signal=(kc == 7))
                    fw.op("act", lambda e: e.activation(sqj, ps, AF.Square, accum_out=stt[:, n:n + 1]), reads=[bp], writes=[bsqj, bst])
                    pss.append((ps, bp))
                fw.op("dve", lambda e: e.tensor_tensor(stt[:, 2:3], stt[:, 0:1], stt[:, 1:2], op=ALU.add), reads=[bst], writes=[bst])
                self.rstd_col(stt[:, 2:3], bst, stt[:, 4:5], stt[:, 3:4], 1, 1.0 / D)
                tt, btt = tr.next()
                for n in range(2):
                    ps, bp = pss[n]
                    fw.op("dve", lambda e: e.scalar_tensor_tensor(tt[:, n * 512:(n + 1) * 512], ps, stt[:, 4:5], bc[:, 0, n * 512:(n + 1) * 512], op0=ALU.mult, op1=ALU.mult),
                          reads=[bp, bst, bbc], writes=[btt])
                ot, bo = outr.next()
                fw.op("pool", lambda e: e.tensor_tensor(ot, tt, xt, op=ALU.add), reads=[btt, bx], writes=[bo])
                fw.dma("sp", self.res1[r0:r0 + 128, :], ot, reads=[bo])
            fw.barrier()

    def mlp_weights(self, st, layer):
        fw, I = self.fw, self.I
        W1 = self.sb(st, "W1", [128, 8, 4 * D], BF16); bW1s = [Buf() for _ in range(8)]
        W2 = self.sb(st, "W2", [128, 32, D], BF16); bW2s = [Buf() for _ in range(8)]
        w1v = I["mlp_w1"][layer].rearrange("(kc p) n -> p kc n", p=128)
        w2v = I["mlp_w2"][layer].rearrange("(kc p) n -> p kc n", p=128)
        for kc in range(8):
            fw.dma("pool", W1[:, kc, :], w1v[:, kc, :], writes=[bW1s[kc]], pref=True)
        for k4 in range(0, 32, 4):
            fw.dma("pool", W2[:, k4:k4 + 4, :], w2v[:, k4:k4 + 4, :], writes=[bW2s[k4 // 4]], pref=True)
        return W1, bW1s, W2, bW2s

    def phase_mlp(self, layer, Wpre):
        nc, fw, I, S, cmb = self.nc, self.fw, self.I, self.S, self.cmb
        with ExitStack() as st:
            W1, bW1s, W2, bW2s = Wpre
            xr = self.ring(st, "mx", [128, D], F32, 4)
            tr = self.ring(st, "mt", [128, D], F32, 2)
            ulr = self.ring(st, "mul", [128, D], BF16, 2)
            uTr = self.ring(st, "muT", [128, 8, 256], BF16, 2)
            rlr = self.ring(st, "mrl", [128, 256], F32, 4)
            hr = self.ring(st, "mh", [128, 256], BF16, 8)
            vr = self.ring(st, "mv", [128, D], F32, 2)
            sqj = self.sb(st, "msq", [128, D], BF16); bsqj = Buf()
            str_ = self.ring(st, "mst", [128, 8], F32, 8)
            accr = self.psring([0, 1, 2, 3])
            wr = self.psring([4, 5, 6, 7])
            t0s = list(range(0, NTILE, 2)) if layer == 0 else list(range(2, NTILE, 2))
            stream_of = lambda t0: 1 if t0 < 2 else 0
            cur = {"stream": None, "bc": None}

            def pro_elem(t0):
                if stream_of(t0) != cur["stream"]:
                    cur["bc"] = self.load_bc(st, "mbc%d" % stream_of(t0), layer, stream_of(t0), [3, 4, 5])
                    cur["stream"] = stream_of(t0)
                bc, bbc = cur["bc"]
                xs = []
                uls = []
                uT, buT = uTr.next()
                for ti in range(2):
                    r0 = (t0 + ti) * 128
                    xt, bx = xr.next()
                    fw.dma("sp", xt, self.res1[r0:r0 + 128, :], writes=[bx])
                    xs.append((xt, bx))
                    stt, bst = str_.next()
                    fw.op("act", lambda e: e.activation(sqj, xt, AF.Square, accum_out=stt[:, 0:1]), reads=[bx], writes=[bsqj, bst])
                    self.rstd_col(stt[:, 0:1], bst, stt[:, 2:3], stt[:, 1:2], 1, 1.0 / D)
                    tt, btt = tr.next()
                    fw.op("dve", lambda e: e.scalar_tensor_tensor(tt, xt, stt[:, 2:3], bc[:, 0, :], op0=ALU.mult, op1=ALU.mult), reads=[bx, bst, bbc], writes=[btt])
                    ul, bul = ulr.next()
                    fw.op("pool", lambda e: e.tensor_tensor(ul, tt, bc[:, 1, :], op=ALU.add), reads=[btt, bbc], writes=[bul])
                    uls.append((ul, bul))
                return {"xs": xs, "uT": uT, "buT": buT, "uls": uls, "done": 0}

            def pro_pe(stn, upto_ti):
                uT, buT = stn["uT"], stn["buT"]
                while stn["done"] < min(upto_ti, 2):
                    ti = stn["done"]
                    ul, bul = stn["uls"][ti]
                    ps, bp = wr.next()
                    psb = ps.bitcast(BF16)
                    for kc in range(8):
                        fw.op("pe", lambda e: e.transpose(psb[:, kc * 128:(kc + 1) * 128], ul[:, kc * 128:(kc + 1) * 128], cmb[:, self.IDENT, :]),
                              reads=[bul, self.bcm], writes=[bp], signal=(kc == 7))
                    fw.op("act", lambda e: e.copy(uT[:, :, ti * 128:(ti + 1) * 128], psb.rearrange("p (k t) -> p k t", k=8)), reads=[bp], writes=[buT])
                    stn["done"] += 1

            def prologue(t0):
                stn = pro_elem(t0)
                pro_pe(stn, 2)
                return stn

            state = prologue(t0s[0])
            for idx, t0 in enumerate(t0s):
                xs, uT, buT = state["xs"], state["uT"], state["buT"]
                bc, bbc = cur["bc"]
                nxt = t0s[idx + 1] if idx + 1 < len(t0s) else None
                hoist = nxt is not None and stream_of(nxt) == cur["stream"]
                state = None
                accs = [accr.next() for _ in range(4)]
                pend = []

                def w1_group(j):
                    ps, bp = wr.next()
                    for kc in range(8):
                        fw.op("pe", lambda e: e.matmul(ps[:, 0:256], lhsT=W1[:, kc, j * 128:(j + 1) * 128], rhs=uT[:, kc, :], start=(kc == 0), stop=(kc == 7)),
                              reads=[bW1s[kc], buT], writes=[bp], signal=(kc == 7))
                    rl, brl = rlr.next()
                    fw.op("act", lambda e: e.activation(rl, ps[:, 0:256], AF.Relu), reads=[bp], writes=[brl])
                    hT, bh = hr.next()
                    fw.op("pool", lambda e: e.tensor_tensor(hT, rl, rl, op=ALU.mult), reads=[brl], writes=[bh])
                    pend.append((j, hT, bh))

                def w2_group():
                    j, hT, bh = pend.pop(0)
                    for ti in range(2):
                        for n in range(2):
                            acc, bacc = accs[ti * 2 + n]
                            fw.op("pe", lambda e: e.matmul(acc, lhsT=hT[:, ti * 128:(ti + 1) * 128], rhs=W2[:, j, n * 512:(n + 1) * 512], start=(j == 0), stop=(j == 31)),
                                  reads=[bh, bW2s[j // 4]], writes=[bacc], signal=(j == 31))

                for j in range(32):
                    w1_group(j)
                    if j == 2 and hoist:
                        state = pro_elem(nxt)
                    if j == 14 and hoist:
                        pro_pe(state, 1)
                    if j == 22 and hoist:
                        pro_pe(state, 2)
                    if len(pend) > 5:
                        w2_group()
                while pend:
                    w2_group()
                vs = []
                for ti in range(2):
                    v, bv = vr.next()
                    for n in range(2):
                        acc, bacc = accs[ti * 2 + n]
                        if n == 0:
                            fw.op("act", lambda e: e.copy(v[:, 0:512], acc), reads=[bacc], writes=[bv])
                        else:
                            fw.op("dve", lambda e: e.tensor_copy(v[:, 512:1024], acc), reads=[bacc], writes=[bv])
                    vs.append((v, bv))
                for ti in range(2):
                    r0 = (t0 + ti) * 128
                    xt, bx = xs[ti]
                    v, bv = vs[ti]
                    stt, bst = str_.next()
                    fw.op("act", lambda e: e.activation(sqj, v, AF.Square, accum_out=stt[:, 0:1]), reads=[bv], writes=[bsqj, bst])
                    self.rstd_col(stt[:, 0:1], bst, stt[:, 2:3], stt[:, 1:2], 1, 1.0 / D)
                    fw.op("dve", lambda e: e.scalar_tensor_tensor(v, v, stt[:, 2:3], bc[:, 2, :], op0=ALU.mult, op1=ALU.mult), reads=[bv, bst, bbc], writes=[bv])
                    fw.op("pool", lambda e: e.tensor_tensor(v, v, xt, op=ALU.add), reads=[bv, bx], writes=[bv])
                    dst = self.res1[r0:r0 + 128, :] if layer == 0 else self.out[r0 - 256:r0 - 128, :]
                    fw.dma("sp", dst, v, reads=[bv])
                if state is None and nxt is not None:
                    state = prologue(nxt)
            fw.barrier()

    def phase_mlstm(self):
        nc, fw, I, S, cm, cmb = self.nc, self.fw, self.I, self.S, self.cm, self.cmb
        S["xcT"] = self.dscr("xcT", [512, NT], BF16)
        S["qmT"] = self.dscr("qmT", [512, NT], BF16)
        S["kmT"] = self.dscr("kmT", [512, NT], BF16)
        S["vm"] = self.dscr("vm", [NT, 512], BF16)
        S["gl"] = self.dscr("gl", [NT, 32])
        with ExitStack() as st:
            xm = self.sb(st, "xm", [128, 4, NT], BF16); bxm = Buf()
            xc = self.sb(st, "xc", [128, 4, NT], BF16); bxc = [Buf() for _ in range(4)]
            fw.dma("sp", xm, S["xmT"].rearrange("(m p) t -> p m t", p=128), writes=[bxm])
            cw = self.sb(st, "cw", [128, 4, 4]); bcw = Buf()
            fw.dma("sp", cw, I["ml_conv"], writes=[bcw])
            BD = self.sb(st, "BD", [128, 3, 4, 128], BF16); bBD = Buf()
            fw.dma("pool", BD, I["ml_bd"].rearrange("t m k n -> k t m n"), writes=[bBD])
            wg = self.sb(st, "mwg", [128, 12, 32], BF16); bwg = Buf()
            fw.dma("pool", wg, I["ml_wg"].rearrange("c k n -> k c n"), writes=[bwg])
            bgb = self.sb(st, "bgb", [128, 32]); bbg = Buf()
            fw.dma("sp", bgb, I["ml_bg"].partition_broadcast(128), writes=[bbg])
            accr = self.ring(st, "cacc", [128, NT], F32, 2)
            segs = [(0, NCTX), (NCTX, NT)]
            for m in range(4):
                acc, bacc = accr.next()
                e1 = e2 = "dve"
                fw.op("dve", lambda e: e.tensor_scalar(acc, xm[:, m, :], cw[:, m, 1:2], cw[:, m, 3:4], op0=ALU.mult, op1=ALU.add), reads=[bxm, bcw], writes=[bacc])
                for (a, b) in segs:
                    fw.op(e1, lambda e: e.scalar_tensor_tensor(acc[:, a + 1:b], xm[:, m, a:b - 1], cw[:, m, 0:1], acc[:, a + 1:b], op0=ALU.mult, op1=ALU.add),
                          reads=[bxm, bcw, bacc], writes=[bacc])
                    fw.op(e2, lambda e: e.scalar_tensor_tensor(acc[:, a:b - 1], xm[:, m, a + 1:b], cw[:, m, 2:3], acc[:, a:b - 1], op0=ALU.mult, op1=ALU.add),
                          reads=[bxm, bcw, bacc], writes=[bacc])
                fw.op("act", lambda e: e.activation(xc[:, m, :], acc, AF.Silu), reads=[bacc], writes=[bxc[m]])
                fw.dma("sp", S["xcT"][m * 128:(m + 1) * 128, :], xc[:, m, :], reads=[bxc[m]])
            ginr = self.ring(st, "gin", [128, 12, 128], BF16, 2)
            vtr = self.ring(st, "vtk", [128, 512], BF16, 2)
            zall = self.sb(st, "zall", [128, NTILE, 32]); bz = Buf()
            for t in range(NTILE):
                r0 = t * 128
                gin, bgin = ginr.next()
                for ty in range(3):
                    src, bsrc = (xc, bxc) if ty < 2 else (xm, [bxm] * 4)
                    ps, bp = self.psr.next()
                    for m in range(4):
                        fw.op("pe", lambda e: e.matmul(ps[:, m * 128:(m + 1) * 128], lhsT=BD[:, ty, m, :], rhs=src[:, m, r0:r0 + 128], start=True, stop=True),
                              reads=[bBD, bsrc[m]], writes=[bp], signal=(m == 3))
                    fw.op("act" if ty != 1 else "dve", lambda e: (e.copy if ty != 1 else e.tensor_copy)(gin[:, ty * 4:(ty + 1) * 4, :], ps.rearrange("p (m t) -> p m t", m=4)),
                          reads=[bp], writes=[bgin])
                    if ty < 2:
                        dstT = S["qmT"] if ty == 0 else S["kmT"]
                        fw.dma("sp", dstT[:, r0:r0 + 128].rearrange("(m p) t -> p m t", p=128), gin[:, ty * 4:(ty + 1) * 4, :], reads=[bgin])
                ps, bp = self.psr.next()
                for m in range(4):
                    fw.op("pe", lambda e: e.matmul(ps[:, m * 128:(m + 1) * 128], lhsT=xm[:, m, r0:r0 + 128], rhs=BD[:, 2, m, :], start=True, stop=True),
                          reads=[bBD, bxm], writes=[bp], signal=(m == 3))
                vt, bvt = vtr.next()
                fw.op("dve", lambda e: e.tensor_copy(vt, ps), reads=[bp], writes=[bvt])
                fw.dma("sp", S["vm"][r0:r0 + 128, :], vt, reads=[bvt])
                ps, bp = self.psr.next()
                for ch in range(12):
                    fw.op("pe", lambda e: e.matmul(ps[:, 0:32], lhsT=gin[:, ch, :], rhs=wg[:, ch, :], start=(ch == 0), stop=(ch == 11)),
                          reads=[bgin, bwg], writes=[bp], signal=(ch == 11))
                fw.op("dve", lambda e: e.tensor_tensor(zall[:, t, :], ps[:, 0:32], bgb, op=ALU.add), reads=[bp, bbg], writes=[bz])
            glt = self.sb(st, "glt", [128, NTILE, 32]); bgl = Buf()
            ef = self.sb(st, "ef", [128, NTILE, 16]); bef = Buf()
            zv = zall.rearrange("p t (d k) -> p t d k", d=2)
            fw.op("act", lambda e: e.activation(ef.rearrange("p t (d h) -> p t d h", d=2), zv[:, :, :, 8:16], AF.Exp, scale=-1.0), reads=[bz], writes=[bef])
            fw.op("act", lambda e: e.activation(ef, ef, AF.Ln, bias=1.0), reads=[bef], writes=[bef])
            fw.op("dve", lambda e: e.tensor_scalar(glt[:, :, 0:16], ef, -1.0, None, op0=ALU.mult), reads=[bef], writes=[bgl])
            fw.op("pool", lambda e: e.tensor_copy(glt[:, :, 16:32].rearrange("p t (d h) -> p t d h", d=2), zv[:, :, :, 0:8]), reads=[bz], writes=[bgl])
            fw.dma("sp", S["gl"].rearrange("(c p) g -> p c g", p=128), glt, reads=[bgl])
            fw.barrier()
        self.phase_scan("ml")

    def phase_ret(self):
        self.phase_scan("ret")

    def phase_scan(self, kind):
        nc, fw, I, S, cm, cmb = self.nc, self.fw, self.I, self.S, self.cm, self.cmb
        ml = kind == "ml"
        Wd = 65 if ml else 64
        qsrc, ksrc, vsrc = (S["qmT"], S["kmT"], S["vm"]) if ml else (S["qDT"], S["kDT"], S["vD"])
        yT = S["yT1"]
        yv = (yT[0:512, :] if ml else yT[512:1024, :]).rearrange("(m p) t -> p m t", p=128)
        with ExitStack() as st:
            hacc = self.sb(st, "hacc", [128, NTILE, 512]); bh = [Buf() for _ in range(NTILE)]
            Cn = [self.sb(st, "Cn%d" % d, [128, 4, Wd]) for d in range(2)]; Cb = [self.sb(st, "Cb%d" % d, [128, 4, Wd], BF16) for d in range(2)]; bC = [Buf(), Buf()]
            ngb = self.sb(st, "ngb", [128, 64]); bng = Buf()
            qr = self.ring(st, "sq", [128, 4, 128], BF16, 8)
            kr = self.ring(st, "sk", [128, 4, 128], BF16, 8)
            vr = self.ring(st, "sv", [128, 512], BF16, 8)
            ktr = self.ring(st, "skt", [128, 512], BF16, 6)
            qbr = self.ring(st, "sqb", [128, 4, 2, 128], BF16, 6)
            for qa, qb_ in zip(qbr.aps, qbr.bufs):
                fw.op("pool", lambda e: e.memset(qa, 0.0), writes=[qb_])
            var = self.ring(st, "sva", [128, 8, Wd], BF16, 6)
            Amr = self.ring(st, "sAm", [128, 8, 128], BF16, 6)
            t32r = self.ring(st, "st32", [128, 512], F32, 3)
            ybr = self.ring(st, "syb", [128, 512], BF16, 2)
            yTr = self.ring(st, "syT", [128, 4, 128], BF16, 2)
            str_ = self.ring(st, "sst", [128, 24], F32, 2)
            gtr = self.ring(st, "sgt", [128, 40], F32, 8)
            if ml:
                fw.dma("sp", ngb, I["ml_ng"].partition_broadcast(128), writes=[bng])
                gl = self.sb(st, "gl", [128, NTILE, 32]); bgl = Buf()
                fw.dma("sp", gl, S["gl"].rearrange("(c p) g -> p c g", p=128), writes=[bgl])
                skip = self.sb(st, "skip", [128, 4]); bsk = Buf()
                fw.dma("sp", skip, I["ml_skip"], writes=[bsk])
                xcr = self.ring(st, "sxc", [128, 4, 128], BF16, 8)
                szr = self.ring(st, "ssz", [128, 4, 128], F32, 8)
                y32r = self.ring(st, "sy32", [128, 4, 128], F32, 2)
            else:
                gr = self.ring(st, "sg", [128, 512], F32, 8)
                rt = self.sb(st, "rt", [128, 16]); brt = Buf()
                jc = self.sb(st, "jc", [128, 2]); bjc = Buf()
                tabs = self.sb(st, "tabs", [128, 2, 24]); btab = Buf()
                fw.dma("sp", rt, I["ret_logit"].partition_broadcast(128), writes=[brt])
                fw.dma("sp", jc, I["jcol"], writes=[bjc])
                fw.op("act", lambda e: e.activation(rt, rt, AF.Exp, scale=-1.0), reads=[brt], writes=[brt])
                fw.op("act", lambda e: e.activation(rt, rt, AF.Ln, bias=1.0), reads=[brt], writes=[brt])
                Ft = self.sb(st, "Ft", [128, 16]); bF = Buf()
                for d in range(2):
                    fw.op("dve", lambda e: e.tensor_scalar(Ft[:, d * 8:(d + 1) * 8], rt[:, d * 8:(d + 1) * 8], jc[:, d:d + 1], None, op0=ALU.mult), reads=[brt, bjc], writes=[bF])
                    fw.op("act", lambda e: e.activation(tabs[:, d, 0:8], Ft[:, d * 8:(d + 1) * 8], AF.Exp, scale=1.0), reads=[bF], writes=[btab])
                    fw.op("act", lambda e: e.activation(tabs[:, d, 8:16], Ft[:, d * 8:(d + 1) * 8], AF.Exp, scale=-1.0), reads=[bF], writes=[btab])
                    fw.op("act", lambda e: e.activation(tabs[:, d, 16:24], rt[:, d * 8:(d + 1) * 8], AF.Exp, scale=-128.0), reads=[brt], writes=[btab])
            psr = self.psr
            orders = [list(range(NTILE)), [1, 0] + list(range(NTILE - 1, 1, -1))]
            pos = [{c: i for i, c in enumerate(o)} for o in orders]
            last_dir = {c: (1 if pos[1][c] >= pos[0][c] else 0) for c in range(NTILE)}
            for d in range(2):
                fw.op("dve", lambda e: e.memset(Cn[d], 0.0), writes=[bC[d]])
                fw.op("dve", lambda e: e.memset(Cb[d], 0.0), writes=[bC[d]])

            def make_dir(d):
                TRI = self.UINC if d == 0 else self.LINC

                def stageL(c):
                    r0 = c * 128
                    qT, bq = qr.next(); kT, bk = kr.next(); vt, bv = vr.next()
                    fw.dma("sp", qT, qsrc[:, r0:r0 + 128].rearrange("(m p) t -> p m t", p=128), writes=[bq])
                    fw.dma("sp", kT, ksrc[:, r0:r0 + 128].rearrange("(m p) t -> p m t", p=128), writes=[bk])
                    fw.dma("sp", vt, vsrc[r0:r0 + 128, :], writes=[bv])
                    xcc = bxcc = szc = bszc = gt = bg = None
                    if last_dir[c] == d:
                        if ml:
                            xcc, bxcc = xcr.next(); szc, bszc = szr.next()
                            fw.dma("sp", xcc, S["xcT"][:, r0:r0 + 128].rearrange("(m p) t -> p m t", p=128), writes=[bxcc])
                            fw.dma("sp", szc, S["szT"][:, r0:r0 + 128].rearrange("(m p) t -> p m t", p=128), writes=[bszc])
                        else:
                            gt, bg = gr.next()
                            fw.dma("sp", gt, S["gD"][r0:r0 + 128, :], writes=[bg])
                    return locals()

                def stageA(c, L):
                    r0, qT, bq, kT, bk, vt, bv = (L[k] for k in ("r0", "qT", "bq", "kT", "bk", "vt", "bv"))
                    xcc, bxcc, szc, bszc, gt, bg = (L[k] for k in ("xcc", "bxcc", "szc", "bszc", "gt", "bg"))
                    if ml:
                        gtt, bgt = gtr.next()
                        pg, bpg = psr.next()
                        lfv = gl[:, c, d * 8:(d + 1) * 8]
                        fw.op("pe", lambda e: e.matmul(pg[:, 0:8], lhsT=cm[:, TRI, :], rhs=lfv, start=True, stop=True), reads=[bgl, self.bcm], writes=[bpg], signal=False)
                        fw.op("pe", lambda e: e.matmul(pg[:, 8:16], lhsT=cm[:, self.ONES, :], rhs=lfv, start=True, stop=True), reads=[bgl, self.bcm], writes=[bpg])
                        fw.op("dve", lambda e: e.tensor_tensor(gtt[:, 24:32], gl[:, c, 16 + d * 8:16 + (d + 1) * 8], pg[:, 0:8], op=ALU.subtract), reads=[bgl, bpg], writes=[bgt])
                        fw.op("act", lambda e: e.activation(gtt[:, 0:8], gtt[:, 24:32], AF.Exp), reads=[bgt], writes=[bgt])
                        fw.op("act", lambda e: e.activation(gtt[:, 8:16], pg[:, 0:8], AF.Exp, bias=-math.log(8.0)), reads=[bpg], writes=[bgt])
                        fw.op("act", lambda e: e.activation(gtt[:, 16:24], pg[:, 8:16], AF.Exp), reads=[bpg], writes=[bgt])
                        av, cv, dv = gtt[:, 0:8], gtt[:, 8:16], gtt[:, 16:24]
                    else:
                        gtt, bgt = tabs, btab
                        av, cv, dv = tabs[:, d, 0:8], tabs[:, d, 8:16], tabs[:, d, 16:24]
                    ps, bp = psr.next()
                    psb = ps.bitcast(BF16)
                    for m in range(4):
                        fw.op("pe", lambda e: e.transpose(psb[:, m * 128:(m + 1) * 128], kT[:, m, :], cmb[:, self.IDENT, :]), reads=[bk, self.bcm], writes=[bp], signal=(m == 3))
                    kt, bkt = ktr.next()
                    fw.op("act", lambda e: e.copy(kt, psb[:, 0:512]), reads=[bp], writes=[bkt])
                    qb, bqb = qbr.next()
                    for hh in range(2):
                        sl = slice(hh * 64, (hh + 1) * 64)
                        fw.op("act", lambda e: e.copy(qb[sl, :, hh, :], qT[sl, :, :]), reads=[bq], writes=[bqb])
                    va, bva = var.next()
                    fw.op("dve", lambda e: e.tensor_tensor(va[:, :, 0:64], vt.rearrange("p (h d) -> p h d", d=64), av.unsqueeze(2).to_broadcast([128, 8, 64]), op=ALU.mult),
                          reads=[bv, bgt], writes=[bva])
                    if ml:
                        fw.op("pool", lambda e: e.tensor_copy(va[:, :, 64:65], av.unsqueeze(2)), reads=[bgt], writes=[bva])
                    Am, bAm = Amr.next()
                    for b2 in range(2):
                        pa, bpa = psr.next()
                        for mm in range(2):
                            m = b2 * 2 + mm
                            fw.op("pe", lambda e: e.matmul(pa[:, mm * 256:(mm + 1) * 256], lhsT=kT[:, m, :], rhs=qb[:, m, :, :].rearrange("p h t -> p (h t)"), start=True, stop=True),
                                  reads=[bk, bqb], writes=[bpa], signal=(mm == 1))
                        fw.op("dve", lambda e: e.tensor_tensor(Am[:, b2 * 4:(b2 + 1) * 4, :], pa.rearrange("p (h t) -> p h t", h=4),
                                                                cm[:, TRI, :].unsqueeze(1).to_broadcast([128, 4, 128]), op=ALU.mult), reads=[bpa, self.bcm], writes=[bAm])
                    return locals()

                def stageB(c, L):
                    r0, qb, bqb, va, bva, Am, bAm, kt, bkt, gtt, bgt, av, cv, dv = (L[k] for k in ("r0", "qb", "bqb", "va", "bva", "Am", "bAm", "kt", "bkt", "gtt", "bgt", "av", "cv", "dv"))
                    xcc, bxcc, szc, bszc, gt, bg = (L[k] for k in ("xcc", "bxcc", "szc", "bszc", "gt", "bg"))
                    first = last_dir[c] != d
                    pos_ = []
                    for b2 in range(2):
                        po, bpo = psr.next()
                        for hq in range(4):
                            h = b2 * 4 + hq
                            m, hh = h // 2, h % 2
                            fw.op("pe", lambda e: e.matmul(po[:, hq * Wd:(hq + 1) * Wd], lhsT=Am[:, h, :], rhs=va[:, h, :], start=True, stop=False),
                                  reads=[bAm, bva], writes=[bpo], signal=False)
                            fw.op("pe", lambda e: e.matmul(po[:, hq * Wd:(hq + 1) * Wd], lhsT=qb[:, m, hh, :], rhs=Cb[d][:, m, :], start=False, stop=True),
                                  reads=[bqb, bC[d]], writes=[bpo], signal=(hq == 3))
                        pos_.append((po, bpo))
                    hdst = hacc[:, c, :]
                    tmpo, btmp = (hdst, bh[c]) if first else t32r.next()
                    if ml:
                        for b2 in range(2):
                            po, bpo = pos_[b2]
                            pv = po[:, 0:4 * Wd].rearrange("p (h w) -> p h w", w=Wd)
                            fw.op("dve", lambda e: e.tensor_tensor(gtt[:, 24 + b2 * 4:28 + b2 * 4].unsqueeze(2), pv[:, :, 64:65], cv[:, b2 * 4:(b2 + 1) * 4].unsqueeze(2), op=ALU.mult),
                                  reads=[bpo, bgt], writes=[bgt])
                        fw.op("dve", lambda e: e.scalar_tensor_tensor(gtt[:, 32:40], gtt[:, 24:32], -1.0, gtt[:, 24:32], op0=ALU.mult, op1=ALU.max), reads=[bgt], writes=[bgt])
                        fw.op("dve", lambda e: e.tensor_scalar(gtt[:, 24:32], gtt[:, 32:40], 1.0, None, op0=ALU.max), reads=[bgt], writes=[bgt])
                        fw.op("dve", lambda e: e.reciprocal(gtt[:, 32:40], gtt[:, 24:32]), reads=[bgt], writes=[bgt])
                        fw.op("dve", lambda e: e.tensor_tensor(gtt[:, 32:40], gtt[:, 32:40], cv, op=ALU.mult), reads=[bgt], writes=[bgt])
                        scv = gtt[:, 32:40]
                    else:
                        scv = cv
                    for b2 in range(2):
                        po, bpo = pos_[b2]
                        pv = po[:, 0:4 * Wd].rearrange("p (h w) -> p h w", w=Wd)
                        fw.op("dve", lambda e: e.tensor_tensor(tmpo[:, b2 * 256:(b2 + 1) * 256].rearrange("p (h d) -> p h d", d=64), pv[:, :, 0:64],
                                                                scv[:, b2 * 4:(b2 + 1) * 4].unsqueeze(2).to_broadcast([128, 4, 64]), op=ALU.mult),
                              reads=[bpo, bgt], writes=[btmp])
                    if not first:
                        fw.op("pool", lambda e: e.tensor_tensor(hdst, hdst, tmpo, op=ALU.add), reads=[btmp, bh[c]], writes=[bh[c]])
                    for b2 in range(2):
                        pu, bpu = psr.next()
                        for mm in range(2):
                            m = b2 * 2 + mm
                            fw.op("pe", lambda e: e.matmul(pu[:, mm * 2 * Wd:(mm + 1) * 2 * Wd], lhsT=kt[:, m * 128:(m + 1) * 128], rhs=va[:, 2 * m:2 * m + 2, :].rearrange("p h w -> p (h w)"), start=True, stop=True),
                                  reads=[bkt, bva], writes=[bpu], signal=(mm == 1))
                        puv = pu[:, 0:4 * Wd].rearrange("p (m h w) -> p m h w", m=2, h=2)
                        for hh in range(2):
                            sl = slice(hh * 64, (hh + 1) * 64)
                            fw.op("dve", lambda e: e.tensor_tensor(Cn[d][sl, b2 * 2:b2 * 2 + 2, :], Cn[d][sl, b2 * 2:b2 * 2 + 2, :], puv[sl, :, hh, :], op=ALU.add), reads=[bpu, bC[d]], writes=[bC[d]])
                    for hh in range(2):
                        sl = slice(hh * 64, (hh + 1) * 64)
                        dvv = dv[sl, hh:8:2].unsqueeze(2).to_broadcast([64, 4, Wd])
                        fw.op("pool", lambda e: e.tensor_tensor(Cn[d][sl, :, :], Cn[d][sl, :, :], dvv, op=ALU.mult), reads=[bC[d], bgt], writes=[bC[d]])
                    fw.op("act", lambda e: e.copy(Cb[d], Cn[d]), reads=[bC[d]], writes=[bC[d]])
                    if not first:
                        if ml:
                            def post(psb_, bp_, dst):
                                y32, by32 = y32r.next()
                                for m in range(4):
                                    fw.op("dve", lambda e: e.scalar_tensor_tensor(y32[:, m, :], xcc[:, m, :], skip[:, m:m + 1], psb_[:, m * 128:(m + 1) * 128], op0=ALU.mult, op1=ALU.add),
                                          reads=[bp_, bxcc, bsk], writes=[by32])
                                yTt, byT = yTr.next()
                                fw.op("pool", lambda e: e.tensor_tensor(yTt, y32, szc, op=ALU.mult), reads=[by32, bszc], writes=[byT])
                                fw.dma("sp", dst, yTt, reads=[byT])
                            self.finalize_tok(hdst, bh[c], ngb.unsqueeze(1).to_broadcast([128, 8, 64]), bng, t32r, str_, ybr, yTr, yv[:, :, r0:r0 + 128], post=post)
                        else:
                            self.finalize_tok(hdst, bh[c], gt, bg, t32r, str_, ybr, yTr, yv[:, :, r0:r0 + 128])

                return stageL, stageA, stageB

            self.scan_driver(orders, [make_dir(0), make_dir(1)])
            fw.barrier()

    IDENT, UINC, LINC, USTR, LSTR, ONES, BLK, BLK32 = range(8)

    def phase_ada(self):
        nc, fw, I = self.nc, self.fw, self.I
        self.modv = self.dscr("modv", [2, 2, 6, D])
        with ExitStack() as st:
            cc = self.sb(st, "cc", [128, 8, 2]); bcc = Buf()
            sc = self.sb(st, "sc", [128, 8, 2]); bsc = Buf()
            fw.dma("sp", cc, I["ccols"], writes=[bcc])
            fw.op("act", lambda e: e.activation(sc, cc, AF.Silu), reads=[bcc], writes=[bsc])
            wr = self.ring(st, "adw", [128, 8, 512], F32, 2)
            mod = self.sb(st, "mod", [2, 6 * D]); bmod = Buf()
            ab = self.sb(st, "adb", [2, 6 * D]); bab = Buf()
            ng = self.sb(st, "ng", [2, 4 * D]); bng = Buf()
            der = self.sb(st, "der", [2, 6 * D]); bder = Buf()
            for layer in range(2):
                fw.dma("sp", ab, I["ada_b"][layer:layer + 1, :].partition_broadcast(2), writes=[bab])
                fw.dma("sp", ng, I["norm_g"][layer:layer + 1].rearrange("o j d -> o (j d)").partition_broadcast(2), writes=[bng])
                wv = I["ada_w"][layer].rearrange("(kc p) n -> p kc n", p=128)
                for nb in range(12):
                    wt, bw = wr.next()
                    fw.dma("sp", wt, wv[:, :, nb * 512:(nb + 1) * 512], writes=[bw])
                    ps, bp = self.psr.next()
                    for kc in range(8):
                        fw.op("pe", lambda e: e.matmul(ps[0:2, :], lhsT=sc[:, kc, :], rhs=wt[:, kc, :], start=(kc == 0), stop=(kc == 7)),
                              reads=[bsc, bw], writes=[bp], signal=(kc == 7))
                    fw.op("dve", lambda e: e.tensor_tensor(mod[:, nb * 512:(nb + 1) * 512], ps[0:2, :], ab[:, nb * 512:(nb + 1) * 512], op=ALU.add),
                          reads=[bp, bab], writes=[bmod])
                m = lambda j: mod[:, j * D:(j + 1) * D]
                g = lambda j: ng[:, j * D:(j + 1) * D]
                dd = lambda j: der[:, j * D:(j + 1) * D]
                rw = dict(reads=[bmod, bng], writes=[bder])
                fw.op("dve", lambda e: e.scalar_tensor_tensor(dd(0), m(1), 1.0, g(0), op0=ALU.add, op1=ALU.mult), **rw)
                fw.op("dve", lambda e: e.tensor_copy(dd(1), m(0)), **rw)
                fw.op("dve", lambda e: e.tensor_tensor(dd(2), m(2), g(1), op=ALU.mult), **rw)
                fw.op("dve", lambda e: e.scalar_tensor_tensor(dd(3), m(4), 1.0, g(2), op0=ALU.add, op1=ALU.mult), **rw)
                fw.op("dve", lambda e: e.tensor_copy(dd(4), m(3)), **rw)
                fw.op("dve", lambda e: e.tensor_tensor(dd(5), m(5), g(3), op=ALU.mult), **rw)
                fw.dma("sp", self.modv[layer].rearrange("s j d -> s (j d)"), der, reads=[bder])
            fw.barrier()

    def load_bc(self, st, name, layer, stream, js):
        key = (id(st), name[:-1])
        if not hasattr(self, "_bc"):
            self._bc = {}
        if key not in self._bc:
            self._bc[key] = (self.sb(st, name, [128, len(js), D]), Buf())
        t, b = self._bc[key]
        for i, j in enumerate(js):
            self.fw.dma("sp", t[:, i, :], self.modv[layer, stream, j:j + 1, :].partition_broadcast(128), writes=[b])
        return t, b

    def rstd_col(self, ss, bss, out, tmp, n, scale):
        fw = self.fw
        fw.op("dve", lambda e: e.tensor_scalar(tmp, ss, scale, EPS, op0=ALU.mult, op1=ALU.add), reads=[bss], writes=[bss])
        fw.op("act", lambda e: e.activation(tmp, tmp, AF.Ln), reads=[bss], writes=[bss])
        fw.op("act", lambda e: e.activation(out, tmp, AF.Exp, scale=-0.5), reads=[bss], writes=[bss])

    def inproj_weights(self, st, layer, pref=False):
        WC = W0C if layer == 0 else W1C
        wsrc = self.I["w_in0"] if layer == 0 else self.I["w_in1"]
        W = self.sb(st, "Win", [128, 8, WC], BF16); bWs = [Buf() for _ in range(8)]
        wv = wsrc.rearrange("(kc p) n -> p kc n", p=128)
        for kc in range(8):
            self.fw.dma("pool", W[:, kc, :], wv[:, kc, :], writes=[bWs[kc]], pref=pref)
        return W, bWs

    def phase_inproj(self, layer, Wpre=None):
        nc, fw, I = self.nc, self.fw, self.I
        cmb = self.cmb
        WC = W0C if layer == 0 else W1C
        wsrc = I["w_in0"] if layer == 0 else I["w_in1"]
        src_res = None if layer == 0 else self.res1
        S = {}
        if layer == 0:
            S["qAT"] = self.dscr("qAT", [256, NT]); S["kAT"] = self.dscr("kAT", [256, NT])
            S["kA"] = self.dscr("kA", [NT, 256]); S["vA"] = self.dscr("vA", [NT, 512], BF16)
            S["gA"] = self.dscr("gA", [NT, 512]); S["lgp"] = self.dscr("lgp", [NT, 512])
            S["qBT"] = self.dscr("qBT", [512, NT], BF16); S["kBT"] = self.dscr("kBT", [128, NT], BF16)
            S["vB"] = self.dscr("vB", [NT, 128], BF16)
        else:
            S["xmT"] = self.dscr("xmT", [512, NT], BF16); S["szT"] = self.dscr("szT", [512, NT])
            S["qDT"] = self.dscr("qDT", [512, NT], BF16); S["kDT"] = self.dscr("kDT", [512, NT], BF16)
            S["vD"] = self.dscr("vD", [NT, 512], BF16); S["gD"] = self.dscr("gD", [NT, 512])
        self.S.update(S)
        with ExitStack() as st:
            if Wpre is not None:
                W, bWs = Wpre
            else:
                W, bWs = self.inproj_weights(st, layer)
            rope = self.sb(st, "rope", [128, 2, NT]); brope = Buf()
            fw.dma("sp", rope, I["rope"].rearrange("n p f -> p n f"), writes=[brope])
            small = self.sb(st, "small", [128, 16]); bsmall = Buf()
            ngbc = self.sb(st, "ngbc", [128, 64]); bng = Buf()
            if layer == 0:
                fw.dma("sp", small[:, 0:4], I["att_g"], writes=[bsmall])
                fw.dma("sp", ngbc, I["gla_ng"].partition_broadcast(128), writes=[bng])
                wg = self.sb(st, "wg", [17, 2, 256]); bwg = Buf()
                fw.dma("sp", wg, I["gla_wg"].rearrange("d k n -> k d n"), writes=[bwg])
                lra = [self.sb(st, "lra%d" % d, [32, 512]) for d in range(2)]
                blra = [Buf(), Buf()]
                for d in range(2):
                    fw.op("pool", lambda e: e.memset(lra[d], 1.0), writes=[blra[d]])
            else:
                fw.dma("sp", ngbc, I["ret_ng"].partition_broadcast(128), writes=[bng])
            xr = self.ring(st, "xt", [128, D], F32, 2)
            sqj = self.sb(st, "sqj", [128, D]); bsqj = Buf()
            xnr = self.ring(st, "xn", [128, D], F32, 2)
            ulr = self.ring(st, "ul", [128, D], BF16, 5)
            ulTr = self.ring(st, "ulT", [128, 8, 512], BF16, 2)
            str_ = self.ring(st, "stat", [128, 4], F32, 4)
            f32r = self.ring(st, "ev32", [128, 512], F32, 4)
            f32r2 = self.ring(st, "ev32b", [128, 512], F32, 4)
            bfr = self.ring(st, "evbf", [128, 512], BF16, 4)
            supers = [(0, 2)] + [(2 + 4 * i, 4) for i in range(8)]
            cur = {"stream": None, "bc": None}

            def pro_elem(t0, ntl):
                stream = 1 if t0 < 2 else 0
                if stream != cur["stream"]:
                    cur["bc"] = self.load_bc(st, "bc%d" % stream, layer, stream, [0, 1])
                    cur["stream"] = stream
                bc, bbc = cur["bc"]
                ulT, bulT = ulTr.next()
                uls = []
                for ti in range(ntl):
                    t = t0 + ti
                    xt, bx = xr.next()
                    if layer == 0:
                        srcap = I["ctx"][t * 128:(t + 1) * 128, :] if t < 2 else I["x"][(t - 2) * 128:(t - 1) * 128, :]
                    else:
                        srcap = src_res[t * 128:(t + 1) * 128, :]
                    fw.dma("sp", xt, srcap, writes=[bx])
                    stt, bst = str_.next()
                    fw.op("act", lambda e: e.activation(sqj, xt, AF.Square, accum_out=stt[:, 0:1]), reads=[bx], writes=[bsqj, bst])
                    self.rstd_col(stt[:, 0:1], bst, stt[:, 2:3], stt[:, 1:2], 1, 1.0 / D)
                    xn, bxn = xnr.next()
                    fw.op("dve", lambda e: e.scalar_tensor_tensor(xn, xt, stt[:, 2:3], bc[:, 0, :], op0=ALU.mult, op1=ALU.mult),
                          reads=[bx, bst, bbc], writes=[bxn])
                    ul, bul = ulr.next()
                    fw.op("pool", lambda e: e.tensor_tensor(ul, xn, bc[:, 1, :], op=ALU.add), reads=[bxn, bbc], writes=[bul])
                    uls.append((ul, bul))
                return {"ulT": ulT, "bulT": bulT, "uls": uls, "ntl": ntl, "done": 0}

            def pro_pe(stn, upto_ti):
                ulT, bulT = stn["ulT"], stn["bulT"]
                while stn["done"] < min(upto_ti, stn["ntl"]):
                    ti = stn["done"]
                    ul, bul = stn["uls"][ti]
                    ps, bp = self.psr.next()
                    psb = ps.bitcast(BF16)
                    for kc in range(8):
                        fw.op("pe", lambda e: e.transpose(psb[:, kc * 128:(kc + 1) * 128], ul[:, kc * 128:(kc + 1) * 128], cmb[:, self.IDENT, :]),
                              reads=[bul, self.bcm], writes=[bp], signal=(kc == 7))
                    fw.op("act", lambda e: e.copy(ulT[:, :, ti * 128:(ti + 1) * 128], psb.rearrange("p (k t) -> p k t", k=8)),
                          reads=[bp], writes=[bulT])
                    stn["done"] += 1

            nstate = pro_elem(*supers[0])
            pro_pe(nstate, 99)
            for si, (t0, ntl) in enumerate(supers):
                ulT, bulT = nstate["ulT"], nstate["bulT"]
                nstate = None
                N = ntl * 128
                c0 = t0 * 128

                def fm(col0, M):
                    ps, bp = self.psr.next()
                    for kc in range(8):
                        fw.op("pe", lambda e: e.matmul(ps[0:M, 0:N], lhsT=W[:, kc, col0:col0 + M], rhs=ulT[:, kc, 0:N], start=(kc == 0), stop=(kc == 7)),
                              reads=[bWs[kc], bulT], writes=[bp], signal=(kc == 7))
                    return ps, bp

                def tm(ti, col0, ncol):
                    ps, bp = self.psr.next()
                    for kc in range(8):
                        fw.op("pe", lambda e: e.matmul(ps[:, 0:ncol], lhsT=ulT[:, kc, ti * 128:(ti + 1) * 128], rhs=W[:, kc, col0:col0 + ncol], start=(kc == 0), stop=(kc == 7)),
                              reads=[bWs[kc], bulT], writes=[bp], signal=(kc == 7))
                    return ps, bp

                def rope_fm(col0, colp, gi, dst, scale, norm):
                    ps, bp = fm(col0, 128)
                    pp, bpp = fm(colp, 128)
                    t1, b1 = f32r.next()
                    t2, b2 = f32r2.next()
                    ob, bo = bfr.next()
                    cosv = rope[:, 0, c0:c0 + N]
                    sinv = rope[:, 1, c0:c0 + N]
                    if norm:
                        sq, bsq = bfr.next()
                        fw.op("act", lambda e: e.activation(sq[:, 0:N], ps[:, 0:N], AF.Square), reads=[bp], writes=[bsq])
                        ps2, bp2 = self.psr.next()
                        fw.op("pe", lambda e: e.matmul(ps2[:, 0:N], lhsT=cmb[:, self.BLK, :], rhs=sq[:, 0:N], start=True, stop=True),
                              reads=[bsq, self.bcm], writes=[bp2])
                        rs, brs = f32r.next()
                        fw.op("dve", lambda e: e.tensor_scalar(rs[:, 0:N], ps2[:, 0:N], 1.0 / 64, EPS, op0=ALU.mult, op1=ALU.add), reads=[bp2], writes=[brs])
                        fw.op("act", lambda e: e.activation(rs[:, 0:N], rs[:, 0:N], AF.Ln), reads=[brs], writes=[brs])
                        fw.op("act", lambda e: e.activation(rs[:, 0:N], rs[:, 0:N], AF.Exp, scale=-0.5), reads=[brs], writes=[brs])
                        fw.op("dve", lambda e: e.scalar_tensor_tensor(t1[:, 0:N], ps[:, 0:N], small[:, gi:gi + 1], cosv, op0=ALU.mult, op1=ALU.mult),
                              reads=[bp, bsmall, brope], writes=[b1])
                        fw.op("dve", lambda e: e.scalar_tensor_tensor(t2[:, 0:N], pp[:, 0:N], small[:, gi + 1:gi + 2], sinv, op0=ALU.mult, op1=ALU.mult),
                              reads=[bpp, bsmall, brope], writes=[b2])
                        fw.op("pool", lambda e: e.tensor_tensor(t1[:, 0:N], t1[:, 0:N], t2[:, 0:N], op=ALU.add), reads=[b1, b2], writes=[b1])
                        fw.op("pool", lambda e: e.tensor_tensor(ob[:, 0:N], t1[:, 0:N], rs[:, 0:N], op=ALU.mult), reads=[b1, brs], writes=[bo])
                    else:
                        fw.op("dve", lambda e: e.scalar_tensor_tensor(t1[:, 0:N], ps[:, 0:N], scale, cosv, op0=ALU.mult, op1=ALU.mult), reads=[bp, brope], writes=[b1])
                        fw.op("dve", lambda e: e.scalar_tensor_tensor(t2[:, 0:N], pp[:, 0:N], scale, sinv, op0=ALU.mult, op1=ALU.mult), reads=[bpp, brope], writes=[b2])
                        fw.op("pool", lambda e: e.tensor_tensor(ob[:, 0:N], t1[:, 0:N], t2[:, 0:N], op=ALU.add), reads=[b1, b2], writes=[bo])
                    fw.dma("sp", dst, ob[:, 0:N], reads=[bo])

                if layer == 0:
                    for j in range(2):
                        ps, bp = fm(0 + j * 128, 128)
                        t1, b1 = f32r.next()
                        fw.op("act", lambda e: e.activation(t1[:, 0:N], ps[:, 0:N], AF.Copy, scale=32 ** -0.5), reads=[bp], writes=[b1])
                        fw.dma("sp", S["qAT"][j * 128:(j + 1) * 128, c0:c0 + N], t1[:, 0:N], reads=[b1])
                        ps, bp = fm(256 + j * 128, 128)
                        t1, b1 = f32r.next()
                        fw.op("dve", lambda e: e.tensor_copy(t1[:, 0:N], ps[:, 0:N]), reads=[bp], writes=[b1])
                        fw.dma("sp", S["kAT"][j * 128:(j + 1) * 128, c0:c0 + N], t1[:, 0:N], reads=[b1])
                    for d in range(2):
                        ps, bp = fm(1536 + 16 * d, 16)
                        fw.op("dve", lambda e: e.tensor_copy(lra[d][0:16, 0:N], ps[0:16, 0:N]), reads=[bp], writes=[blra[d]])
                    for m in range(4):
                        rope_fm(1568 + m * 128, 2336 + m * 128, 0, S["qBT"][m * 128:(m + 1) * 128, c0:c0 + N], 1.0, True)
                    rope_fm(2080, 2336 + 512, 2, S["kBT"][:, c0:c0 + N], 1.0, True)
                    if si + 1 < len(supers):
                        nstate = pro_elem(*supers[si + 1])
                    for ti in range(ntl):
                        r0 = c0 + ti * 128
                        ps, bp = tm(ti, 256, 256)
                        t1, b1 = f32r.next()
                        fw.op("act", lambda e: e.copy(t1[:, 0:256], ps[:, 0:256]), reads=[bp], writes=[b1])
                        fw.dma("sp", S["kA"][r0:r0 + 128, :], t1[:, 0:256], reads=[b1])
                        ps, bp = tm(ti, 2208, 128)
                        ob, bo = bfr.next()
                        fw.op("dve", lambda e: e.tensor_copy(ob[:, 0:128], ps[:, 0:128]), reads=[bp], writes=[bo])
                        fw.dma("sp", S["vB"][r0:r0 + 128, :], ob[:, 0:128], reads=[bo])
                        ps, bp = tm(ti, 512, 512)
                        ob, bo = bfr.next()
                        fw.op("act", lambda e: e.copy(ob, ps), reads=[bp], writes=[bo])
                        fw.dma("sp", S["vA"][r0:r0 + 128, :], ob, reads=[bo])
                        ps, bp = tm(ti, 1024, 512)
                        t1, b1 = f32r.next()
                        fw.op("act", lambda e: e.activation(t1, ps, AF.Silu), reads=[bp], writes=[b1])
                        t2, b2 = f32r2.next()
                        fw.op("pool", lambda e: e.tensor_tensor(t2.rearrange("p (h d) -> p h d", d=64), t1.rearrange("p (h d) -> p h d", d=64),
                                                                 ngbc.unsqueeze(1).to_broadcast([128, 8, 64]), op=ALU.mult), reads=[b1, bng], writes=[b2])
                        fw.dma("sp", S["gA"][r0:r0 + 128, :], t2, reads=[b2])
                        ps, bp = self.psr.next()
                        for d in range(2):
                            fw.op("pe", lambda e: e.matmul(ps[:, d * 256:(d + 1) * 256], lhsT=lra[d][0:17, ti * 128:(ti + 1) * 128], rhs=wg[:, d, :], start=True, stop=True),
                                  reads=[blra[d], bwg], writes=[bp], signal=(d == 1))
                        t1, b1 = f32r.next()
                        fw.op("act", lambda e: e.activation(t1, ps, AF.Exp, scale=-1.0), reads=[bp], writes=[b1])
                        t2, b2 = f32r2.next()
                        fw.op("act", lambda e: e.activation(t2, t1, AF.Ln, bias=1.0), reads=[b1], writes=[b2])
                        fw.dma("sp", S["lgp"][r0:r0 + 128, :], t2, reads=[b2])
                        if nstate is not None:
                            pro_pe(nstate, ti + 1)
                    if nstate is not None:
                        pro_pe(nstate, 99)
                else:
                    for j in range(4):
                        ps, bp = fm(0 + j * 128, 128)
                        ob, bo = bfr.next()
                        fw.op("act", lambda e: e.copy(ob[:, 0:N], ps[:, 0:N]), reads=[bp], writes=[bo])
                        fw.dma("sp", S["xmT"][j * 128:(j + 1) * 128, c0:c0 + N], ob[:, 0:N], reads=[bo])
                        ps, bp = fm(512 + j * 128, 128)
                        t1, b1 = f32r.next()
                        fw.op("act", lambda e: e.activation(t1[:, 0:N], ps[:, 0:N], AF.Silu), reads=[bp], writes=[b1])
                        fw.dma("sp", S["szT"][j * 128:(j + 1) * 128, c0:c0 + N], t1[:, 0:N], reads=[b1])
                        rope_fm(1024 + j * 128, 3072 + j * 128, 0, S["qDT"][j * 128:(j + 1) * 128, c0:c0 + N], 0.125, False)
                        rope_fm(1536 + j * 128, 3072 + 512 + j * 128, 0, S["kDT"][j * 128:(j + 1) * 128, c0:c0 + N], 1.0, False)
                    if si + 1 < len(supers):
                        nstate = pro_elem(*supers[si + 1])
                    for ti in range(ntl):
                        r0 = c0 + ti * 128
                        ps, bp = tm(ti, 2048, 512)
                        ob, bo = bfr.next()
                        fw.op("act", lambda e: e.copy(ob, ps), reads=[bp], writes=[bo])
                        fw.dma("sp", S["vD"][r0:r0 + 128, :], ob, reads=[bo])
                        ps, bp = tm(ti, 2560, 512)
                        t1, b1 = f32r.next()
                        fw.op("act", lambda e: e.activation(t1, ps, AF.Silu), reads=[bp], writes=[b1])
                        t2, b2 = f32r2.next()
                        fw.op("pool", lambda e: e.tensor_tensor(t2.rearrange("p (h d) -> p h d", d=64), t1.rearrange("p (h d) -> p h d", d=64),
                                                                 ngbc.unsqueeze(1).to_broadcast([128, 8, 64]), op=ALU.mult), reads=[b1, bng], writes=[b2])
                        fw.dma("sp", S["gD"][r0:r0 + 128, :], t2, reads=[b2])
                        if nstate is not None:
                            pro_pe(nstate, ti + 1)
                    if nstate is not None:
                        pro_pe(nstate, 99)
            fw.barrier()


def _perm64():
    p = np.arange(64)
    return np.concatenate([p[16:32], p[0:16], p[48:64], p[32:48]])


def _rope_tables():
    axis_dim = 32
    inv = 10000.0 ** (-np.arange(0, axis_dim, 2, dtype=np.float32) / axis_dim)
    t = np.arange(NLAT)
    ang_r = (t // 64).astype(np.float32)[:, None] * inv[None, :]
    ang_c = (t % 64).astype(np.float32)[:, None] * inv[None, :]
    cos = np.concatenate([np.cos(ang_r), np.cos(ang_r), np.cos(ang_c), np.cos(ang_c)], axis=1)
    sin = np.concatenate([-np.sin(ang_r), np.sin(ang_r), -np.sin(ang_c), np.sin(ang_c)], axis=1)
    tab = np.zeros((2, 128, NT), np.float32)
    tab[0, :, :NCTX] = 1.0
    for hh in range(2):
        tab[0, hh * 64:(hh + 1) * 64, NCTX:] = cos.T
        tab[1, hh * 64:(hh + 1) * 64, NCTX:] = sin.T
    return tab


def _cmat():
    j = np.arange(128)[:, None]
    i = np.arange(128)[None, :]
    m = np.zeros((8, 128, 128), np.float32)
    m[0] = (j == i)
    m[1] = (j <= i)
    m[2] = (j >= i)
    m[3] = (j < i)
    m[4] = (j > i)
    m[5] = 1.0
    m[6] = ((j // 64) == (i // 64))
    m[7] = ((j // 32) == (i // 32))
    return m


def host_inputs(inp):
    f = lambda a: np.ascontiguousarray(np.asarray(a, dtype=np.float32))
    perm = _perm64()
    ab = f(inp["ab_w_in"][0])
    qB = ab[:, 1568:2080].reshape(D, 8, 64)[:, :, perm].reshape(D, 512)
    kB = ab[:, 2080:2208].reshape(D, 2, 64)[:, :, perm].reshape(D, 128)
    w_in0 = np.concatenate([ab, qB, kB], axis=1)
    cd = f(inp["cd_w_in"][0])
    qD = cd[:, 1024:1536].reshape(D, 8, 64)[:, :, perm].reshape(D, 512)
    kD = cd[:, 1536:2048].reshape(D, 8, 64)[:, :, perm].reshape(D, 512)
    w_in1 = np.concatenate([cd, qD, kD], axis=1)
    gla_wg = np.concatenate([f(inp["gla_w_gate"][0]), f(inp["gla_b_gate"][0])[:, None, :]], axis=1)
    aq = f(inp["att_qk_norm_g"][0])
    att_g = np.stack([np.tile(aq[0], 2), np.tile(aq[0][perm], 2), np.tile(aq[1], 2), np.tile(aq[1][perm], 2)], axis=1)
    cw = f(inp["ml_conv_w"][0])
    cb = f(inp["ml_conv_b"][0])
    ml_conv = np.stack([cw[0], cw[1], cw[2], cb], axis=1).reshape(4, 128, 4).transpose(1, 0, 2)
    wq = f(inp["ml_w_qkv"][0])
    ml_bd = np.zeros((3, 4, 128, 128), np.float32)
    for t in range(3):
        for h in range(8):
            m, hh = h // 2, h % 2
            ml_bd[t, m, hh * 64:(hh + 1) * 64, hh * 64:(hh + 1) * 64] = wq[t, h]
    mg = f(inp["ml_w_gate"][0])
    ml_wg = np.concatenate([mg[0], mg[1]], axis=1).reshape(12, 128, 32)
    ml_bg = f(inp["ml_b_gate"][0]).reshape(1, 32)
    ml_skip = f(inp["ml_skip"][0]).reshape(4, 128).T
    jj = np.arange(128, dtype=np.float32)
    jcol = np.stack([jj + 1, 128 - jj], axis=1)
    shared = {
        "ada_w": f(inp["ada_w"]), "ada_b": f(inp["ada_b"]), "norm_g": f(inp["norm_g"]), "w_out": f(inp["w_out"]),
        "mlp_w1": f(inp["mlp_w1"]), "mlp_w2": f(inp["mlp_w2"]), "w_in0": f(w_in0), "w_in1": f(w_in1),
        "gla_wg": f(gla_wg), "gla_ng": f(inp["gla_norm_g"]).reshape(1, 64), "att_g": f(att_g), "ml_conv": f(ml_conv),
        "ml_bd": ml_bd, "ml_wg": f(ml_wg), "ml_bg": ml_bg, "ml_ng": f(inp["ml_norm_g"]).reshape(1, 64), "ml_skip": f(ml_skip),
        "ret_logit": f(inp["ret_decay_logit"]).reshape(1, 16), "ret_ng": f(inp["ret_norm_g"]).reshape(1, 64),
        "cmat": _cmat(), "rope": _rope_tables(), "jcol": f(jcol),
    }
    maps = []
    x = np.asarray(inp["x"]); c = np.asarray(inp["c"]); ctx = np.asarray(inp["ctx"]); cc = np.asarray(inp["c_ctx"])
    for b in range(x.shape[0]):
        m = dict(shared)
        m["x"] = f(x[b]); m["ctx"] = f(ctx[b])
        m["ccols"] = f(np.stack([c[b].reshape(8, 128).T, cc.reshape(8, 128).T], axis=2))
        maps.append(m)
    return maps


_PROG = {}


def kernel(**inputs):
    maps = host_inputs(inputs)
    if "p" not in _PROG:
        p = Prog()
        p.build()
        _PROG["p"] = p
    p = _PROG["p"]
    res = run_bass_kernel_spmd(p.nc, maps, core_ids=list(range(8)))
    return np.stack([np.asarray(r["out"], dtype=np.float32) for r in res.results], axis=0)
```

```python
import math
from contextlib import ExitStack

import numpy as np
import concourse.bass as bass
import concourse.mybir as mybir
from concourse.bass_utils import run_bass_kernel_spmd

F32 = mybir.dt.float32
BF16 = mybir.dt.bfloat16
ALU = mybir.AluOpType
AF = mybir.ActivationFunctionType
AX = mybir.AxisListType

D = 1024
NCTX = 256
NLAT = 4096
NT = NCTX + NLAT
NTILE = NT // 128
EPS = 1e-6
W0C = 2336 + 512 + 128
W1C = 3072 + 512 + 512


class Buf:
    __slots__ = ("w", "r")

    def __init__(self):
        self.w = None
        self.r = {}


class FW:
    NDMA = 24

    def __init__(self, nc, stack):
        self.nc = nc
        self.eng = {"pe": nc.tensor, "act": nc.scalar, "dve": nc.vector, "pool": nc.gpsimd, "sp": nc.sync}
        self.sems = {}
        self.cnt = {}
        for k in ("pe", "act", "dve", "pool"):
            self.sems[k] = stack.enter_context(nc.semaphore("s_" + k))
            self.cnt[k] = 0
        for i in range(self.NDMA):
            k = "d%d" % i
            self.sems[k] = stack.enter_context(nc.semaphore("s_" + k))
            self.cnt[k] = 0
        self.rr = 0
        self.seen = {e: {} for e in self.eng}
        self.nins = 0

    def wait(self, e, ticket):
        if ticket is None:
            return
        k, v = ticket
        if self.seen[e].get(k, 0) >= v:
            return
        self.eng[e].wait_ge(self.sems[k], v)
        self.seen[e][k] = v

    def deps(self, e, reads, writes):
        for b in reads:
            self.wait(e, b.w)
        for b in writes:
            if b.w is not None and b.w[0] != e:
                self.wait(e, b.w)
            for k, v in b.r.items():
                if k != e:
                    self.wait(e, (k, v))

    def mark(self, ticket, reads, writes):
        k, v = ticket
        for b in reads:
            if b.r.get(k, 0) < v:
                b.r[k] = v
        for b in writes:
            b.w = ticket
            b.r = {}

    def op(self, e, fn, reads=(), writes=(), signal=True):
        self.deps(e, reads, writes)
        ins = fn(self.eng[e])
        self.nins += 1
        if signal:
            self.cnt[e] += 1
            ins.then_inc(self.sems[e], 1)
            t = (e, self.cnt[e])
        else:
            t = (e, self.cnt[e] + 1)
        self.mark(t, reads, writes)
        return t

    def dma(self, e, out, in_, reads=(), writes=(), **kw):
        self.deps(e, reads, writes)
        i = self.rr
        self.rr = (self.rr + 1) % self.NDMA
        k = "d%d" % i
        if self.cnt[k] > 0:
            self.wait(e, (k, self.cnt[k]))
        ins = self.eng[e].dma_start(out=out, in_=in_, **kw)
        self.cnt[k] += 16
        ins.then_inc(self.sems[k], 16)
        self.nins += 1
        t = (k, self.cnt[k])
        self.mark(t, reads, writes)
        return t

    def barrier(self):
        for e in self.eng:
            for k, v in self.cnt.items():
                if v > 0:
                    self.wait(e, (k, v))


def TP(hp):
    return {"tile_position": (96, 0)} if hp == 3 else {}


class Ring:
    def __init__(self, aps):
        self.aps = aps
        self.bufs = [Buf() for _ in aps]
        self.i = 0

    def next(self):
        j = self.i
        self.i = (self.i + 1) % len(self.aps)
        return self.aps[j], self.bufs[j]


class Prog:
    def __init__(self, dbg=False, upto=99):
        self.dbg = dbg
        self.upto = upto
        self.nc = bass.Bass("TRN2", target_bir_lowering=False)
        self.dbg_names = []

    def din(self, name, shape, dt=F32):
        return self.nc.dram_tensor(name, list(shape), dt, kind="ExternalInput").ap()

    def dscr(self, name, shape, dt=F32):
        if self.dbg:
            self.dbg_names.append(name)
            return self.nc.dram_tensor(name, list(shape), dt, kind="ExternalOutput").ap()
        return self.nc.dram_tensor(name, list(shape), dt).ap()

    def sb(self, st, name, shape, dt=F32):
        self._n = getattr(self, "_n", 0) + 1
        return st.enter_context(self.nc.sbuf_tensor("sb%d_%s" % (self._n, name), list(shape), dt)).ap()

    def ring(self, st, name, shape, dt, n):
        return Ring([self.sb(st, "%s%d" % (name, i), shape, dt) for i in range(n)])

    def build(self):
        nc = self.nc
        I = {}
        I["x"] = self.din("x", [NLAT, D])
        I["ctx"] = self.din("ctx", [NCTX, D])
        I["ccols"] = self.din("ccols", [128, 8, 2])
        I["ada_w"] = self.din("ada_w", [2, D, 6 * D])
        I["ada_b"] = self.din("ada_b", [2, 6 * D])
        I["norm_g"] = self.din("norm_g", [2, 4, D])
        I["w_out"] = self.din("w_out", [2, D, D])
        I["mlp_w1"] = self.din("mlp_w1", [2, D, 4 * D])
        I["mlp_w2"] = self.din("mlp_w2", [2, 4 * D, D])
        I["w_in0"] = self.din("w_in0", [D, W0C])
        I["w_in1"] = self.din("w_in1", [D, W1C])
        I["gla_wg"] = self.din("gla_wg", [2, 17, 256])
        I["gla_ng"] = self.din("gla_ng", [1, 64])
        I["att_g"] = self.din("att_g", [128, 4])
        I["ml_conv"] = self.din("ml_conv", [128, 4, 4])
        I["ml_bd"] = self.din("ml_bd", [3, 4, 128, 128])
        I["ml_wg"] = self.din("ml_wg", [12, 128, 32])
        I["ml_bg"] = self.din("ml_bg", [1, 32])
        I["ml_ng"] = self.din("ml_ng", [1, 64])
        I["ml_skip"] = self.din("ml_skip", [128, 4])
        I["ret_logit"] = self.din("ret_logit", [1, 16])
        I["ret_ng"] = self.din("ret_ng", [1, 64])
        I["cmat"] = self.din("cmat", [8, 128, 128])
        I["rope"] = self.din("rope", [2, 128, NT])
        I["jcol"] = self.din("jcol", [128, 2])
        self.I = I
        self.out = nc.dram_tensor("out", [NLAT, D], F32, kind="ExternalOutput").ap()

        with ExitStack() as gst:
            self.fw = FW(nc, gst)
            fw = self.fw
            self.psr = Ring([gst.enter_context(nc.psum_tensor("ps%d" % i, [128, 512], F32)).ap() for i in range(8)])
            self.cm = self.sb(gst, "cm", [128, 8, 128], F32)
            self.cmb = self.sb(gst, "cmb", [128, 8, 128], BF16)
            self.bcm = Buf()
            fw.dma("sp", self.cm, I["cmat"].rearrange("n p f -> p n f"), writes=[self.bcm])
            fw.dma("pool", self.cmb, I["cmat"].rearrange("n p f -> p n f"), writes=[self.bcm])
            self.psb = self.psr.aps
            self.S = {}
            self.S["yT0"] = self.dscr("yT0", [D, NT], BF16)
            self.S["yT1"] = self.dscr("yT1", [D, NT], BF16)
            self.res1 = self.dscr("res1", [NT, D])
            self.phase_ada()
            for layer in range(2):
                u = self.upto - 10 * layer
                if u >= 1:
                    self.phase_inproj(layer)
                if layer == 0:
                    if u >= 2:
                        self.phase_attn()
                    if u >= 3:
                        self.phase_gla()
                else:
                    if u >= 2:
                        self.phase_mlstm()
                    if u >= 3:
                        self.phase_ret()
                if u >= 4:
                    self.phase_outproj(layer)
                if u >= 5:
                    self.phase_mlp(layer)
            fw.barrier()
        return nc

    def psring(self, idx):
        return Ring([self.psb[i] for i in idx])

    def phase_attn(self):
        nc, fw, S = self.nc, self.fw, self.S
        yT = S["yT0"]
        with ExitStack() as st:
            kd = [self.sb(st, "kd%d" % g, [128, NT], BF16) for g in range(2)]
            bkd = [Buf(), Buf()]
            va = [self.sb(st, "va%d" % g, [128, NTILE, 128], BF16) for g in range(2)]
            bva = [Buf(), Buf()]
            for g in range(2):
                for hh in range(2):
                    fw.dma("sp", kd[g][hh * 64:(hh + 1) * 64, :], S["kBT"][g * 64:(g + 1) * 64, :], writes=[bkd[g]])
                fw.op("pool", lambda e: e.memset(va[g], 1.0), writes=[bva[g]])
                vsrc = S["vB"][:, g * 64:(g + 1) * 64].rearrange("(kb p) d -> p kb d", p=128)
                for q4 in range(0, NTILE, 8):
                    q5 = min(NTILE, q4 + 8)
                    fw.dma("sp", va[g][:, q4:q5, 0:64], vsrc[:, q4:q5, :], writes=[bva[g]])
            qr = self.ring(st, "qT", [128, 2, 256], BF16, 4)
            for qa, qb_ in zip(qr.aps, qr.bufs):
                fw.op("pool", lambda e: e.memset(qa, 0.0), writes=[qb_])
            pr = self.ring(st, "pT", [128, 512], BF16, 4)
            recr = self.ring(st, "rec", [64, 512], F32, 2)
            outr = self.ring(st, "ao", [64, 512], BF16, 3)
            accr = self.psring([0, 1])
            sr = self.psring([2, 3, 4, 5, 6, 7])
            work = [(t0, m) for t0 in range(0, NTILE, 2) for m in range(4)]

            def load_q(t0, m):
                qT, bq = qr.next()
                for hh in range(2):
                    fw.dma("sp", qT[hh * 64:(hh + 1) * 64, hh, :], S["qBT"][m * 128 + hh * 64:m * 128 + (hh + 1) * 64, t0 * 128:t0 * 128 + 256], writes=[bq])
                return qT, bq

            qnext = load_q(*work[0])
            for wi, (t0, m) in enumerate(work):
                if True:
                    c0 = t0 * 128
                    blocks = [0, 1] if t0 < 2 else list(range(NTILE))
                    g = m // 2
                    qT, bq = qnext
                    if wi + 1 < len(work):
                        qnext = load_q(*work[wi + 1])
                    acc, bacc = accr.next()
                    pend = []

                    def s_mm(kb):
                        s_, bs_ = sr.next()
                        fw.op("pe", lambda e: e.matmul(s_, lhsT=kd[g][:, kb * 128:(kb + 1) * 128], rhs=qT.rearrange("p h t -> p (h t)"), start=True, stop=True),
                              reads=[bkd[g], bq], writes=[bs_])
                        p_, bp_ = pr.next()
                        fw.op("act", lambda e: e.activation(p_, s_, AF.Exp, scale=0.125), reads=[bs_], writes=[bp_])
                        pend.append((kb, p_, bp_))

                    def pv_mm():
                        kb, p_, bp_ = pend.pop(0)
                        for hh in range(2):
                            fw.op("pe", lambda e: e.matmul(acc[:, hh * 256:(hh + 1) * 256], lhsT=va[g][:, kb, :], rhs=p_[:, hh * 256:(hh + 1) * 256], start=(kb == blocks[0] and hh == 0), stop=(kb == blocks[-1])),
                                  reads=[bva[g], bp_], writes=[bacc], signal=(hh == 1))

                    for kb in blocks:
                        s_mm(kb)
                        if len(pend) > 2:
                            pv_mm()
                    while pend:
                        pv_mm()
                    rec, brec = recr.next()
                    fw.op("dve", lambda e: e.reciprocal(rec, acc[64:128, :]), reads=[bacc], writes=[brec])
                    ao, bao = outr.next()
                    fw.op("dve", lambda e: e.tensor_tensor(ao, acc[0:64, :], rec, op=ALU.mult), reads=[bacc, brec], writes=[bao])
                    fw.dma("sp", yT[512 + m * 128:512 + (m + 1) * 128, c0:c0 + 256].rearrange("(hh d) t -> d hh t", hh=2), ao.rearrange("p (hh t) -> p hh t", hh=2), reads=[bao])
            fw.barrier()

    def scan_driver(self, orders, mk):
        n = len(orders[0])
        Ls = [dict(), dict()]
        As = [dict(), dict()]
        for step in range(-2, n):
            for d in range(2):
                if 0 <= step + 2 < n:
                    Ls[d][step + 2] = mk[d][0](orders[d][step + 2])
            for d in range(2):
                if 0 <= step + 1 < n:
                    As[d][step + 1] = mk[d][1](orders[d][step + 1], Ls[d].pop(step + 1))
            for d in range(2):
                if step >= 0:
                    mk[d][2](orders[d][step], As[d].pop(step))

    def phase_gla(self):
        nc, fw, S, cm, cmb = self.nc, self.fw, self.S, self.cm, self.cmb
        yT = S["yT0"]
        with ExitStack() as st:
            oacc = self.sb(st, "oacc", [128, NTILE, 512]); boacc = [Buf() for _ in range(NTILE)]
            Sf = [[self.sb(st, "Sf%d%d" % (d, i), [128, 64]) for i in range(2)] for d in range(2)]
            Sb = [[self.sb(st, "Sb%d%d" % (d, i), [128, 64], BF16) for i in range(2)] for d in range(2)]
            bS = [[Buf(), Buf()], [Buf(), Buf()]]
            hm = self.sb(st, "hm", [128, 4]); bhm = Buf()
            fw.op("dve", lambda e: e.tensor_copy(hm, cm[:, self.BLK32, 0:128:32]), reads=[self.bcm], writes=[bhm])
            RL, RW = 8, 6
            qTr = self.ring(st, "gq", [128, 2, 128], F32, RL)
            kTr = self.ring(st, "gk", [128, 2, 128], F32, RL)
            ktr = self.ring(st, "gkt", [128, 256], F32, RL)
            vr = self.ring(st, "gv", [128, 512], BF16, RL)
            lgr = self.ring(st, "glg", [128, 256], F32, RL)
            gr = self.ring(st, "gg", [128, 512], F32, 8)
            Eqr = self.ring(st, "Eq", [128, 2, 128], F32, RW)
            Ekr = self.ring(st, "Ek", [128, 2, 128], F32, 4)
            Err = self.ring(st, "Er", [128, 256], F32, 4)
            qddr = self.ring(st, "qdd", [128, 2, 128], F32, 4)
            qdr = self.ring(st, "qd", [128, 2, 4, 128], BF16, RW)
            kir = self.ring(st, "ki", [128, 2, 128], BF16, 4)
            kdr = self.ring(st, "kdd", [128, 256], BF16, RW)
            Amr = self.ring(st, "Am", [128, 8, 128], BF16, RW)
            t32r = self.ring(st, "gt32", [128, 512], F32, 3)
            ybr = self.ring(st, "gyb", [128, 512], BF16, 2)
            yTr = self.ring(st, "gyT", [128, 4, 128], BF16, 2)
            str_ = self.ring(st, "gst", [128, 24], F32, 2)
            psr = self.psr
            yv = yT[0:512, :].rearrange("(m p) t -> p m t", p=128)
            orders = [list(range(NTILE)), [1, 0] + list(range(NTILE - 1, 1, -1))]
            pos = [{c: i for i, c in enumerate(o)} for o in orders]
            last_dir = {c: (1 if pos[1][c] >= pos[0][c] else 0) for c in range(NTILE)}
            for d in range(2):
                for hc in range(2):
                    fw.op("dve", lambda e: e.memset(Sf[d][hc], 0.0), writes=[bS[d][hc]])
                    fw.op("dve", lambda e: e.memset(Sb[d][hc], 0.0), writes=[bS[d][hc]])

            def make_dir(d):
                TRI = self.UINC if d == 0 else self.LINC
                TRIS = self.LSTR if d == 0 else self.USTR
                ecol = 127 if d == 0 else 0

                def stageL(c):
                    r0 = c * 128
                    qT, bq = qTr.next(); kT, bk = kTr.next(); kt, bkt = ktr.next(); v, bv = vr.next(); lg, blg = lgr.next()
                    fw.dma("sp", qT, S["qAT"][:, r0:r0 + 128].rearrange("(hc p) t -> p hc t", p=128), writes=[bq])
                    fw.dma("sp", kT, S["kAT"][:, r0:r0 + 128].rearrange("(hc p) t -> p hc t", p=128), writes=[bk])
                    fw.dma("sp", kt, S["kA"][r0:r0 + 128, :], writes=[bkt])
                    fw.dma("sp", v, S["vA"][r0:r0 + 128, :], writes=[bv])
                    fw.dma("sp", lg, S["lgp"][r0:r0 + 128, d * 256:(d + 1) * 256], writes=[blg])
                    gt = bg = None
                    if last_dir[c] == d:
                        gt, bg = gr.next()
                        fw.dma("sp", gt, S["gA"][r0:r0 + 128, :], writes=[bg])
                    return locals()

                def stageA(c, L):
                    qT, bq, kT, bk, kt, bkt, v, bv, lg, blg = (L[k] for k in ("qT", "bq", "kT", "bk", "kt", "bkt", "v", "bv", "lg", "blg"))
                    gt, bg, r0 = L["gt"], L["bg"], L["r0"]
                    pc, bpc = psr.next()
                    for hc in range(2):
                        fw.op("pe", lambda e: e.matmul(pc[:, hc * 128:(hc + 1) * 128], lhsT=lg[:, hc * 128:(hc + 1) * 128], rhs=cm[:, TRI, :], start=True, stop=True),
                              reads=[blg, self.bcm], writes=[bpc], signal=False)
                    fw.op("pe", lambda e: e.matmul(pc[:, 256:512], lhsT=cm[:, TRIS, :], rhs=lg, start=True, stop=True), reads=[blg, self.bcm], writes=[bpc])
                    Eq, bEq = Eqr.next(); Ek, bEk = Ekr.next(); Er, bEr = Err.next()
                    fw.op("act", lambda e: e.activation(Eq.rearrange("p a b -> p (a b)"), pc[:, 0:256], AF.Exp, scale=-1.0 / 16), reads=[bpc], writes=[bEq])
                    fw.op("act", lambda e: e.activation(Ek.rearrange("p a b -> p (a b)"), pc[:, 0:256], AF.Exp, scale=1.0 / 16), reads=[bpc], writes=[bEk])
                    fw.op("act", lambda e: e.activation(Er, pc[:, 256:512], AF.Exp, scale=-1.0 / 16), reads=[bpc], writes=[bEr])
                    qdd, bqdd = qddr.next(); qd, bqd = qdr.next(); ki, bki = kir.next(); kdd, bkdd = kdr.next()
                    fw.op("dve", lambda e: e.tensor_tensor(qdd, qT, Eq, op=ALU.mult), reads=[bq, bEq], writes=[bqdd])
                    import os
                    if os.environ.get("GLA_Q4D", "1") == "1":
                        for hc in range(2):
                            fw.op("dve", lambda e: e.tensor_tensor(qd[:, hc, :, :], qdd[:, hc, :].unsqueeze(1).to_broadcast([128, 4, 128]),
                                                                    hm.unsqueeze(2).to_broadcast([128, 4, 128]), op=ALU.mult), reads=[bqdd, bhm], writes=[bqd])
                    else:
                        for hc in range(2):
                            for hp in range(4):
                                fw.op("dve", lambda e: e.tensor_scalar(qd[:, hc, hp, :], qdd[:, hc, :], hm[:, hp:hp + 1], None, op0=ALU.mult), reads=[bqdd, bhm], writes=[bqd])
                    fw.op("pool", lambda e: e.tensor_tensor(ki, kT, Ek, op=ALU.mult), reads=[bk, bEk], writes=[bki])
                    fw.op("pool", lambda e: e.tensor_tensor(kdd, kt, Er, op=ALU.mult), reads=[bkt, bEr], writes=[bkdd])
                    Am, bAm = Amr.next()
                    for hc in range(2):
                        pa, bpa = psr.next()
                        fw.op("pe", lambda e: e.matmul(pa, lhsT=ki[:, hc, :], rhs=qd[:, hc, :, :].rearrange("p h t -> p (h t)"), start=True, stop=True),
                              reads=[bki, bqd], writes=[bpa])
                        fw.op("dve", lambda e: e.tensor_tensor(Am[:, hc * 4:(hc + 1) * 4, :], pa.rearrange("p (h t) -> p h t", h=4),
                                                                cm[:, TRI, :].unsqueeze(1).to_broadcast([128, 4, 128]), op=ALU.mult), reads=[bpa, self.bcm], writes=[bAm])
                    return locals()

                def stageB(c, L):
                    r0, qd, bqd, v, bv, Am, bAm, kdd, bkdd, Eq, bEq = (L[k] for k in ("r0", "qd", "bqd", "v", "bv", "Am", "bAm", "kdd", "bkdd", "Eq", "bEq"))
                    gt, bg = L["gt"], L["bg"]
                    po, bpo = psr.next()
                    for h in range(8):
                        hc, hp = h // 4, h % 4
                        fw.op("pe", lambda e: e.matmul(po[:, h * 64:(h + 1) * 64], lhsT=Am[:, h, :], rhs=v[:, h * 64:(h + 1) * 64], start=True, stop=False),
                              reads=[bAm, bv], writes=[bpo], signal=False)
                        fw.op("pe", lambda e: e.matmul(po[:, h * 64:(h + 1) * 64], lhsT=qd[:, hc, hp, :], rhs=Sb[d][hc], start=False, stop=True),
                              reads=[bqd, bS[d][hc]], writes=[bpo], signal=(h == 7))
                    if last_dir[c] != d:
                        fw.op("act", lambda e: e.copy(oacc[:, c, :], po), reads=[bpo], writes=[boacc[c]])
                    else:
                        fw.op("dve", lambda e: e.tensor_tensor(oacc[:, c, :], oacc[:, c, :], po, op=ALU.add), reads=[bpo, boacc[c]], writes=[boacc[c]])
                    for hc in range(2):
                        pu, bpu = psr.next()
                        fw.op("pe", lambda e: e.matmul(pu, lhsT=kdd[:, hc * 128:(hc + 1) * 128], rhs=v, start=True, stop=True), reads=[bkdd, bv], writes=[bpu])
                        for hp in range(4):
                            h = hc * 4 + hp
                            sl = slice(hp * 32, (hp + 1) * 32)
                            fw.op("dve", lambda e: e.scalar_tensor_tensor(Sf[d][hc][sl, :], Sf[d][hc][sl, :], Eq[sl, hc, ecol:ecol + 1], pu[sl, h * 64:(h + 1) * 64], op0=ALU.mult, op1=ALU.add),
                                  reads=[bpu, bEq, bS[d][hc]], writes=[bS[d][hc]])
                        fw.op("act", lambda e: e.copy(Sb[d][hc], Sf[d][hc]), reads=[bS[d][hc]], writes=[bS[d][hc]])
                    if last_dir[c] == d:
                        self.finalize_tok(oacc[:, c, :], boacc[c], gt, bg, t32r, str_, ybr, yTr, yv[:, :, r0:r0 + 128])

                return stageL, stageA, stageB

            self.scan_driver(orders, [make_dir(0), make_dir(1)])
            fw.barrier()

    def finalize_tok(self, o, bo, gt, bg, t32r, str_, ybr, yTr, dst, post=None):
        fw, cmb = self.fw, self.cmb
        t1, b1 = t32r.next()
        fw.op("pool", lambda e: e.tensor_tensor(t1, o, o, op=ALU.mult), reads=[bo], writes=[b1])
        stt, bst = str_.next()
        fw.op("dve", lambda e: e.tensor_reduce(stt[:, 0:8], t1.rearrange("p (h d) -> p h d", d=64), axis=AX.X, op=ALU.add), reads=[b1], writes=[bst])
        self.rstd_col(stt[:, 0:8], bst, stt[:, 16:24], stt[:, 8:16], 8, 1.0 / 64)
        t2, b2 = t32r.next()
        fw.op("dve", lambda e: e.tensor_tensor(t2.rearrange("p (h d) -> p h d", d=64), o.rearrange("p (h d) -> p h d", d=64),
                                                stt[:, 16:24].unsqueeze(2).to_broadcast([128, 8, 64]), op=ALU.mult), reads=[bo, bst], writes=[b2])
        yb, byb = ybr.next()
        if len(gt.shape) == 3:
            fw.op("pool", lambda e: e.tensor_tensor(yb.rearrange("p (h d) -> p h d", d=64), t2.rearrange("p (h d) -> p h d", d=64), gt, op=ALU.mult), reads=[b2, bg], writes=[byb])
        else:
            fw.op("pool", lambda e: e.tensor_tensor(yb, t2, gt, op=ALU.mult), reads=[b2, bg], writes=[byb])
        ps, bp = self.psr.next()
        psb = ps.bitcast(BF16)
        for m in range(4):
            fw.op("pe", lambda e: e.transpose(psb[:, m * 128:(m + 1) * 128], yb[:, m * 128:(m + 1) * 128], cmb[:, self.IDENT, :]),
                  reads=[byb, self.bcm], writes=[bp], signal=(m == 3))
        if post is not None:
            post(psb, bp, dst)
            return
        yTt, byT = yTr.next()
        fw.op("act", lambda e: e.copy(yTt, psb[:, 0:512].rearrange("p (m t) -> p m t", m=4)), reads=[bp], writes=[byT])
        fw.dma("sp", dst, yTt, reads=[byT])

    def phase_outproj(self, layer):
        nc, fw, I, S, cmb = self.nc, self.fw, self.I, self.S, self.cmb
        yT = S["yT%d" % layer]
        with ExitStack() as st:
            Wo = self.sb(st, "Wo", [128, 8, D], BF16); bWo = Buf()
            fw.dma("pool", Wo, I["w_out"][layer].rearrange("(kc p) n -> p kc n", p=128), writes=[bWo])
            yr = self.ring(st, "oy", [128, 8, 128], BF16, 3)
            xr = self.ring(st, "ox", [128, D], F32, 3)
            tr = self.ring(st, "ot", [128, D], F32, 2)
            outr = self.ring(st, "oo", [128, D], F32, 2)
            sqj = self.sb(st, "osq", [128, 512]); bsqj = Buf()
            str_ = self.ring(st, "ost", [128, 8], F32, 4)
            cur_stream = None
            tiles = list(range(NTILE)) if layer == 0 else list(range(2, NTILE))
            for t in tiles:
                stream = 1 if t < 2 else 0
                if stream != cur_stream:
                    bc, bbc = self.load_bc(st, "obc%d" % stream, layer, stream, [2])
                    cur_stream = stream
                r0 = t * 128
                yt, by = yr.next()
                fw.dma("sp", yt, yT[:, r0:r0 + 128].rearrange("(kc p) t -> p kc t", p=128), writes=[by])
                xt, bx = xr.next()
                if layer == 0:
                    srcap = I["ctx"][r0:r0 + 128, :] if t < 2 else I["x"][r0 - 256:r0 - 128, :]
                else:
                    srcap = self.res1[r0:r0 + 128, :]
                fw.dma("sp", xt, srcap, writes=[bx])
                pss = []
                stt, bst = str_.next()
                for n in range(2):
                    ps, bp = self.psr.next()
                    for kc in range(8):
                        fw.op("pe", lambda e: e.matmul(ps, lhsT=yt[:, kc, :], rhs=Wo[:, kc, n * 512:(n + 1) * 512], start=(kc == 0), stop=(kc == 7)),
                              reads=[by, bWo], writes=[bp], signal=(kc == 7))
                    fw.op("act", lambda e: e.activation(sqj, ps, AF.Square, accum_out=stt[:, n:n + 1]), reads=[bp], writes=[bsqj, bst])
                    pss.append((ps, bp))
                fw.op("dve", lambda e: e.tensor_tensor(stt[:, 2:3], stt[:, 0:1], stt[:, 1:2], op=ALU.add), reads=[bst], writes=[bst])
                self.rstd_col(stt[:, 2:3], bst, stt[:, 4:5], stt[:, 3:4], 1, 1.0 / D)
                tt, btt = tr.next()
                for n in range(2):
                    ps, bp = pss[n]
                    fw.op("dve", lambda e: e.scalar_tensor_tensor(tt[:, n * 512:(n + 1) * 512], ps, stt[:, 4:5], bc[:, 0, n * 512:(n + 1) * 512], op0=ALU.mult, op1=ALU.mult),
                          reads=[bp, bst, bbc], writes=[btt])
                ot, bo = outr.next()
                fw.op("pool", lambda e: e.tensor_tensor(ot, tt, xt, op=ALU.add), reads=[btt, bx], writes=[bo])
                fw.dma("sp", self.res1[r0:r0 + 128, :], ot, reads=[bo])
            fw.barrier()

    def phase_mlp(self, layer):
        nc, fw, I, S, cmb = self.nc, self.fw, self.I, self.S, self.cmb
        with ExitStack() as st:
            W1 = self.sb(st, "W1", [128, 8, 4 * D], BF16); bW1s = [Buf() for _ in range(8)]
            W2 = self.sb(st, "W2", [128, 32, D], BF16); bW2s = [Buf() for _ in range(8)]
            w1v = I["mlp_w1"][layer].rearrange("(kc p) n -> p kc n", p=128)
            w2v = I["mlp_w2"][layer].rearrange("(kc p) n -> p kc n", p=128)
            for kc in range(8):
                fw.dma("pool", W1[:, kc, :], w1v[:, kc, :], writes=[bW1s[kc]])
            for k4 in range(0, 32, 4):
                fw.dma("pool", W2[:, k4:k4 + 4, :], w2v[:, k4:k4 + 4, :], writes=[bW2s[k4 // 4]])
            xr = self.ring(st, "mx", [128, D], F32, 4)
            tr = self.ring(st, "mt", [128, D], F32, 2)
            ulr = self.ring(st, "mul", [128, D], BF16, 2)
            uTr = self.ring(st, "muT", [128, 8, 256], BF16, 2)
            rlr = self.ring(st, "mrl", [128, 256], F32, 4)
            hr = self.ring(st, "mh", [128, 256], BF16, 8)
            vr = self.ring(st, "mv", [128, D], F32, 2)
            sqj = self.sb(st, "msq", [128, D], BF16); bsqj = Buf()
            str_ = self.ring(st, "mst", [128, 8], F32, 8)
            accr = self.psring([0, 1, 2, 3])
            wr = self.psring([4, 5, 6, 7])
            t0s = list(range(0, NTILE, 2)) if layer == 0 else list(range(2, NTILE, 2))
            stream_of = lambda t0: 1 if t0 < 2 else 0
            cur = {"stream": None, "bc": None}

            def pro_elem(t0):
                if stream_of(t0) != cur["stream"]:
                    cur["bc"] = self.load_bc(st, "mbc%d" % stream_of(t0), layer, stream_of(t0), [3, 4, 5])
                    cur["stream"] = stream_of(t0)
                bc, bbc = cur["bc"]
                xs = []
                uls = []
                uT, buT = uTr.next()
                for ti in range(2):
                    r0 = (t0 + ti) * 128
                    xt, bx = xr.next()
                    fw.dma("sp", xt, self.res1[r0:r0 + 128, :], writes=[bx])
                    xs.append((xt, bx))
                    stt, bst = str_.next()
                    fw.op("act", lambda e: e.activation(sqj, xt, AF.Square, accum_out=stt[:, 0:1]), reads=[bx], writes=[bsqj, bst])
                    self.rstd_col(stt[:, 0:1], bst, stt[:, 2:3], stt[:, 1:2], 1, 1.0 / D)
                    tt, btt = tr.next()
                    fw.op("dve", lambda e: e.scalar_tensor_tensor(tt, xt, stt[:, 2:3], bc[:, 0, :], op0=ALU.mult, op1=ALU.mult), reads=[bx, bst, bbc], writes=[btt])
                    ul, bul = ulr.next()
                    fw.op("pool", lambda e: e.tensor_tensor(ul, tt, bc[:, 1, :], op=ALU.add), reads=[btt, bbc], writes=[bul])
                    uls.append((ul, bul))
                return {"xs": xs, "uT": uT, "buT": buT, "uls": uls, "done": 0}

            def pro_pe(stn, upto_ti):
                uT, buT = stn["uT"], stn["buT"]
                while stn["done"] < min(upto_ti, 2):
                    ti = stn["done"]
                    ul, bul = stn["uls"][ti]
                    ps, bp = wr.next()
                    psb = ps.bitcast(BF16)
                    for kc in range(8):
                        fw.op("pe", lambda e: e.transpose(psb[:, kc * 128:(kc + 1) * 128], ul[:, kc * 128:(kc + 1) * 128], cmb[:, self.IDENT, :]),
                              reads=[bul, self.bcm], writes=[bp], signal=(kc == 7))
                    fw.op("act", lambda e: e.copy(uT[:, :, ti * 128:(ti + 1) * 128], psb.rearrange("p (k t) -> p k t", k=8)), reads=[bp], writes=[buT])
                    stn["done"] += 1

            def prologue(t0):
                stn = pro_elem(t0)
                pro_pe(stn, 2)
                return stn

            state = prologue(t0s[0])
            for idx, t0 in enumerate(t0s):
                xs, uT, buT = state["xs"], state["uT"], state["buT"]
                bc, bbc = cur["bc"]
                nxt = t0s[idx + 1] if idx + 1 < len(t0s) else None
                hoist = nxt is not None and stream_of(nxt) == cur["stream"]
                state = None
                accs = [accr.next() for _ in range(4)]
                pend = []

                def w1_group(j):
                    ps, bp = wr.next()
                    for kc in range(8):
                        fw.op("pe", lambda e: e.matmul(ps[:, 0:256], lhsT=W1[:, kc, j * 128:(j + 1) * 128], rhs=uT[:, kc, :], start=(kc == 0), stop=(kc == 7)),
                              reads=[bW1s[kc], buT], writes=[bp], signal=(kc == 7))
                    rl, brl = rlr.next()
                    fw.op("act", lambda e: e.activation(rl, ps[:, 0:256], AF.Relu), reads=[bp], writes=[brl])
                    hT, bh = hr.next()
                    fw.op("pool", lambda e: e.tensor_tensor(hT, rl, rl, op=ALU.mult), reads=[brl], writes=[bh])
                    pend.append((j, hT, bh))

                def w2_group():
                    j, hT, bh = pend.pop(0)
                    for ti in range(2):
                        for n in range(2):
                            acc, bacc = accs[ti * 2 + n]
                            fw.op("pe", lambda e: e.matmul(acc, lhsT=hT[:, ti * 128:(ti + 1) * 128], rhs=W2[:, j, n * 512:(n + 1) * 512], start=(j == 0), stop=(j == 31)),
                                  reads=[bh, bW2s[j // 4]], writes=[bacc], signal=(j == 31))

                for j in range(32):
                    w1_group(j)
                    if j == 2 and hoist:
                        state = pro_elem(nxt)
                    if j == 14 and hoist:
                        pro_pe(state, 1)
                    if j == 22 and hoist:
                        pro_pe(state, 2)
                    if len(pend) > 5:
                        w2_group()
                while pend:
                    w2_group()
                vs = []
                for ti in range(2):
                    v, bv = vr.next()
                    for n in range(2):
                        acc, bacc = accs[ti * 2 + n]
                        if n == 0:
                            fw.op("act", lambda e: e.copy(v[:, 0:512], acc), reads=[bacc], writes=[bv])
                        else:
                            fw.op("dve", lambda e: e.tensor_copy(v[:, 512:1024], acc), reads=[bacc], writes=[bv])
                    vs.append((v, bv))
                for ti in range(2):
                    r0 = (t0 + ti) * 128
                    xt, bx = xs[ti]
                    v, bv = vs[ti]
                    stt, bst = str_.next()
                    fw.op("act", lambda e: e.activation(sqj, v, AF.Square, accum_out=stt[:, 0:1]), reads=[bv], writes=[bsqj, bst])
                    self.rstd_col(stt[:, 0:1], bst, stt[:, 2:3], stt[:, 1:2], 1, 1.0 / D)
                    fw.op("dve", lambda e: e.scalar_tensor_tensor(v, v, stt[:, 2:3], bc[:, 2, :], op0=ALU.mult, op1=ALU.mult), reads=[bv, bst, bbc], writes=[bv])
                    fw.op("pool", lambda e: e.tensor_tensor(v, v, xt, op=ALU.add), reads=[bv, bx], writes=[bv])
                    dst = self.res1[r0:r0 + 128, :] if layer == 0 else self.out[r0 - 256:r0 - 128, :]
                    fw.dma("sp", dst, v, reads=[bv])
                if state is None and nxt is not None:
                    state = prologue(nxt)
            fw.barrier()

    def phase_mlstm(self):
        nc, fw, I, S, cm, cmb = self.nc, self.fw, self.I, self.S, self.cm, self.cmb
        S["xcT"] = self.dscr("xcT", [512, NT], BF16)
        S["qmT"] = self.dscr("qmT", [512, NT], BF16)
        S["kmT"] = self.dscr("kmT", [512, NT], BF16)
        S["vm"] = self.dscr("vm", [NT, 512], BF16)
        S["gl"] = self.dscr("gl", [NT, 32])
        with ExitStack() as st:
            xm = self.sb(st, "xm", [128, 4, NT], BF16); bxm = Buf()
            xc = self.sb(st, "xc", [128, 4, NT], BF16); bxc = [Buf() for _ in range(4)]
            fw.dma("sp", xm, S["xmT"].rearrange("(m p) t -> p m t", p=128), writes=[bxm])
            cw = self.sb(st, "cw", [128, 4, 4]); bcw = Buf()
            fw.dma("sp", cw, I["ml_conv"], writes=[bcw])
            BD = self.sb(st, "BD", [128, 3, 4, 128], BF16); bBD = Buf()
            fw.dma("pool", BD, I["ml_bd"].rearrange("t m k n -> k t m n"), writes=[bBD])
            wg = self.sb(st, "mwg", [128, 12, 32], BF16); bwg = Buf()
            fw.dma("pool", wg, I["ml_wg"].rearrange("c k n -> k c n"), writes=[bwg])
            bgb = self.sb(st, "bgb", [128, 32]); bbg = Buf()
            fw.dma("sp", bgb, I["ml_bg"].partition_broadcast(128), writes=[bbg])
            accr = self.ring(st, "cacc", [128, NT], F32, 2)
            segs = [(0, NCTX), (NCTX, NT)]
            for m in range(4):
                acc, bacc = accr.next()
                e1 = e2 = "dve"
                fw.op("dve", lambda e: e.tensor_scalar(acc, xm[:, m, :], cw[:, m, 1:2], cw[:, m, 3:4], op0=ALU.mult, op1=ALU.add), reads=[bxm, bcw], writes=[bacc])
                for (a, b) in segs:
                    fw.op(e1, lambda e: e.scalar_tensor_tensor(acc[:, a + 1:b], xm[:, m, a:b - 1], cw[:, m, 0:1], acc[:, a + 1:b], op0=ALU.mult, op1=ALU.add),
                          reads=[bxm, bcw, bacc], writes=[bacc])
                    fw.op(e2, lambda e: e.scalar_tensor_tensor(acc[:, a:b - 1], xm[:, m, a + 1:b], cw[:, m, 2:3], acc[:, a:b - 1], op0=ALU.mult, op1=ALU.add),
                          reads=[bxm, bcw, bacc], writes=[bacc])
                fw.op("act", lambda e: e.activation(xc[:, m, :], acc, AF.Silu), reads=[bacc], writes=[bxc[m]])
                fw.dma("sp", S["xcT"][m * 128:(m + 1) * 128, :], xc[:, m, :], reads=[bxc[m]])
            ginr = self.ring(st, "gin", [128, 12, 128], BF16, 2)
            vtr = self.ring(st, "vtk", [128, 512], BF16, 2)
            zall = self.sb(st, "zall", [128, NTILE, 32]); bz = Buf()
            for t in range(NTILE):
                r0 = t * 128
                gin, bgin = ginr.next()
                for ty in range(3):
                    src, bsrc = (xc, bxc) if ty < 2 else (xm, [bxm] * 4)
                    ps, bp = self.psr.next()
                    for m in range(4):
                        fw.op("pe", lambda e: e.matmul(ps[:, m * 128:(m + 1) * 128], lhsT=BD[:, ty, m, :], rhs=src[:, m, r0:r0 + 128], start=True, stop=True),
                              reads=[bBD, bsrc[m]], writes=[bp], signal=(m == 3))
                    fw.op("act" if ty != 1 else "dve", lambda e: (e.copy if ty != 1 else e.tensor_copy)(gin[:, ty * 4:(ty + 1) * 4, :], ps.rearrange("p (m t) -> p m t", m=4)),
                          reads=[bp], writes=[bgin])
                    if ty < 2:
                        dstT = S["qmT"] if ty == 0 else S["kmT"]
                        fw.dma("sp", dstT[:, r0:r0 + 128].rearrange("(m p) t -> p m t", p=128), gin[:, ty * 4:(ty + 1) * 4, :], reads=[bgin])
                ps, bp = self.psr.next()
                for m in range(4):
                    fw.op("pe", lambda e: e.matmul(ps[:, m * 128:(m + 1) * 128], lhsT=xm[:, m, r0:r0 + 128], rhs=BD[:, 2, m, :], start=True, stop=True),
                          reads=[bBD, bxm], writes=[bp], signal=(m == 3))
                vt, bvt = vtr.next()
                fw.op("dve", lambda e: e.tensor_copy(vt, ps), reads=[bp], writes=[bvt])
                fw.dma("sp", S["vm"][r0:r0 + 128, :], vt, reads=[bvt])
                ps, bp = self.psr.next()
                for ch in range(12):
                    fw.op("pe", lambda e: e.matmul(ps[:, 0:32], lhsT=gin[:, ch, :], rhs=wg[:, ch, :], start=(ch == 0), stop=(ch == 11)),
                          reads=[bgin, bwg], writes=[bp], signal=(ch == 11))
                fw.op("dve", lambda e: e.tensor_tensor(zall[:, t, :], ps[:, 0:32], bgb, op=ALU.add), reads=[bp, bbg], writes=[bz])
            glt = self.sb(st, "glt", [128, NTILE, 32]); bgl = Buf()
            ef = self.sb(st, "ef", [128, NTILE, 16]); bef = Buf()
            zv = zall.rearrange("p t (d k) -> p t d k", d=2)
            fw.op("act", lambda e: e.activation(ef.rearrange("p t (d h) -> p t d h", d=2), zv[:, :, :, 8:16], AF.Exp, scale=-1.0), reads=[bz], writes=[bef])
            fw.op("act", lambda e: e.activation(ef, ef, AF.Ln, bias=1.0), reads=[bef], writes=[bef])
            fw.op("dve", lambda e: e.tensor_scalar(glt[:, :, 0:16], ef, -1.0, None, op0=ALU.mult), reads=[bef], writes=[bgl])
            fw.op("pool", lambda e: e.tensor_copy(glt[:, :, 16:32].rearrange("p t (d h) -> p t d h", d=2), zv[:, :, :, 0:8]), reads=[bz], writes=[bgl])
            fw.dma("sp", S["gl"].rearrange("(c p) g -> p c g", p=128), glt, reads=[bgl])
            fw.barrier()
        self.phase_scan("ml")

    def phase_ret(self):
        self.phase_scan("ret")

    def phase_scan(self, kind):
        nc, fw, I, S, cm, cmb = self.nc, self.fw, self.I, self.S, self.cm, self.cmb
        ml = kind == "ml"
        Wd = 65 if ml else 64
        qsrc, ksrc, vsrc = (S["qmT"], S["kmT"], S["vm"]) if ml else (S["qDT"], S["kDT"], S["vD"])
        yT = S["yT1"]
        yv = (yT[0:512, :] if ml else yT[512:1024, :]).rearrange("(m p) t -> p m t", p=128)
        with ExitStack() as st:
            hacc = self.sb(st, "hacc", [128, NTILE, 512]); bh = [Buf() for _ in range(NTILE)]
            Cn = [self.sb(st, "Cn%d" % d, [128, 4, Wd]) for d in range(2)]; Cb = [self.sb(st, "Cb%d" % d, [128, 4, Wd], BF16) for d in range(2)]; bC = [Buf(), Buf()]
            ngb = self.sb(st, "ngb", [128, 64]); bng = Buf()
            qr = self.ring(st, "sq", [128, 4, 128], BF16, 8)
            kr = self.ring(st, "sk", [128, 4, 128], BF16, 8)
            vr = self.ring(st, "sv", [128, 512], BF16, 8)
            ktr = self.ring(st, "skt", [128, 512], BF16, 6)
            qbr = self.ring(st, "sqb", [128, 4, 2, 128], BF16, 6)
            for qa, qb_ in zip(qbr.aps, qbr.bufs):
                fw.op("pool", lambda e: e.memset(qa, 0.0), writes=[qb_])
            var = self.ring(st, "sva", [128, 8, Wd], BF16, 6)
            Amr = self.ring(st, "sAm", [128, 8, 128], BF16, 6)
            t32r = self.ring(st, "st32", [128, 512], F32, 3)
            ybr = self.ring(st, "syb", [128, 512], BF16, 2)
            yTr = self.ring(st, "syT", [128, 4, 128], BF16, 2)
            str_ = self.ring(st, "sst", [128, 24], F32, 2)
            gtr = self.ring(st, "sgt", [128, 40], F32, 8)
            if ml:
                fw.dma("sp", ngb, I["ml_ng"].partition_broadcast(128), writes=[bng])
                gl = self.sb(st, "gl", [128, NTILE, 32]); bgl = Buf()
                fw.dma("sp", gl, S["gl"].rearrange("(c p) g -> p c g", p=128), writes=[bgl])
                skip = self.sb(st, "skip", [128, 4]); bsk = Buf()
                fw.dma("sp", skip, I["ml_skip"], writes=[bsk])
                xcr = self.ring(st, "sxc", [128, 4, 128], BF16, 8)
                szr = self.ring(st, "ssz", [128, 4, 128], F32, 8)
                y32r = self.ring(st, "sy32", [128, 4, 128], F32, 2)
            else:
                gr = self.ring(st, "sg", [128, 512], F32, 8)
                rt = self.sb(st, "rt", [128, 16]); brt = Buf()
                jc = self.sb(st, "jc", [128, 2]); bjc = Buf()
                tabs = self.sb(st, "tabs", [128, 2, 24]); btab = Buf()
                fw.dma("sp", rt, I["ret_logit"].partition_broadcast(128), writes=[brt])
                fw.dma("sp", jc, I["jcol"], writes=[bjc])
                fw.op("act", lambda e: e.activation(rt, rt, AF.Exp, scale=-1.0), reads=[brt], writes=[brt])
                fw.op("act", lambda e: e.activation(rt, rt, AF.Ln, bias=1.0), reads=[brt], writes=[brt])
                Ft = self.sb(st, "Ft", [128, 16]); bF = Buf()
                for d in range(2):
                    fw.op("dve", lambda e: e.tensor_scalar(Ft[:, d * 8:(d + 1) * 8], rt[:, d * 8:(d + 1) * 8], jc[:, d:d + 1], None, op0=ALU.mult), reads=[brt, bjc], writes=[bF])
                    fw.op("act", lambda e: e.activation(tabs[:, d, 0:8], Ft[:, d * 8:(d + 1) * 8], AF.Exp, scale=1.0), reads=[bF], writes=[btab])
                    fw.op("act", lambda e: e.activation(tabs[:, d, 8:16], Ft[:, d * 8:(d + 1) * 8], AF.Exp, scale=-1.0), reads=[bF], writes=[btab])
                    fw.op("act", lambda e: e.activation(tabs[:, d, 16:24], rt[:, d * 8:(d + 1) * 8], AF.Exp, scale=-128.0), reads=[brt], writes=[btab])
            psr = self.psr
            orders = [list(range(NTILE)), [1, 0] + list(range(NTILE - 1, 1, -1))]
            pos = [{c: i for i, c in enumerate(o)} for o in orders]
            last_dir = {c: (1 if pos[1][c] >= pos[0][c] else 0) for c in range(NTILE)}
            for d in range(2):
                fw.op("dve", lambda e: e.memset(Cn[d], 0.0), writes=[bC[d]])
                fw.op("dve", lambda e: e.memset(Cb[d], 0.0), writes=[bC[d]])

            def make_dir(d):
                TRI = self.UINC if d == 0 else self.LINC

                def stageL(c):
                    r0 = c * 128
                    qT, bq = qr.next(); kT, bk = kr.next(); vt, bv = vr.next()
                    fw.dma("sp", qT, qsrc[:, r0:r0 + 128].rearrange("(m p) t -> p m t", p=128), writes=[bq])
                    fw.dma("sp", kT, ksrc[:, r0:r0 + 128].rearrange("(m p) t -> p m t", p=128), writes=[bk])
                    fw.dma("sp", vt, vsrc[r0:r0 + 128, :], writes=[bv])
                    xcc = bxcc = szc = bszc = gt = bg = None
                    if last_dir[c] == d:
                        if ml:
                            xcc, bxcc = xcr.next(); szc, bszc = szr.next()
                            fw.dma("sp", xcc, S["xcT"][:, r0:r0 + 128].rearrange("(m p) t -> p m t", p=128), writes=[bxcc])
                            fw.dma("sp", szc, S["szT"][:, r0:r0 + 128].rearrange("(m p) t -> p m t", p=128), writes=[bszc])
                        else:
                            gt, bg = gr.next()
                            fw.dma("sp", gt, S["gD"][r0:r0 + 128, :], writes=[bg])
                    return locals()

                def stageA(c, L):
                    r0, qT, bq, kT, bk, vt, bv = (L[k] for k in ("r0", "qT", "bq", "kT", "bk", "vt", "bv"))
                    xcc, bxcc, szc, bszc, gt, bg = (L[k] for k in ("xcc", "bxcc", "szc", "bszc", "gt", "bg"))
                    if ml:
                        gtt, bgt = gtr.next()
                        pg, bpg = psr.next()
                        lfv = gl[:, c, d * 8:(d + 1) * 8]
                        fw.op("pe", lambda e: e.matmul(pg[:, 0:8], lhsT=cm[:, TRI, :], rhs=lfv, start=True, stop=True), reads=[bgl, self.bcm], writes=[bpg], signal=False)
                        fw.op("pe", lambda e: e.matmul(pg[:, 8:16], lhsT=cm[:, self.ONES, :], rhs=lfv, start=True, stop=True), reads=[bgl, self.bcm], writes=[bpg])
                        fw.op("dve", lambda e: e.tensor_tensor(gtt[:, 24:32], gl[:, c, 16 + d * 8:16 + (d + 1) * 8], pg[:, 0:8], op=ALU.subtract), reads=[bgl, bpg], writes=[bgt])
                        fw.op("act", lambda e: e.activation(gtt[:, 0:8], gtt[:, 24:32], AF.Exp), reads=[bgt], writes=[bgt])
                        fw.op("act", lambda e: e.activation(gtt[:, 8:16], pg[:, 0:8], AF.Exp, bias=-math.log(8.0)), reads=[bpg], writes=[bgt])
                        fw.op("act", lambda e: e.activation(gtt[:, 16:24], pg[:, 8:16], AF.Exp), reads=[bpg], writes=[bgt])
                        av, cv, dv = gtt[:, 0:8], gtt[:, 8:16], gtt[:, 16:24]
                    else:
                        gtt, bgt = tabs, btab
                        av, cv, dv = tabs[:, d, 0:8], tabs[:, d, 8:16], tabs[:, d, 16:24]
                    ps, bp = psr.next()
                    psb = ps.bitcast(BF16)
                    for m in range(4):
                        fw.op("pe", lambda e: e.transpose(psb[:, m * 128:(m + 1) * 128], kT[:, m, :], cmb[:, self.IDENT, :]), reads=[bk, self.bcm], writes=[bp], signal=(m == 3))
                    kt, bkt = ktr.next()
                    fw.op("act", lambda e: e.copy(kt, psb[:, 0:512]), reads=[bp], writes=[bkt])
                    qb, bqb = qbr.next()
                    for hh in range(2):
                        sl = slice(hh * 64, (hh + 1) * 64)
                        fw.op("act", lambda e: e.copy(qb[sl, :, hh, :], qT[sl, :, :]), reads=[bq], writes=[bqb])
                    va, bva = var.next()
                    fw.op("dve", lambda e: e.tensor_tensor(va[:, :, 0:64], vt.rearrange("p (h d) -> p h d", d=64), av.unsqueeze(2).to_broadcast([128, 8, 64]), op=ALU.mult),
                          reads=[bv, bgt], writes=[bva])
                    if ml:
                        fw.op("pool", lambda e: e.tensor_copy(va[:, :, 64:65], av.unsqueeze(2)), reads=[bgt], writes=[bva])
                    Am, bAm = Amr.next()
                    for b2 in range(2):
                        pa, bpa = psr.next()
                        for mm in range(2):
                            m = b2 * 2 + mm
                            fw.op("pe", lambda e: e.matmul(pa[:, mm * 256:(mm + 1) * 256], lhsT=kT[:, m, :], rhs=qb[:, m, :, :].rearrange("p h t -> p (h t)"), start=True, stop=True),
                                  reads=[bk, bqb], writes=[bpa], signal=(mm == 1))
                        fw.op("dve", lambda e: e.tensor_tensor(Am[:, b2 * 4:(b2 + 1) * 4, :], pa.rearrange("p (h t) -> p h t", h=4),
                                                                cm[:, TRI, :].unsqueeze(1).to_broadcast([128, 4, 128]), op=ALU.mult), reads=[bpa, self.bcm], writes=[bAm])
                    return locals()

                def stageB(c, L):
                    r0, qb, bqb, va, bva, Am, bAm, kt, bkt, gtt, bgt, av, cv, dv = (L[k] for k in ("r0", "qb", "bqb", "va", "bva", "Am", "bAm", "kt", "bkt", "gtt", "bgt", "av", "cv", "dv"))
                    xcc, bxcc, szc, bszc, gt, bg = (L[k] for k in ("xcc", "bxcc", "szc", "bszc", "gt", "bg"))
                    first = last_dir[c] != d
                    pos_ = []
                    for b2 in range(2):
                        po, bpo = psr.next()
                        for hq in range(4):
                            h = b2 * 4 + hq
                            m, hh = h // 2, h % 2
                            fw.op("pe", lambda e: e.matmul(po[:, hq * Wd:(hq + 1) * Wd], lhsT=Am[:, h, :], rhs=va[:, h, :], start=True, stop=False),
                                  reads=[bAm, bva], writes=[bpo], signal=False)
                            fw.op("pe", lambda e: e.matmul(po[:, hq * Wd:(hq + 1) * Wd], lhsT=qb[:, m, hh, :], rhs=Cb[d][:, m, :], start=False, stop=True),
                                  reads=[bqb, bC[d]], writes=[bpo], signal=(hq == 3))
                        pos_.append((po, bpo))
                    hdst = hacc[:, c, :]
                    tmpo, btmp = (hdst, bh[c]) if first else t32r.next()
                    if ml:
                        for b2 in range(2):
                            po, bpo = pos_[b2]
                            pv = po[:, 0:4 * Wd].rearrange("p (h w) -> p h w", w=Wd)
                            fw.op("dve", lambda e: e.tensor_tensor(gtt[:, 24 + b2 * 4:28 + b2 * 4].unsqueeze(2), pv[:, :, 64:65], cv[:, b2 * 4:(b2 + 1) * 4].unsqueeze(2), op=ALU.mult),
                                  reads=[bpo, bgt], writes=[bgt])
                        fw.op("dve", lambda e: e.scalar_tensor_tensor(gtt[:, 32:40], gtt[:, 24:32], -1.0, gtt[:, 24:32], op0=ALU.mult, op1=ALU.max), reads=[bgt], writes=[bgt])
                        fw.op("dve", lambda e: e.tensor_scalar(gtt[:, 24:32], gtt[:, 32:40], 1.0, None, op0=ALU.max), reads=[bgt], writes=[bgt])
                        fw.op("dve", lambda e: e.reciprocal(gtt[:, 32:40], gtt[:, 24:32]), reads=[bgt], writes=[bgt])
                        fw.op("dve", lambda e: e.tensor_tensor(gtt[:, 32:40], gtt[:, 32:40], cv, op=ALU.mult), reads=[bgt], writes=[bgt])
                        scv = gtt[:, 32:40]
                    else:
                        scv = cv
                    for b2 in range(2):
                        po, bpo = pos_[b2]
                        pv = po[:, 0:4 * Wd].rearrange("p (h w) -> p h w", w=Wd)
                        fw.op("dve", lambda e: e.tensor_tensor(tmpo[:, b2 * 256:(b2 + 1) * 256].rearrange("p (h d) -> p h d", d=64), pv[:, :, 0:64],
                                                                scv[:, b2 * 4:(b2 + 1) * 4].unsqueeze(2).to_broadcast([128, 4, 64]), op=ALU.mult),
                              reads=[bpo, bgt], writes=[btmp])
                    if not first:
                        fw.op("pool", lambda e: e.tensor_tensor(hdst, hdst, tmpo, op=ALU.add), reads=[btmp, bh[c]], writes=[bh[c]])
                    for b2 in range(2):
                        pu, bpu = psr.next()
                        for mm in range(2):
                            m = b2 * 2 + mm
                            fw.op("pe", lambda e: e.matmul(pu[:, mm * 2 * Wd:(mm + 1) * 2 * Wd], lhsT=kt[:, m * 128:(m + 1) * 128], rhs=va[:, 2 * m:2 * m + 2, :].rearrange("p h w -> p (h w)"), start=True, stop=True),
                                  reads=[bkt, bva], writes=[bpu], signal=(mm == 1))
                        puv = pu[:, 0:4 * Wd].rearrange("p (m h w) -> p m h w", m=2, h=2)
                        for hh in range(2):
                            sl = slice(hh * 64, (hh + 1) * 64)
                            fw.op("dve", lambda e: e.tensor_tensor(Cn[d][sl, b2 * 2:b2 * 2 + 2, :], Cn[d][sl, b2 * 2:b2 * 2 + 2, :], puv[sl, :, hh, :], op=ALU.add), reads=[bpu, bC[d]], writes=[bC[d]])
                    for hh in range(2):
                        sl = slice(hh * 64, (hh + 1) * 64)
                        dvv = dv[sl, hh:8:2].unsqueeze(2).to_broadcast([64, 4, Wd])
                        fw.op("pool", lambda e: e.tensor_tensor(Cn[d][sl, :, :], Cn[d][sl, :, :], dvv, op=ALU.mult), reads=[bC[d], bgt], writes=[bC[d]])
                    fw.op("act", lambda e: e.copy(Cb[d], Cn[d]), reads=[bC[d]], writes=[bC[d]])
                    if not first:
                        if ml:
                            def post(psb_, bp_, dst):
                                y32, by32 = y32r.next()
                                for m in range(4):
                                    fw.op("dve", lambda e: e.scalar_tensor_tensor(y32[:, m, :], xcc[:, m, :], skip[:, m:m + 1], psb_[:, m * 128:(m + 1) * 128], op0=ALU.mult, op1=ALU.add),
                                          reads=[bp_, bxcc, bsk], writes=[by32])
                                yTt, byT = yTr.next()
                                fw.op("pool", lambda e: e.tensor_tensor(yTt, y32, szc, op=ALU.mult), reads=[by32, bszc], writes=[byT])
                                fw.dma("sp", dst, yTt, reads=[byT])
                            self.finalize_tok(hdst, bh[c], ngb.unsqueeze(1).to_broadcast([128, 8, 64]), bng, t32r, str_, ybr, yTr, yv[:, :, r0:r0 + 128], post=post)
                        else:
                            self.finalize_tok(hdst, bh[c], gt, bg, t32r, str_, ybr, yTr, yv[:, :, r0:r0 + 128])

                return stageL, stageA, stageB

            self.scan_driver(orders, [make_dir(0), make_dir(1)])
            fw.barrier()

    IDENT, UINC, LINC, USTR, LSTR, ONES, BLK, BLK32 = range(8)

    def phase_ada(self):
        nc, fw, I = self.nc, self.fw, self.I
        self.modv = self.dscr("modv", [2, 2, 6, D])
        with ExitStack() as st:
            cc = self.sb(st, "cc", [128, 8, 2]); bcc = Buf()
            sc = self.sb(st, "sc", [128, 8, 2]); bsc = Buf()
            fw.dma("sp", cc, I["ccols"], writes=[bcc])
            fw.op("act", lambda e: e.activation(sc, cc, AF.Silu), reads=[bcc], writes=[bsc])
            wr = self.ring(st, "adw", [128, 8, 512], F32, 2)
            mod = self.sb(st, "mod", [2, 6 * D]); bmod = Buf()
            ab = self.sb(st, "adb", [2, 6 * D]); bab = Buf()
            ng = self.sb(st, "ng", [2, 4 * D]); bng = Buf()
            der = self.sb(st, "der", [2, 6 * D]); bder = Buf()
            for layer in range(2):
                fw.dma("sp", ab, I["ada_b"][layer:layer + 1, :].partition_broadcast(2), writes=[bab])
                fw.dma("sp", ng, I["norm_g"][layer:layer + 1].rearrange("o j d -> o (j d)").partition_broadcast(2), writes=[bng])
                wv = I["ada_w"][layer].rearrange("(kc p) n -> p kc n", p=128)
                for nb in range(12):
                    wt, bw = wr.next()
                    fw.dma("sp", wt, wv[:, :, nb * 512:(nb + 1) * 512], writes=[bw])
                    ps, bp = self.psr.next()
                    for kc in range(8):
                        fw.op("pe", lambda e: e.matmul(ps[0:2, :], lhsT=sc[:, kc, :], rhs=wt[:, kc, :], start=(kc == 0), stop=(kc == 7)),
                              reads=[bsc, bw], writes=[bp], signal=(kc == 7))
                    fw.op("dve", lambda e: e.tensor_tensor(mod[:, nb * 512:(nb + 1) * 512], ps[0:2, :], ab[:, nb * 512:(nb + 1) * 512], op=ALU.add),
                          reads=[bp, bab], writes=[bmod])
                m = lambda j: mod[:, j * D:(j + 1) * D]
                g = lambda j: ng[:, j * D:(j + 1) * D]
                dd = lambda j: der[:, j * D:(j + 1) * D]
                rw = dict(reads=[bmod, bng], writes=[bder])
                fw.op("dve", lambda e: e.scalar_tensor_tensor(dd(0), m(1), 1.0, g(0), op0=ALU.add, op1=ALU.mult), **rw)
                fw.op("dve", lambda e: e.tensor_copy(dd(1), m(0)), **rw)
                fw.op("dve", lambda e: e.tensor_tensor(dd(2), m(2), g(1), op=ALU.mult), **rw)
                fw.op("dve", lambda e: e.scalar_tensor_tensor(dd(3), m(4), 1.0, g(2), op0=ALU.add, op1=ALU.mult), **rw)
                fw.op("dve", lambda e: e.tensor_copy(dd(4), m(3)), **rw)
                fw.op("dve", lambda e: e.tensor_tensor(dd(5), m(5), g(3), op=ALU.mult), **rw)
                fw.dma("sp", self.modv[layer].rearrange("s j d -> s (j d)"), der, reads=[bder])
            fw.barrier()

    def load_bc(self, st, name, layer, stream, js):
        key = (id(st), name[:-1])
        if not hasattr(self, "_bc"):
            self._bc = {}
        if key not in self._bc:
            self._bc[key] = (self.sb(st, name, [128, len(js), D]), Buf())
        t, b = self._bc[key]
        for i, j in enumerate(js):
            self.fw.dma("sp", t[:, i, :], self.modv[layer, stream, j:j + 1, :].partition_broadcast(128), writes=[b])
        return t, b

    def rstd_col(self, ss, bss, out, tmp, n, scale):
        fw = self.fw
        fw.op("dve", lambda e: e.tensor_scalar(tmp, ss, scale, EPS, op0=ALU.mult, op1=ALU.add), reads=[bss], writes=[bss])
        fw.op("act", lambda e: e.activation(tmp, tmp, AF.Ln), reads=[bss], writes=[bss])
        fw.op("act", lambda e: e.activation(out, tmp, AF.Exp, scale=-0.5), reads=[bss], writes=[bss])

    def phase_inproj(self, layer):
        nc, fw, I = self.nc, self.fw, self.I
        cmb = self.cmb
        WC = W0C if layer == 0 else W1C
        wsrc = I["w_in0"] if layer == 0 else I["w_in1"]
        src_res = None if layer == 0 else self.res1
        S = {}
        if layer == 0:
            S["qAT"] = self.dscr("qAT", [256, NT]); S["kAT"] = self.dscr("kAT", [256, NT])
            S["kA"] = self.dscr("kA", [NT, 256]); S["vA"] = self.dscr("vA", [NT, 512], BF16)
            S["gA"] = self.dscr("gA", [NT, 512]); S["lgp"] = self.dscr("lgp", [NT, 512])
            S["qBT"] = self.dscr("qBT", [512, NT], BF16); S["kBT"] = self.dscr("kBT", [128, NT], BF16)
            S["vB"] = self.dscr("vB", [NT, 128], BF16)
        else:
            S["xmT"] = self.dscr("xmT", [512, NT], BF16); S["szT"] = self.dscr("szT", [512, NT])
            S["qDT"] = self.dscr("qDT", [512, NT], BF16); S["kDT"] = self.dscr("kDT", [512, NT], BF16)
            S["vD"] = self.dscr("vD", [NT, 512], BF16); S["gD"] = self.dscr("gD", [NT, 512])
        self.S.update(S)
        with ExitStack() as st:
            W = self.sb(st, "Win", [128, 8, WC], BF16); bWs = [Buf() for _ in range(8)]
            wv = wsrc.rearrange("(kc p) n -> p kc n", p=128)
            for kc in range(8):
                fw.dma("pool", W[:, kc, :], wv[:, kc, :], writes=[bWs[kc]])
            rope = self.sb(st, "rope", [128, 2, NT]); brope = Buf()
            fw.dma("sp", rope, I["rope"].rearrange("n p f -> p n f"), writes=[brope])
            small = self.sb(st, "small", [128, 16]); bsmall = Buf()
            ngbc = self.sb(st, "ngbc", [128, 64]); bng = Buf()
            if layer == 0:
                fw.dma("sp", small[:, 0:4], I["att_g"], writes=[bsmall])
                fw.dma("sp", ngbc, I["gla_ng"].partition_broadcast(128), writes=[bng])
                wg = self.sb(st, "wg", [17, 2, 256]); bwg = Buf()
                fw.dma("sp", wg, I["gla_wg"].rearrange("d k n -> k d n"), writes=[bwg])
                lra = [self.sb(st, "lra%d" % d, [32, 512]) for d in range(2)]
                blra = [Buf(), Buf()]
                for d in range(2):
                    fw.op("pool", lambda e: e.memset(lra[d], 1.0), writes=[blra[d]])
            else:
                fw.dma("sp", ngbc, I["ret_ng"].partition_broadcast(128), writes=[bng])
            xr = self.ring(st, "xt", [128, D], F32, 2)
            sqj = self.sb(st, "sqj", [128, D]); bsqj = Buf()
            xnr = self.ring(st, "xn", [128, D], F32, 2)
            ulr = self.ring(st, "ul", [128, D], BF16, 5)
            ulTr = self.ring(st, "ulT", [128, 8, 512], BF16, 2)
            str_ = self.ring(st, "stat", [128, 4], F32, 4)
            f32r = self.ring(st, "ev32", [128, 512], F32, 4)
            f32r2 = self.ring(st, "ev32b", [128, 512], F32, 4)
            bfr = self.ring(st, "evbf", [128, 512], BF16, 4)
            supers = [(0, 2)] + [(2 + 4 * i, 4) for i in range(8)]
            cur = {"stream": None, "bc": None}

            def pro_elem(t0, ntl):
                stream = 1 if t0 < 2 else 0
                if stream != cur["stream"]:
                    cur["bc"] = self.load_bc(st, "bc%d" % stream, layer, stream, [0, 1])
                    cur["stream"] = stream
                bc, bbc = cur["bc"]
                ulT, bulT = ulTr.next()
                uls = []
                for ti in range(ntl):
                    t = t0 + ti
                    xt, bx = xr.next()
                    if layer == 0:
                        srcap = I["ctx"][t * 128:(t + 1) * 128, :] if t < 2 else I["x"][(t - 2) * 128:(t - 1) * 128, :]
                    else:
                        srcap = src_res[t * 128:(t + 1) * 128, :]
                    fw.dma("sp", xt, srcap, writes=[bx])
                    stt, bst = str_.next()
                    fw.op("act", lambda e: e.activation(sqj, xt, AF.Square, accum_out=stt[:, 0:1]), reads=[bx], writes=[bsqj, bst])
                    self.rstd_col(stt[:, 0:1], bst, stt[:, 2:3], stt[:, 1:2], 1, 1.0 / D)
                    xn, bxn = xnr.next()
                    fw.op("dve", lambda e: e.scalar_tensor_tensor(xn, xt, stt[:, 2:3], bc[:, 0, :], op0=ALU.mult, op1=ALU.mult),
                          reads=[bx, bst, bbc], writes=[bxn])
                    ul, bul = ulr.next()
                    fw.op("pool", lambda e: e.tensor_tensor(ul, xn, bc[:, 1, :], op=ALU.add), reads=[bxn, bbc], writes=[bul])
                    uls.append((ul, bul))
                return {"ulT": ulT, "bulT": bulT, "uls": uls, "ntl": ntl, "done": 0}

            def pro_pe(stn, upto_ti):
                ulT, bulT = stn["ulT"], stn["bulT"]
                while stn["done"] < min(upto_ti, stn["ntl"]):
                    ti = stn["done"]
                    ul, bul = stn["uls"][ti]
                    ps, bp = self.psr.next()
                    psb = ps.bitcast(BF16)
                    for kc in range(8):
                        fw.op("pe", lambda e: e.transpose(psb[:, kc * 128:(kc + 1) * 128], ul[:, kc * 128:(kc + 1) * 128], cmb[:, self.IDENT, :]),
                              reads=[bul, self.bcm], writes=[bp], signal=(kc == 7))
                    fw.op("act", lambda e: e.copy(ulT[:, :, ti * 128:(ti + 1) * 128], psb.rearrange("p (k t) -> p k t", k=8)),
                          reads=[bp], writes=[bulT])
                    stn["done"] += 1

            nstate = pro_elem(*supers[0])
            pro_pe(nstate, 99)
            for si, (t0, ntl) in enumerate(supers):
                ulT, bulT = nstate["ulT"], nstate["bulT"]
                nstate = None
                N = ntl * 128
                c0 = t0 * 128

                def fm(col0, M):
                    ps, bp = self.psr.next()
                    for kc in range(8):
                        fw.op("pe", lambda e: e.matmul(ps[0:M, 0:N], lhsT=W[:, kc, col0:col0 + M], rhs=ulT[:, kc, 0:N], start=(kc == 0), stop=(kc == 7)),
                              reads=[bWs[kc], bulT], writes=[bp], signal=(kc == 7))
                    return ps, bp

                def tm(ti, col0, ncol):
                    ps, bp = self.psr.next()
                    for kc in range(8):
                        fw.op("pe", lambda e: e.matmul(ps[:, 0:ncol], lhsT=ulT[:, kc, ti * 128:(ti + 1) * 128], rhs=W[:, kc, col0:col0 + ncol], start=(kc == 0), stop=(kc == 7)),
                              reads=[bWs[kc], bulT], writes=[bp], signal=(kc == 7))
                    return ps, bp

                def rope_fm(col0, colp, gi, dst, scale, norm):
                    ps, bp = fm(col0, 128)
                    pp, bpp = fm(colp, 128)
                    t1, b1 = f32r.next()
                    t2, b2 = f32r2.next()
                    ob, bo = bfr.next()
                    cosv = rope[:, 0, c0:c0 + N]
                    sinv = rope[:, 1, c0:c0 + N]
                    if norm:
                        sq, bsq = bfr.next()
                        fw.op("act", lambda e: e.activation(sq[:, 0:N], ps[:, 0:N], AF.Square), reads=[bp], writes=[bsq])
                        ps2, bp2 = self.psr.next()
                        fw.op("pe", lambda e: e.matmul(ps2[:, 0:N], lhsT=cmb[:, self.BLK, :], rhs=sq[:, 0:N], start=True, stop=True),
                              reads=[bsq, self.bcm], writes=[bp2])
                        rs, brs = f32r.next()
                        fw.op("dve", lambda e: e.tensor_scalar(rs[:, 0:N], ps2[:, 0:N], 1.0 / 64, EPS, op0=ALU.mult, op1=ALU.add), reads=[bp2], writes=[brs])
                        fw.op("act", lambda e: e.activation(rs[:, 0:N], rs[:, 0:N], AF.Ln), reads=[brs], writes=[brs])
                        fw.op("act", lambda e: e.activation(rs[:, 0:N], rs[:, 0:N], AF.Exp, scale=-0.5), reads=[brs], writes=[brs])
                        fw.op("dve", lambda e: e.scalar_tensor_tensor(t1[:, 0:N], ps[:, 0:N], small[:, gi:gi + 1], cosv, op0=ALU.mult, op1=ALU.mult),
                              reads=[bp, bsmall, brope], writes=[b1])
                        fw.op("dve", lambda e: e.scalar_tensor_tensor(t2[:, 0:N], pp[:, 0:N], small[:, gi + 1:gi + 2], sinv, op0=ALU.mult, op1=ALU.mult),
                              reads=[bpp, bsmall, brope], writes=[b2])
                        fw.op("pool", lambda e: e.tensor_tensor(t1[:, 0:N], t1[:, 0:N], t2[:, 0:N], op=ALU.add), reads=[b1, b2], writes=[b1])
                        fw.op("pool", lambda e: e.tensor_tensor(ob[:, 0:N], t1[:, 0:N], rs[:, 0:N], op=ALU.mult), reads=[b1, brs], writes=[bo])
                    else:
                        fw.op("dve", lambda e: e.scalar_tensor_tensor(t1[:, 0:N], ps[:, 0:N], scale, cosv, op0=ALU.mult, op1=ALU.mult), reads=[bp, brope], writes=[b1])
                        fw.op("dve", lambda e: e.scalar_tensor_tensor(t2[:, 0:N], pp[:, 0:N], scale, sinv, op0=ALU.mult, op1=ALU.mult), reads=[bpp, brope], writes=[b2])
                        fw.op("pool", lambda e: e.tensor_tensor(ob[:, 0:N], t1[:, 0:N], t2[:, 0:N], op=ALU.add), reads=[b1, b2], writes=[bo])
                    fw.dma("sp", dst, ob[:, 0:N], reads=[bo])

                if layer == 0:
                    for j in range(2):
                        ps, bp = fm(0 + j * 128, 128)
                        t1, b1 = f32r.next()
                        fw.op("act", lambda e: e.activation(t1[:, 0:N], ps[:, 0:N], AF.Copy, scale=32 ** -0.5), reads=[bp], writes=[b1])
                        fw.dma("sp", S["qAT"][j * 128:(j + 1) * 128, c0:c0 + N], t1[:, 0:N], reads=[b1])
                        ps, bp = fm(256 + j * 128, 128)
                        t1, b1 = f32r.next()
                        fw.op("dve", lambda e: e.tensor_copy(t1[:, 0:N], ps[:, 0:N]), reads=[bp], writes=[b1])
                        fw.dma("sp", S["kAT"][j * 128:(j + 1) * 128, c0:c0 + N], t1[:, 0:N], reads=[b1])
                    for d in range(2):
                        ps, bp = fm(1536 + 16 * d, 16)
                        fw.op("dve", lambda e: e.tensor_copy(lra[d][0:16, 0:N], ps[0:16, 0:N]), reads=[bp], writes=[blra[d]])
                    for m in range(4):
                        rope_fm(1568 + m * 128, 2336 + m * 128, 0, S["qBT"][m * 128:(m + 1) * 128, c0:c0 + N], 1.0, True)
                    rope_fm(2080, 2336 + 512, 2, S["kBT"][:, c0:c0 + N], 1.0, True)
                    if si + 1 < len(supers):
                        nstate = pro_elem(*supers[si + 1])
                    for ti in range(ntl):
                        r0 = c0 + ti * 128
                        ps, bp = tm(ti, 256, 256)
                        t1, b1 = f32r.next()
                        fw.op("act", lambda e: e.copy(t1[:, 0:256], ps[:, 0:256]), reads=[bp], writes=[b1])
                        fw.dma("sp", S["kA"][r0:r0 + 128, :], t1[:, 0:256], reads=[b1])
                        ps, bp = tm(ti, 2208, 128)
                        ob, bo = bfr.next()
                        fw.op("dve", lambda e: e.tensor_copy(ob[:, 0:128], ps[:, 0:128]), reads=[bp], writes=[bo])
                        fw.dma("sp", S["vB"][r0:r0 + 128, :], ob[:, 0:128], reads=[bo])
                        ps, bp = tm(ti, 512, 512)
                        ob, bo = bfr.next()
                        fw.op("act", lambda e: e.copy(ob, ps), reads=[bp], writes=[bo])
                        fw.dma("sp", S["vA"][r0:r0 + 128, :], ob, reads=[bo])
                        ps, bp = tm(ti, 1024, 512)
                        t1, b1 = f32r.next()
                        fw.op("act", lambda e: e.activation(t1, ps, AF.Silu), reads=[bp], writes=[b1])
                        t2, b2 = f32r2.next()
                        fw.op("pool", lambda e: e.tensor_tensor(t2.rearrange("p (h d) -> p h d", d=64), t1.rearrange("p (h d) -> p h d", d=64),
                                                                 ngbc.unsqueeze(1).to_broadcast([128, 8, 64]), op=ALU.mult), reads=[b1, bng], writes=[b2])
                        fw.dma("sp", S["gA"][r0:r0 + 128, :], t2, reads=[b2])
                        ps, bp = self.psr.next()
                        for d in range(2):
                            fw.op("pe", lambda e: e.matmul(ps[:, d * 256:(d + 1) * 256], lhsT=lra[d][0:17, ti * 128:(ti + 1) * 128], rhs=wg[:, d, :], start=True, stop=True),
                                  reads=[blra[d], bwg], writes=[bp], signal=(d == 1))
                        t1, b1 = f32r.next()
                        fw.op("act", lambda e: e.activation(t1, ps, AF.Exp, scale=-1.0), reads=[bp], writes=[b1])
                        t2, b2 = f32r2.next()
                        fw.op("act", lambda e: e.activation(t2, t1, AF.Ln, bias=1.0), reads=[b1], writes=[b2])
                        fw.dma("sp", S["lgp"][r0:r0 + 128, :], t2, reads=[b2])
                        if nstate is not None:
                            pro_pe(nstate, ti + 1)
                    if nstate is not None:
                        pro_pe(nstate, 99)
                else:
                    for j in range(4):
                        ps, bp = fm(0 + j * 128, 128)
                        ob, bo = bfr.next()
                        fw.op("act", lambda e: e.copy(ob[:, 0:N], ps[:, 0:N]), reads=[bp], writes=[bo])
                        fw.dma("sp", S["xmT"][j * 128:(j + 1) * 128, c0:c0 + N], ob[:, 0:N], reads=[bo])
                        ps, bp = fm(512 + j * 128, 128)
                        t1, b1 = f32r.next()
                        fw.op("act", lambda e: e.activation(t1[:, 0:N], ps[:, 0:N], AF.Silu), reads=[bp], writes=[b1])
                        fw.dma("sp", S["szT"][j * 128:(j + 1) * 128, c0:c0 + N], t1[:, 0:N], reads=[b1])
                        rope_fm(1024 + j * 128, 3072 + j * 128, 0, S["qDT"][j * 128:(j + 1) * 128, c0:c0 + N], 0.125, False)
                        rope_fm(1536 + j * 128, 3072 + 512 + j * 128, 0, S["kDT"][j * 128:(j + 1) * 128, c0:c0 + N], 1.0, False)
                    if si + 1 < len(supers):
                        nstate = pro_elem(*supers[si + 1])
                    for ti in range(ntl):
                        r0 = c0 + ti * 128
                        ps, bp = tm(ti, 2048, 512)
                        ob, bo = bfr.next()
                        fw.op("act", lambda e: e.copy(ob, ps), reads=[bp], writes=[bo])
                        fw.dma("sp", S["vD"][r0:r0 + 128, :], ob, reads=[bo])
                        ps, bp = tm(ti, 2560, 512)
                        t1, b1 = f32r.next()
                        fw.op("act", lambda e: e.activation(t1, ps, AF.Silu), reads=[bp], writes=[b1])
                        t2, b2 = f32r2.next()
                        fw.op("pool", lambda e: e.tensor_tensor(t2.rearrange("p (h d) -> p h d", d=64), t1.rearrange("p (h d) -> p h d", d=64),
                                                                 ngbc.unsqueeze(1).to_broadcast([128, 8, 64]), op=ALU.mult), reads=[b1, bng], writes=[b2])
                        fw.dma("sp", S["gD"][r0:r0 + 128, :], t2, reads=[b2])
                        if nstate is not None:
                            pro_pe(nstate, ti + 1)
                    if nstate is not None:
                        pro_pe(nstate, 99)
            fw.barrier()


def _perm64():
    p = np.arange(64)
    return np.concatenate([p[16:32], p[0:16], p[48:64], p[32:48]])


def _rope_tables():
    axis_dim = 32
    inv = 10000.0 ** (-np.arange(0, axis_dim, 2, dtype=np.float32) / axis_dim)
    t = np.arange(NLAT)
    ang_r = (t // 64).astype(np.float32)[:, None] * inv[None, :]
    ang_c = (t % 64).astype(np.float32)[:, None] * inv[None, :]
    cos = np.concatenate([np.cos(ang_r), np.cos(ang_r), np.cos(ang_c), np.cos(ang_c)], axis=1)
    sin = np.concatenate([-np.sin(ang_r), np.sin(ang_r), -np.sin(ang_c), np.sin(ang_c)], axis=1)
    tab = np.zeros((2, 128, NT), np.float32)
    tab[0, :, :NCTX] = 1.0
    for hh in range(2):
        tab[0, hh * 64:(hh + 1) * 64, NCTX:] = cos.T
        tab[1, hh * 64:(hh + 1) * 64, NCTX:] = sin.T
    return tab


def _cmat():
    j = np.arange(128)[:, None]
    i = np.arange(128)[None, :]
    m = np.zeros((8, 128, 128), np.float32)
    m[0] = (j == i)
    m[1] = (j <= i)
    m[2] = (j >= i)
    m[3] = (j < i)
    m[4] = (j > i)
    m[5] = 1.0
    m[6] = ((j // 64) == (i // 64))
    m[7] = ((j // 32) == (i // 32))
    return m


def host_inputs(inp):
    f = lambda a: np.ascontiguousarray(np.asarray(a, dtype=np.float32))
    perm = _perm64()
    ab = f(inp["ab_w_in"][0])
    qB = ab[:, 1568:2080].reshape(D, 8, 64)[:, :, perm].reshape(D, 512)
    kB = ab[:, 2080:2208].reshape(D, 2, 64)[:, :, perm].reshape(D, 128)
    w_in0 = np.concatenate([ab, qB, kB], axis=1)
    cd = f(inp["cd_w_in"][0])
    qD = cd[:, 1024:1536].reshape(D, 8, 64)[:, :, perm].reshape(D, 512)
    kD = cd[:, 1536:2048].reshape(D, 8, 64)[:, :, perm].reshape(D, 512)
    w_in1 = np.concatenate([cd, qD, kD], axis=1)
    gla_wg = np.concatenate([f(inp["gla_w_gate"][0]), f(inp["gla_b_gate"][0])[:, None, :]], axis=1)
    aq = f(inp["att_qk_norm_g"][0])
    att_g = np.stack([np.tile(aq[0], 2), np.tile(aq[0][perm], 2), np.tile(aq[1], 2), np.tile(aq[1][perm], 2)], axis=1)
    cw = f(inp["ml_conv_w"][0])
    cb = f(inp["ml_conv_b"][0])
    ml_conv = np.stack([cw[0], cw[1], cw[2], cb], axis=1).reshape(4, 128, 4).transpose(1, 0, 2)
    wq = f(inp["ml_w_qkv"][0])
    ml_bd = np.zeros((3, 4, 128, 128), np.float32)
    for t in range(3):
        for h in range(8):
            m, hh = h // 2, h % 2
            ml_bd[t, m, hh * 64:(hh + 1) * 64, hh * 64:(hh + 1) * 64] = wq[t, h]
    mg = f(inp["ml_w_gate"][0])
    ml_wg = np.concatenate([mg[0], mg[1]], axis=1).reshape(12, 128, 32)
    ml_bg = f(inp["ml_b_gate"][0]).reshape(1, 32)
    ml_skip = f(inp["ml_skip"][0]).reshape(4, 128).T
    jj = np.arange(128, dtype=np.float32)
    jcol = np.stack([jj + 1, 128 - jj], axis=1)
    shared = {
        "ada_w": f(inp["ada_w"]), "ada_b": f(inp["ada_b"]), "norm_g": f(inp["norm_g"]), "w_out": f(inp["w_out"]),
        "mlp_w1": f(inp["mlp_w1"]), "mlp_w2": f(inp["mlp_w2"]), "w_in0": f(w_in0), "w_in1": f(w_in1),
        "gla_wg": f(gla_wg), "gla_ng": f(inp["gla_norm_g"]).reshape(1, 64), "att_g": f(att_g), "ml_conv": f(ml_conv),
        "ml_bd": ml_bd, "ml_wg": f(ml_wg), "ml_bg": ml_bg, "ml_ng": f(inp["ml_norm_g"]).reshape(1, 64), "ml_skip": f(ml_skip),
        "ret_logit": f(inp["ret_decay_logit"]).reshape(1, 16), "ret_ng": f(inp["ret_norm_g"]).reshape(1, 64),
        "cmat": _cmat(), "rope": _rope_tables(), "jcol": f(jcol),
    }
    maps = []
    x = np.asarray(inp["x"]); c = np.asarray(inp["c"]); ctx = np.asarray(inp["ctx"]); cc = np.asarray(inp["c_ctx"])
    for b in range(x.shape[0]):
        m = dict(shared)
        m["x"] = f(x[b]); m["ctx"] = f(ctx[b])
        m["ccols"] = f(np.stack([c[b].reshape(8, 128).T, cc.reshape(8, 128).T], axis=2))
        maps.append(m)
    return maps


_PROG = {}


def kernel(**inputs):
    maps = host_inputs(inputs)
    if "p" not in _PROG:
        p = Prog()
        p.build()
        _PROG["p"] = p
    p = _PROG["p"]
    res = run_bass_kernel_spmd(p.nc, maps, core_ids=list(range(8)))
    return np.stack([np.asarray(r["out"], dtype=np.float32) for r in res.results], axis=0)
```

```python
import math
from contextlib import ExitStack

import numpy as np
import concourse.bass as bass
import concourse.mybir as mybir
from concourse.bass_utils import run_bass_kernel_spmd

F32 = mybir.dt.float32
BF16 = mybir.dt.bfloat16
ALU = mybir.AluOpType
AF = mybir.ActivationFunctionType
AX = mybir.AxisListType

D = 1024
NCTX = 256
NLAT = 4096
NT = NCTX + NLAT
NTILE = NT // 128
EPS = 1e-6
W0C = 2336 + 512 + 128
W1C = 3072 + 512 + 512


class Buf:
    __slots__ = ("w", "r")

    def __init__(self):
        self.w = None
        self.r = {}


class FW:
    NDMA = 24

    def __init__(self, nc, stack):
        self.nc = nc
        self.eng = {"pe": nc.tensor, "act": nc.scalar, "dve": nc.vector, "pool": nc.gpsimd, "sp": nc.sync}
        self.sems = {}
        self.cnt = {}
        for k in ("pe", "act", "dve", "pool"):
            self.sems[k] = stack.enter_context(nc.semaphore("s_" + k))
            self.cnt[k] = 0
        for i in range(self.NDMA):
            k = "d%d" % i
            self.sems[k] = stack.enter_context(nc.semaphore("s_" + k))
            self.cnt[k] = 0
        self.rr = 0
        self.seen = {e: {} for e in self.eng}
        self.nins = 0

    def wait(self, e, ticket):
        if ticket is None:
            return
        k, v = ticket
        if self.seen[e].get(k, 0) >= v:
            return
        self.eng[e].wait_ge(self.sems[k], v)
        self.seen[e][k] = v

    def deps(self, e, reads, writes):
        for b in reads:
            self.wait(e, b.w)
        for b in writes:
            if b.w is not None and b.w[0] != e:
                self.wait(e, b.w)
            for k, v in b.r.items():
                if k != e:
                    self.wait(e, (k, v))

    def mark(self, ticket, reads, writes):
        k, v = ticket
        for b in reads:
            if b.r.get(k, 0) < v:
                b.r[k] = v
        for b in writes:
            b.w = ticket
            b.r = {}

    def op(self, e, fn, reads=(), writes=(), signal=True):
        self.deps(e, reads, writes)
        ins = fn(self.eng[e])
        self.nins += 1
        if signal:
            self.cnt[e] += 1
            ins.then_inc(self.sems[e], 1)
            t = (e, self.cnt[e])
        else:
            t = (e, self.cnt[e] + 1)
        self.mark(t, reads, writes)
        return t

    def dma(self, e, out, in_, reads=(), writes=(), **kw):
        self.deps(e, reads, writes)
        i = self.rr
        self.rr = (self.rr + 1) % self.NDMA
        k = "d%d" % i
        if self.cnt[k] > 0:
            self.wait(e, (k, self.cnt[k]))
        ins = self.eng[e].dma_start(out=out, in_=in_, **kw)
        self.cnt[k] += 16
        ins.then_inc(self.sems[k], 16)
        self.nins += 1
        t = (k, self.cnt[k])
        self.mark(t, reads, writes)
        return t

    def barrier(self):
        for e in self.eng:
            for k, v in self.cnt.items():
                if v > 0:
                    self.wait(e, (k, v))


def TP(hp):
    return {"tile_position": (96, 0)} if hp == 3 else {}


class Ring:
    def __init__(self, aps):
        self.aps = aps
        self.bufs = [Buf() for _ in aps]
        self.i = 0

    def next(self):
        j = self.i
        self.i = (self.i + 1) % len(self.aps)
        return self.aps[j], self.bufs[j]


class Prog:
    def __init__(self, dbg=False, upto=99):
        self.dbg = dbg
        self.upto = upto
        self.nc = bass.Bass("TRN2", target_bir_lowering=False)
        self.dbg_names = []

    def din(self, name, shape, dt=F32):
        return self.nc.dram_tensor(name, list(shape), dt, kind="ExternalInput").ap()

    def dscr(self, name, shape, dt=F32):
        if self.dbg:
            self.dbg_names.append(name)
            return self.nc.dram_tensor(name, list(shape), dt, kind="ExternalOutput").ap()
        return self.nc.dram_tensor(name, list(shape), dt).ap()

    def sb(self, st, name, shape, dt=F32):
        self._n = getattr(self, "_n", 0) + 1
        return st.enter_context(self.nc.sbuf_tensor("sb%d_%s" % (self._n, name), list(shape), dt)).ap()

    def ring(self, st, name, shape, dt, n):
        return Ring([self.sb(st, "%s%d" % (name, i), shape, dt) for i in range(n)])

    def build(self):
        nc = self.nc
        I = {}
        I["x"] = self.din("x", [NLAT, D])
        I["ctx"] = self.din("ctx", [NCTX, D])
        I["ccols"] = self.din("ccols", [128, 8, 2])
        I["ada_w"] = self.din("ada_w", [2, D, 6 * D])
        I["ada_b"] = self.din("ada_b", [2, 6 * D])
        I["norm_g"] = self.din("norm_g", [2, 4, D])
        I["w_out"] = self.din("w_out", [2, D, D])
        I["mlp_w1"] = self.din("mlp_w1", [2, D, 4 * D])
        I["mlp_w2"] = self.din("mlp_w2", [2, 4 * D, D])
        I["w_in0"] = self.din("w_in0", [D, W0C])
        I["w_in1"] = self.din("w_in1", [D, W1C])
        I["gla_wg"] = self.din("gla_wg", [2, 17, 256])
        I["gla_ng"] = self.din("gla_ng", [1, 64])
        I["att_g"] = self.din("att_g", [128, 4])
        I["ml_conv"] = self.din("ml_conv", [128, 4, 4])
        I["ml_bd"] = self.din("ml_bd", [3, 4, 128, 128])
        I["ml_wg"] = self.din("ml_wg", [12, 128, 32])
        I["ml_bg"] = self.din("ml_bg", [1, 32])
        I["ml_ng"] = self.din("ml_ng", [1, 64])
        I["ml_skip"] = self.din("ml_skip", [128, 4])
        I["ret_logit"] = self.din("ret_logit", [1, 16])
        I["ret_ng"] = self.din("ret_ng", [1, 64])
        I["cmat"] = self.din("cmat", [8, 128, 128])
        I["rope"] = self.din("rope", [2, 128, NT])
        I["jcol"] = self.din("jcol", [128, 2])
        self.I = I
        self.out = nc.dram_tensor("out", [NLAT, D], F32, kind="ExternalOutput").ap()

        with ExitStack() as gst:
            self.fw = FW(nc, gst)
            fw = self.fw
            self.psr = Ring([gst.enter_context(nc.psum_tensor("ps%d" % i, [128, 512], F32)).ap() for i in range(8)])
            self.cm = self.sb(gst, "cm", [128, 8, 128], F32)
            self.cmb = self.sb(gst, "cmb", [128, 8, 128], BF16)
            self.bcm = Buf()
            fw.dma("sp", self.cm, I["cmat"].rearrange("n p f -> p n f"), writes=[self.bcm])
            fw.dma("pool", self.cmb, I["cmat"].rearrange("n p f -> p n f"), writes=[self.bcm])
            self.psb = self.psr.aps
            self.S = {}
            self.S["yT0"] = self.dscr("yT0", [D, NT], BF16)
            self.S["yT1"] = self.dscr("yT1", [D, NT], BF16)
            self.res1 = self.dscr("res1", [NT, D])
            self.phase_ada()
            for layer in range(2):
                u = self.upto - 10 * layer
                if u >= 1:
                    self.phase_inproj(layer)
                if layer == 0:
                    if u >= 2:
                        self.phase_attn()
                    if u >= 3:
                        self.phase_gla()
                else:
                    if u >= 2:
                        self.phase_mlstm()
                    if u >= 3:
                        self.phase_ret()
                if u >= 4:
                    self.phase_outproj(layer)
                if u >= 5:
                    self.phase_mlp(layer)
            fw.barrier()
        return nc

    def psring(self, idx):
        return Ring([self.psb[i] for i in idx])

    def phase_attn(self):
        nc, fw, S = self.nc, self.fw, self.S
        yT = S["yT0"]
        with ExitStack() as st:
            kd = [self.sb(st, "kd%d" % g, [128, NT], BF16) for g in range(2)]
            bkd = [Buf(), Buf()]
            va = [self.sb(st, "va%d" % g, [128, NTILE, 128], BF16) for g in range(2)]
            bva = [Buf(), Buf()]
            for g in range(2):
                for hh in range(2):
                    fw.dma("sp", kd[g][hh * 64:(hh + 1) * 64, :], S["kBT"][g * 64:(g + 1) * 64, :], writes=[bkd[g]])
                fw.op("pool", lambda e: e.memset(va[g], 1.0), writes=[bva[g]])
                vsrc = S["vB"][:, g * 64:(g + 1) * 64].rearrange("(kb p) d -> p kb d", p=128)
                for q4 in range(0, NTILE, 8):
                    q5 = min(NTILE, q4 + 8)
                    fw.dma("sp", va[g][:, q4:q5, 0:64], vsrc[:, q4:q5, :], writes=[bva[g]])
            qr = self.ring(st, "qT", [128, 2, 256], BF16, 4)
            for qa, qb_ in zip(qr.aps, qr.bufs):
                fw.op("pool", lambda e: e.memset(qa, 0.0), writes=[qb_])
            pr = self.ring(st, "pT", [128, 512], BF16, 4)
            recr = self.ring(st, "rec", [64, 512], F32, 2)
            outr = self.ring(st, "ao", [64, 512], BF16, 3)
            accr = self.psring([0, 1])
            sr = self.psring([2, 3, 4, 5, 6, 7])
            work = [(t0, m) for t0 in range(0, NTILE, 2) for m in range(4)]

            def load_q(t0, m):
                qT, bq = qr.next()
                for hh in range(2):
                    fw.dma("sp", qT[hh * 64:(hh + 1) * 64, hh, :], S["qBT"][m * 128 + hh * 64:m * 128 + (hh + 1) * 64, t0 * 128:t0 * 128 + 256], writes=[bq])
                return qT, bq

            qnext = load_q(*work[0])
            for wi, (t0, m) in enumerate(work):
                if True:
                    c0 = t0 * 128
                    blocks = [0, 1] if t0 < 2 else list(range(NTILE))
                    g = m // 2
                    qT, bq = qnext
                    if wi + 1 < len(work):
                        qnext = load_q(*work[wi + 1])
                    acc, bacc = accr.next()
                    pend = []

                    def s_mm(kb):
                        s_, bs_ = sr.next()
                        fw.op("pe", lambda e: e.matmul(s_, lhsT=kd[g][:, kb * 128:(kb + 1) * 128], rhs=qT.rearrange("p h t -> p (h t)"), start=True, stop=True),
                              reads=[bkd[g], bq], writes=[bs_])
                        p_, bp_ = pr.next()
                        fw.op("act", lambda e: e.activation(p_, s_, AF.Exp, scale=0.125), reads=[bs_], writes=[bp_])
                        pend.append((kb, p_, bp_))

                    def pv_mm():
                        kb, p_, bp_ = pend.pop(0)
                        for hh in range(2):
                            fw.op("pe", lambda e: e.matmul(acc[:, hh * 256:(hh + 1) * 256], lhsT=va[g][:, kb, :], rhs=p_[:, hh * 256:(hh + 1) * 256], start=(kb == blocks[0] and hh == 0), stop=(kb == blocks[-1])),
                                  reads=[bva[g], bp_], writes=[bacc], signal=(hh == 1))

                    for kb in blocks:
                        s_mm(kb)
                        if len(pend) > 2:
                            pv_mm()
                    while pend:
                        pv_mm()
                    rec, brec = recr.next()
                    fw.op("dve", lambda e: e.reciprocal(rec, acc[64:128, :]), reads=[bacc], writes=[brec])
                    ao, bao = outr.next()
                    fw.op("dve", lambda e: e.tensor_tensor(ao, acc[0:64, :], rec, op=ALU.mult), reads=[bacc, brec], writes=[bao])
                    fw.dma("sp", yT[512 + m * 128:512 + (m + 1) * 128, c0:c0 + 256].rearrange("(hh d) t -> d hh t", hh=2), ao.rearrange("p (hh t) -> p hh t", hh=2), reads=[bao])
            fw.barrier()

    def scan_driver(self, orders, mk):
        n = len(orders[0])
        Ls = [dict(), dict()]
        As = [dict(), dict()]
        for step in range(-2, n):
            for d in range(2):
                if 0 <= step + 2 < n:
                    Ls[d][step + 2] = mk[d][0](orders[d][step + 2])
            for d in range(2):
                if 0 <= step + 1 < n:
                    As[d][step + 1] = mk[d][1](orders[d][step + 1], Ls[d].pop(step + 1))
            for d in range(2):
                if step >= 0:
                    mk[d][2](orders[d][step], As[d].pop(step))

    def phase_gla(self):
        nc, fw, S, cm, cmb = self.nc, self.fw, self.S, self.cm, self.cmb
        yT = S["yT0"]
        with ExitStack() as st:
            oacc = self.sb(st, "oacc", [128, NTILE, 512]); boacc = [Buf() for _ in range(NTILE)]
            Sf = [[self.sb(st, "Sf%d%d" % (d, i), [128, 64]) for i in range(2)] for d in range(2)]
            Sb = [[self.sb(st, "Sb%d%d" % (d, i), [128, 64], BF16) for i in range(2)] for d in range(2)]
            bS = [[Buf(), Buf()], [Buf(), Buf()]]
            hm = self.sb(st, "hm", [128, 4]); bhm = Buf()
            fw.op("dve", lambda e: e.tensor_copy(hm, cm[:, self.BLK32, 0:128:32]), reads=[self.bcm], writes=[bhm])
            RL, RW = 8, 6
            qTr = self.ring(st, "gq", [128, 2, 128], F32, RL)
            kTr = self.ring(st, "gk", [128, 2, 128], F32, RL)
            ktr = self.ring(st, "gkt", [128, 256], F32, RL)
            vr = self.ring(st, "gv", [128, 512], BF16, RL)
            lgr = self.ring(st, "glg", [128, 256], F32, RL)
            gr = self.ring(st, "gg", [128, 512], F32, 8)
            Eqr = self.ring(st, "Eq", [128, 2, 128], F32, RW)
            Ekr = self.ring(st, "Ek", [128, 2, 128], F32, 4)
            Err = self.ring(st, "Er", [128, 256], F32, 4)
            qddr = self.ring(st, "qdd", [128, 2, 128], F32, 4)
            qdr = self.ring(st, "qd", [128, 2, 4, 128], BF16, RW)
            kir = self.ring(st, "ki", [128, 2, 128], BF16, 4)
            kdr = self.ring(st, "kdd", [128, 256], BF16, RW)
            Amr = self.ring(st, "Am", [128, 8, 128], BF16, RW)
            t32r = self.ring(st, "gt32", [128, 512], F32, 3)
            ybr = self.ring(st, "gyb", [128, 512], BF16, 2)
            yTr = self.ring(st, "gyT", [128, 4, 128], BF16, 2)
            str_ = self.ring(st, "gst", [128, 24], F32, 2)
            psr = self.psr
            yv = yT[0:512, :].rearrange("(m p) t -> p m t", p=128)
            orders = [list(range(NTILE)), [1, 0] + list(range(NTILE - 1, 1, -1))]
            pos = [{c: i for i, c in enumerate(o)} for o in orders]
            last_dir = {c: (1 if pos[1][c] >= pos[0][c] else 0) for c in range(NTILE)}
            for d in range(2):
                for hc in range(2):
                    fw.op("dve", lambda e: e.memset(Sf[d][hc], 0.0), writes=[bS[d][hc]])
                    fw.op("dve", lambda e: e.memset(Sb[d][hc], 0.0), writes=[bS[d][hc]])

            def make_dir(d):
                TRI = self.UINC if d == 0 else self.LINC
                TRIS = self.LSTR if d == 0 else self.USTR
                ecol = 127 if d == 0 else 0

                def stageL(c):
                    r0 = c * 128
                    qT, bq = qTr.next(); kT, bk = kTr.next(); kt, bkt = ktr.next(); v, bv = vr.next(); lg, blg = lgr.next()
                    fw.dma("sp", qT, S["qAT"][:, r0:r0 + 128].rearrange("(hc p) t -> p hc t", p=128), writes=[bq])
                    fw.dma("sp", kT, S["kAT"][:, r0:r0 + 128].rearrange("(hc p) t -> p hc t", p=128), writes=[bk])
                    fw.dma("sp", kt, S["kA"][r0:r0 + 128, :], writes=[bkt])
                    fw.dma("sp", v, S["vA"][r0:r0 + 128, :], writes=[bv])
                    fw.dma("sp", lg, S["lgp"][r0:r0 + 128, d * 256:(d + 1) * 256], writes=[blg])
                    gt = bg = None
                    if last_dir[c] == d:
                        gt, bg = gr.next()
                        fw.dma("sp", gt, S["gA"][r0:r0 + 128, :], writes=[bg])
                    return locals()

                def stageA(c, L):
                    qT, bq, kT, bk, kt, bkt, v, bv, lg, blg = (L[k] for k in ("qT", "bq", "kT", "bk", "kt", "bkt", "v", "bv", "lg", "blg"))
                    gt, bg, r0 = L["gt"], L["bg"], L["r0"]
                    pc, bpc = psr.next()
                    for hc in range(2):
                        fw.op("pe", lambda e: e.matmul(pc[:, hc * 128:(hc + 1) * 128], lhsT=lg[:, hc * 128:(hc + 1) * 128], rhs=cm[:, TRI, :], start=True, stop=True),
                              reads=[blg, self.bcm], writes=[bpc], signal=False)
                    fw.op("pe", lambda e: e.matmul(pc[:, 256:512], lhsT=cm[:, TRIS, :], rhs=lg, start=True, stop=True), reads=[blg, self.bcm], writes=[bpc])
                    Eq, bEq = Eqr.next(); Ek, bEk = Ekr.next(); Er, bEr = Err.next()
                    fw.op("act", lambda e: e.activation(Eq.rearrange("p a b -> p (a b)"), pc[:, 0:256], AF.Exp, scale=-1.0 / 16), reads=[bpc], writes=[bEq])
                    fw.op("act", lambda e: e.activation(Ek.rearrange("p a b -> p (a b)"), pc[:, 0:256], AF.Exp, scale=1.0 / 16), reads=[bpc], writes=[bEk])
                    fw.op("act", lambda e: e.activation(Er, pc[:, 256:512], AF.Exp, scale=-1.0 / 16), reads=[bpc], writes=[bEr])
                    qdd, bqdd = qddr.next(); qd, bqd = qdr.next(); ki, bki = kir.next(); kdd, bkdd = kdr.next()
                    fw.op("dve", lambda e: e.tensor_tensor(qdd, qT, Eq, op=ALU.mult), reads=[bq, bEq], writes=[bqdd])
                    import os
                    if os.environ.get("GLA_Q4D", "1") == "1":
                        for hc in range(2):
                            fw.op("dve", lambda e: e.tensor_tensor(qd[:, hc, :, :], qdd[:, hc, :].unsqueeze(1).to_broadcast([128, 4, 128]),
                                                                    hm.unsqueeze(2).to_broadcast([128, 4, 128]), op=ALU.mult), reads=[bqdd, bhm], writes=[bqd])
                    else:
                        for hc in range(2):
                            for hp in range(4):
                                fw.op("dve", lambda e: e.tensor_scalar(qd[:, hc, hp, :], qdd[:, hc, :], hm[:, hp:hp + 1], None, op0=ALU.mult), reads=[bqdd, bhm], writes=[bqd])
                    fw.op("pool", lambda e: e.tensor_tensor(ki, kT, Ek, op=ALU.mult), reads=[bk, bEk], writes=[bki])
                    fw.op("pool", lambda e: e.tensor_tensor(kdd, kt, Er, op=ALU.mult), reads=[bkt, bEr], writes=[bkdd])
                    Am, bAm = Amr.next()
                    for hc in range(2):
                        pa, bpa = psr.next()
                        fw.op("pe", lambda e: e.matmul(pa, lhsT=ki[:, hc, :], rhs=qd[:, hc, :, :].rearrange("p h t -> p (h t)"), start=True, stop=True),
                              reads=[bki, bqd], writes=[bpa])
                        fw.op("dve", lambda e: e.tensor_tensor(Am[:, hc * 4:(hc + 1) * 4, :], pa.rearrange("p (h t) -> p h t", h=4),
                                                                cm[:, TRI, :].unsqueeze(1).to_broadcast([128, 4, 128]), op=ALU.mult), reads=[bpa, self.bcm], writes=[bAm])
                    return locals()

                def stageB(c, L):
                    r0, qd, bqd, v, bv, Am, bAm, kdd, bkdd, Eq, bEq = (L[k] for k in ("r0", "qd", "bqd", "v", "bv", "Am", "bAm", "kdd", "bkdd", "Eq", "bEq"))
                    gt, bg = L["gt"], L["bg"]
                    po, bpo = psr.next()
                    for h in range(8):
                        hc, hp = h // 4, h % 4
                        fw.op("pe", lambda e: e.matmul(po[:, h * 64:(h + 1) * 64], lhsT=Am[:, h, :], rhs=v[:, h * 64:(h + 1) * 64], start=True, stop=False),
                              reads=[bAm, bv], writes=[bpo], signal=False)
                        fw.op("pe", lambda e: e.matmul(po[:, h * 64:(h + 1) * 64], lhsT=qd[:, hc, hp, :], rhs=Sb[d][hc], start=False, stop=True),
                              reads=[bqd, bS[d][hc]], writes=[bpo], signal=(h == 7))
                    if last_dir[c] != d:
                        fw.op("act", lambda e: e.copy(oacc[:, c, :], po), reads=[bpo], writes=[boacc[c]])
                    else:
                        fw.op("dve", lambda e: e.tensor_tensor(oacc[:, c, :], oacc[:, c, :], po, op=ALU.add), reads=[bpo, boacc[c]], writes=[boacc[c]])
                    for hc in range(2):
                        pu, bpu = psr.next()
                        fw.op("pe", lambda e: e.matmul(pu, lhsT=kdd[:, hc * 128:(hc + 1) * 128], rhs=v, start=True, stop=True), reads=[bkdd, bv], writes=[bpu])
                        for hp in range(4):
                            h = hc * 4 + hp
                            sl = slice(hp * 32, (hp + 1) * 32)
                            fw.op("dve", lambda e: e.scalar_tensor_tensor(Sf[d][hc][sl, :], Sf[d][hc][sl, :], Eq[sl, hc, ecol:ecol + 1], pu[sl, h * 64:(h + 1) * 64], op0=ALU.mult, op1=ALU.add),
                                  reads=[bpu, bEq, bS[d][hc]], writes=[bS[d][hc]])
                        fw.op("act", lambda e: e.copy(Sb[d][hc], Sf[d][hc]), reads=[bS[d][hc]], writes=[bS[d][hc]])
                    if last_dir[c] == d:
                        self.finalize_tok(oacc[:, c, :], boacc[c], gt, bg, t32r, str_, ybr, yTr, yv[:, :, r0:r0 + 128])

                return stageL, stageA, stageB

            self.scan_driver(orders, [make_dir(0), make_dir(1)])
            fw.barrier()

    def finalize_tok(self, o, bo, gt, bg, t32r, str_, ybr, yTr, dst, post=None):
        fw, cmb = self.fw, self.cmb
        t1, b1 = t32r.next()
        fw.op("pool", lambda e: e.tensor_tensor(t1, o, o, op=ALU.mult), reads=[bo], writes=[b1])
        stt, bst = str_.next()
        fw.op("dve", lambda e: e.tensor_reduce(stt[:, 0:8], t1.rearrange("p (h d) -> p h d", d=64), axis=AX.X, op=ALU.add), reads=[b1], writes=[bst])
        self.rstd_col(stt[:, 0:8], bst, stt[:, 16:24], stt[:, 8:16], 8, 1.0 / 64)
        t2, b2 = t32r.next()
        fw.op("dve", lambda e: e.tensor_tensor(t2.rearrange("p (h d) -> p h d", d=64), o.rearrange("p (h d) -> p h d", d=64),
                                                stt[:, 16:24].unsqueeze(2).to_broadcast([128, 8, 64]), op=ALU.mult), reads=[bo, bst], writes=[b2])
        yb, byb = ybr.next()
        if len(gt.shape) == 3:
            fw.op("pool", lambda e: e.tensor_tensor(yb.rearrange("p (h d) -> p h d", d=64), t2.rearrange("p (h d) -> p h d", d=64), gt, op=ALU.mult), reads=[b2, bg], writes=[byb])
        else:
            fw.op("pool", lambda e: e.tensor_tensor(yb, t2, gt, op=ALU.mult), reads=[b2, bg], writes=[byb])
        ps, bp = self.psr.next()
        psb = ps.bitcast(BF16)
        for m in range(4):
            fw.op("pe", lambda e: e.transpose(psb[:, m * 128:(m + 1) * 128], yb[:, m * 128:(m + 1) * 128], cmb[:, self.IDENT, :]),
                  reads=[byb, self.bcm], writes=[bp], signal=(m == 3))
        if post is not None:
            post(psb, bp, dst)
            return
        yTt, byT = yTr.next()
        fw.op("act", lambda e: e.copy(yTt, psb[:, 0:512].rearrange("p (m t) -> p m t", m=4)), reads=[bp], writes=[byT])
        fw.dma("sp", dst, yTt, reads=[byT])

    def phase_outproj(self, layer):
        nc, fw, I, S, cmb = self.nc, self.fw, self.I, self.S, self.cmb
        yT = S["yT%d" % layer]
        with ExitStack() as st:
            Wo = self.sb(st, "Wo", [128, 8, D], BF16); bWo = Buf()
            fw.dma("pool", Wo, I["w_out"][layer].rearrange("(kc p) n -> p kc n", p=128), writes=[bWo])
            yr = self.ring(st, "oy", [128, 8, 128], BF16, 3)
            xr = self.ring(st, "ox", [128, D], F32, 3)
            tr = self.ring(st, "ot", [128, D], F32, 2)
            outr = self.ring(st, "oo", [128, D], F32, 2)
            sqj = self.sb(st, "osq", [128, 512]); bsqj = Buf()
            str_ = self.ring(st, "ost", [128, 8], F32, 4)
            cur_stream = None
            tiles = list(range(NTILE)) if layer == 0 else list(range(2, NTILE))
            for t in tiles:
                stream = 1 if t < 2 else 0
                if stream != cur_stream:
                    bc, bbc = self.load_bc(st, "obc%d" % stream, layer, stream, [2])
                    cur_stream = stream
                r0 = t * 128
                yt, by = yr.next()
                fw.dma("sp", yt, yT[:, r0:r0 + 128].rearrange("(kc p) t -> p kc t", p=128), writes=[by])
                xt, bx = xr.next()
                if layer == 0:
                    srcap = I["ctx"][r0:r0 + 128, :] if t < 2 else I["x"][r0 - 256:r0 - 128, :]
                else:
                    srcap = self.res1[r0:r0 + 128, :]
                fw.dma("sp", xt, srcap, writes=[bx])
                pss = []
                stt, bst = str_.next()
                for n in range(2):
                    ps, bp = self.psr.next()
                    for kc in range(8):
                        fw.op("pe", lambda e: e.matmul(ps, lhsT=yt[:, kc, :], rhs=Wo[:, kc, n * 512:(n + 1) * 512], start=(kc == 0), stop=(kc == 7)),
                              reads=[by, bWo], writes=[bp], signal=(kc == 7))
                    fw.op("act", lambda e: e.activation(sqj, ps, AF.Square, accum_out=stt[:, n:n + 1]), reads=[bp], writes=[bsqj, bst])
                    pss.append((ps, bp))
                fw.op("dve", lambda e: e.tensor_tensor(stt[:, 2:3], stt[:, 0:1], stt[:, 1:2], op=ALU.add), reads=[bst], writes=[bst])
                self.rstd_col(stt[:, 2:3], bst, stt[:, 4:5], stt[:, 3:4], 1, 1.0 / D)
                tt, btt = tr.next()
                for n in range(2):
                    ps, bp = pss[n]
                    fw.op("dve", lambda e: e.scalar_tensor_tensor(tt[:, n * 512:(n + 1) * 512], ps, stt[:, 4:5], bc[:, 0, n * 512:(n + 1) * 512], op0=ALU.mult, op1=ALU.mult),
                          reads=[bp, bst, bbc], writes=[btt])
                ot, bo = outr.next()
                fw.op("pool", lambda e: e.tensor_tensor(ot, tt, xt, op=ALU.add), reads=[btt, bx], writes=[bo])
                fw.dma("sp", self.res1[r0:r0 + 128, :], ot, reads=[bo])
            fw.barrier()

    def phase_mlp(self, layer):
        nc, fw, I, S, cmb = self.nc, self.fw, self.I, self.S, self.cmb
        with ExitStack() as st:
            W1 = self.sb(st, "W1", [128, 8, 4 * D], BF16); bW1s = [Buf() for _ in range(8)]
            W2 = self.sb(st, "W2", [128, 32, D], BF16); bW2s = [Buf() for _ in range(8)]
            w1v = I["mlp_w1"][layer].rearrange("(kc p) n -> p kc n", p=128)
            w2v = I["mlp_w2"][layer].rearrange("(kc p) n -> p kc n", p=128)
            for kc in range(8):
                fw.dma("pool", W1[:, kc, :], w1v[:, kc, :], writes=[bW1s[kc]])
            for k4 in range(0, 32, 4):
                fw.dma("pool", W2[:, k4:k4 + 4, :], w2v[:, k4:k4 + 4, :], writes=[bW2s[k4 // 4]])
            xr = self.ring(st, "mx", [128, D], F32, 4)
            tr = self.ring(st, "mt", [128, D], F32, 2)
            ulr = self.ring(st, "mul", [128, D], BF16, 2)
            uTr = self.ring(st, "muT", [128, 8, 256], BF16, 2)
            rlr = self.ring(st, "mrl", [128, 256], F32, 4)
            hr = self.ring(st, "mh", [128, 256], BF16, 8)
            vr = self.ring(st, "mv", [128, D], F32, 2)
            sqj = self.sb(st, "msq", [128, D], BF16); bsqj = Buf()
            str_ = self.ring(st, "mst", [128, 8], F32, 8)
            accr = self.psring([0, 1, 2, 3])
            wr = self.psring([4, 5, 6, 7])
            t0s = list(range(0, NTILE, 2)) if layer == 0 else list(range(2, NTILE, 2))
            stream_of = lambda t0: 1 if t0 < 2 else 0
            cur = {"stream": None, "bc": None}

            def pro_elem(t0):
                if stream_of(t0) != cur["stream"]:
                    cur["bc"] = self.load_bc(st, "mbc%d" % stream_of(t0), layer, stream_of(t0), [3, 4, 5])
                    cur["stream"] = stream_of(t0)
                bc, bbc = cur["bc"]
                xs = []
                uls = []
                uT, buT = uTr.next()
                for ti in range(2):
                    r0 = (t0 + ti) * 128
                    xt, bx = xr.next()
                    fw.dma("sp", xt, self.res1[r0:r0 + 128, :], writes=[bx])
                    xs.append((xt, bx))
                    stt, bst = str_.next()
                    fw.op("act", lambda e: e.activation(sqj, xt, AF.Square, accum_out=stt[:, 0:1]), reads=[bx], writes=[bsqj, bst])
                    self.rstd_col(stt[:, 0:1], bst, stt[:, 2:3], stt[:, 1:2], 1, 1.0 / D)
                    tt, btt = tr.next()
                    fw.op("dve", lambda e: e.scalar_tensor_tensor(tt, xt, stt[:, 2:3], bc[:, 0, :], op0=ALU.mult, op1=ALU.mult), reads=[bx, bst, bbc], writes=[btt])
                    ul, bul = ulr.next()
                    fw.op("pool", lambda e: e.tensor_tensor(ul, tt, bc[:, 1, :], op=ALU.add), reads=[btt, bbc], writes=[bul])
                    uls.append((ul, bul))
                return {"xs": xs, "uT": uT, "buT": buT, "uls": uls, "done": 0}

            def pro_pe(stn, upto_ti):
                uT, buT = stn["uT"], stn["buT"]
                while stn["done"] < min(upto_ti, 2):
                    ti = stn["done"]
                    ul, bul = stn["uls"][ti]
                    ps, bp = wr.next()
                    psb = ps.bitcast(BF16)
                    for kc in range(8):
                        fw.op("pe", lambda e: e.transpose(psb[:, kc * 128:(kc + 1) * 128], ul[:, kc * 128:(kc + 1) * 128], cmb[:, self.IDENT, :]),
                              reads=[bul, self.bcm], writes=[bp], signal=(kc == 7))
                    fw.op("act", lambda e: e.copy(uT[:, :, ti * 128:(ti + 1) * 128], psb.rearrange("p (k t) -> p k t", k=8)), reads=[bp], writes=[buT])
                    stn["done"] += 1

            def prologue(t0):
                stn = pro_elem(t0)
                pro_pe(stn, 2)
                return stn

            state = prologue(t0s[0])
            for idx, t0 in enumerate(t0s):
                xs, uT, buT = state["xs"], state["uT"], state["buT"]
                bc, bbc = cur["bc"]
                nxt = t0s[idx + 1] if idx + 1 < len(t0s) else None
                hoist = nxt is not None and stream_of(nxt) == cur["stream"]
                state = None
                accs = [accr.next() for _ in range(4)]
                pend = []

                def w1_group(j):
                    ps, bp = wr.next()
                    for kc in range(8):
                        fw.op("pe", lambda e: e.matmul(ps[:, 0:256], lhsT=W1[:, kc, j * 128:(j + 1) * 128], rhs=uT[:, kc, :], start=(kc == 0), stop=(kc == 7)),
                              reads=[bW1s[kc], buT], writes=[bp], signal=(kc == 7))
                    rl, brl = rlr.next()
                    fw.op("act", lambda e: e.activation(rl, ps[:, 0:256], AF.Relu), reads=[bp], writes=[brl])
                    hT, bh = hr.next()
                    fw.op("pool", lambda e: e.tensor_tensor(hT, rl, rl, op=ALU.mult), reads=[brl], writes=[bh])
                    pend.append((j, hT, bh))

                def w2_group():
                    j, hT, bh = pend.pop(0)
                    for ti in range(2):
                        for n in range(2):
                            acc, bacc = accs[ti * 2 + n]
                            fw.op("pe", lambda e: e.matmul(acc, lhsT=hT[:, ti * 128:(ti + 1) * 128], rhs=W2[:, j, n * 512:(n + 1) * 512], start=(j == 0), stop=(j == 31)),
                                  reads=[bh, bW2s[j // 4]], writes=[bacc], signal=(j == 31))

                for j in range(32):
                    w1_group(j)
                    if j == 2 and hoist:
                        state = pro_elem(nxt)
                    if j == 14 and hoist:
                        pro_pe(state, 1)
                    if j == 22 and hoist:
                        pro_pe(state, 2)
                    if len(pend) > 5:
                        w2_group()
                while pend:
                    w2_group()
                vs = []
                for ti in range(2):
                    v, bv = vr.next()
                    for n in range(2):
                        acc, bacc = accs[ti * 2 + n]
                        if n == 0:
                            fw.op("act", lambda e: e.copy(v[:, 0:512], acc), reads=[bacc], writes=[bv])
                        else:
                            fw.op("dve", lambda e: e.tensor_copy(v[:, 512:1024], acc), reads=[bacc], writes=[bv])
                    vs.append((v, bv))
                for ti in range(2):
                    r0 = (t0 + ti) * 128
                    xt, bx = xs[ti]
                    v, bv = vs[ti]
                    stt, bst = str_.next()
                    fw.op("act", lambda e: e.activation(sqj, v, AF.Square, accum_out=stt[:, 0:1]), reads=[bv], writes=[bsqj, bst])
                    self.rstd_col(stt[:, 0:1], bst, stt[:, 2:3], stt[:, 1:2], 1, 1.0 / D)
                    fw.op("dve", lambda e: e.scalar_tensor_tensor(v, v, stt[:, 2:3], bc[:, 2, :], op0=ALU.mult, op1=ALU.mult), reads=[bv, bst, bbc], writes=[bv])
                    fw.op("pool", lambda e: e.tensor_tensor(v, v, xt, op=ALU.add), reads=[bv, bx], writes=[bv])
                    dst = self.res1[r0:r0 + 128, :] if layer == 0 else self.out[r0 - 256:r0 - 128, :]
                    fw.dma("sp", dst, v, reads=[bv])
                if state is None and nxt is not None:
                    state = prologue(nxt)
            fw.barrier()

    def phase_mlstm(self):
        nc, fw, I, S, cm, cmb = self.nc, self.fw, self.I, self.S, self.cm, self.cmb
        S["xcT"] = self.dscr("xcT", [512, NT], BF16)
        S["qmT"] = self.dscr("qmT", [512, NT], BF16)
        S["kmT"] = self.dscr("kmT", [512, NT], BF16)
        S["vm"] = self.dscr("vm", [NT, 512], BF16)
        S["gl"] = self.dscr("gl", [NT, 32])
        with ExitStack() as st:
            xm = self.sb(st, "xm", [128, 4, NT], BF16); bxm = Buf()
            xc = self.sb(st, "xc", [128, 4, NT], BF16); bxc = [Buf() for _ in range(4)]
            fw.dma("sp", xm, S["xmT"].rearrange("(m p) t -> p m t", p=128), writes=[bxm])
            cw = self.sb(st, "cw", [128, 4, 4]); bcw = Buf()
            fw.dma("sp", cw, I["ml_conv"], writes=[bcw])
            BD = self.sb(st, "BD", [128, 3, 4, 128], BF16); bBD = Buf()
            fw.dma("pool", BD, I["ml_bd"].rearrange("t m k n -> k t m n"), writes=[bBD])
            wg = self.sb(st, "mwg", [128, 12, 32], BF16); bwg = Buf()
            fw.dma("pool", wg, I["ml_wg"].rearrange("c k n -> k c n"), writes=[bwg])
            bgb = self.sb(st, "bgb", [128, 32]); bbg = Buf()
            fw.dma("sp", bgb, I["ml_bg"].partition_broadcast(128), writes=[bbg])
            accr = self.ring(st, "cacc", [128, NT], F32, 2)
            segs = [(0, NCTX), (NCTX, NT)]
            for m in range(4):
                acc, bacc = accr.next()
                e1 = e2 = "dve"
                fw.op("dve", lambda e: e.tensor_scalar(acc, xm[:, m, :], cw[:, m, 1:2], cw[:, m, 3:4], op0=ALU.mult, op1=ALU.add), reads=[bxm, bcw], writes=[bacc])
                for (a, b) in segs:
                    fw.op(e1, lambda e: e.scalar_tensor_tensor(acc[:, a + 1:b], xm[:, m, a:b - 1], cw[:, m, 0:1], acc[:, a + 1:b], op0=ALU.mult, op1=ALU.add),
                          reads=[bxm, bcw, bacc], writes=[bacc])
                    fw.op(e2, lambda e: e.scalar_tensor_tensor(acc[:, a:b - 1], xm[:, m, a + 1:b], cw[:, m, 2:3], acc[:, a:b - 1], op0=ALU.mult, op1=ALU.add),
                          reads=[bxm, bcw, bacc], writes=[bacc])
                fw.op("act", lambda e: e.activation(xc[:, m, :], acc, AF.Silu), reads=[bacc], writes=[bxc[m]])
                fw.dma("sp", S["xcT"][m * 128:(m + 1) * 128, :], xc[:, m, :], reads=[bxc[m]])
            ginr = self.ring(st, "gin", [128, 12, 128], BF16, 2)
            vtr = self.ring(st, "vtk", [128, 512], BF16, 2)
            zall = self.sb(st, "zall", [128, NTILE, 32]); bz = Buf()
            for t in range(NTILE):
                r0 = t * 128
                gin, bgin = ginr.next()
                for ty in range(3):
                    src, bsrc = (xc, bxc) if ty < 2 else (xm, [bxm] * 4)
                    ps, bp = self.psr.next()
                    for m in range(4):
                        fw.op("pe", lambda e: e.matmul(ps[:, m * 128:(m + 1) * 128], lhsT=BD[:, ty, m, :], rhs=src[:, m, r0:r0 + 128], start=True, stop=True),
                              reads=[bBD, bsrc[m]], writes=[bp], signal=(m == 3))
                    fw.op("act" if ty != 1 else "dve", lambda e: (e.copy if ty != 1 else e.tensor_copy)(gin[:, ty * 4:(ty + 1) * 4, :], ps.rearrange("p (m t) -> p m t", m=4)),
                          reads=[bp], writes=[bgin])
                    if ty < 2:
                        dstT = S["qmT"] if ty == 0 else S["kmT"]
                        fw.dma("sp", dstT[:, r0:r0 + 128].rearrange("(m p) t -> p m t", p=128), gin[:, ty * 4:(ty + 1) * 4, :], reads=[bgin])
                ps, bp = self.psr.next()
                for m in range(4):
                    fw.op("pe", lambda e: e.matmul(ps[:, m * 128:(m + 1) * 128], lhsT=xm[:, m, r0:r0 + 128], rhs=BD[:, 2, m, :], start=True, stop=True),
                          reads=[bBD, bxm], writes=[bp], signal=(m == 3))
                vt, bvt = vtr.next()
                fw.op("dve", lambda e: e.tensor_copy(vt, ps), reads=[bp], writes=[bvt])
                fw.dma("sp", S["vm"][r0:r0 + 128, :], vt, reads=[bvt])
                ps, bp = self.psr.next()
                for ch in range(12):
                    fw.op("pe", lambda e: e.matmul(ps[:, 0:32], lhsT=gin[:, ch, :], rhs=wg[:, ch, :], start=(ch == 0), stop=(ch == 11)),
                          reads=[bgin, bwg], writes=[bp], signal=(ch == 11))
                fw.op("dve", lambda e: e.tensor_tensor(zall[:, t, :], ps[:, 0:32], bgb, op=ALU.add), reads=[bp, bbg], writes=[bz])
            glt = self.sb(st, "glt", [128, NTILE, 32]); bgl = Buf()
            ef = self.sb(st, "ef", [128, NTILE, 16]); bef = Buf()
            zv = zall.rearrange("p t (d k) -> p t d k", d=2)
            fw.op("act", lambda e: e.activation(ef.rearrange("p t (d h) -> p t d h", d=2), zv[:, :, :, 8:16], AF.Exp, scale=-1.0), reads=[bz], writes=[bef])
            fw.op("act", lambda e: e.activation(ef, ef, AF.Ln, bias=1.0), reads=[bef], writes=[bef])
            fw.op("dve", lambda e: e.tensor_scalar(glt[:, :, 0:16], ef, -1.0, None, op0=ALU.mult), reads=[bef], writes=[bgl])
            fw.op("pool", lambda e: e.tensor_copy(glt[:, :, 16:32].rearrange("p t (d h) -> p t d h", d=2), zv[:, :, :, 0:8]), reads=[bz], writes=[bgl])
            fw.dma("sp", S["gl"].rearrange("(c p) g -> p c g", p=128), glt, reads=[bgl])
            fw.barrier()
        self.phase_scan("ml")

    def phase_ret(self):
        self.phase_scan("ret")

    def phase_scan(self, kind):
        nc, fw, I, S, cm, cmb = self.nc, self.fw, self.I, self.S, self.cm, self.cmb
        ml = kind == "ml"
        Wd = 65 if ml else 64
        qsrc, ksrc, vsrc = (S["qmT"], S["kmT"], S["vm"]) if ml else (S["qDT"], S["kDT"], S["vD"])
        yT = S["yT1"]
        yv = (yT[0:512, :] if ml else yT[512:1024, :]).rearrange("(m p) t -> p m t", p=128)
        with ExitStack() as st:
            hacc = self.sb(st, "hacc", [128, NTILE, 512]); bh = [Buf() for _ in range(NTILE)]
            Cn = [self.sb(st, "Cn%d" % d, [128, 4, Wd]) for d in range(2)]; Cb = [self.sb(st, "Cb%d" % d, [128, 4, Wd], BF16) for d in range(2)]; bC = [Buf(), Buf()]
            ngb = self.sb(st, "ngb", [128, 64]); bng = Buf()
            qr = self.ring(st, "sq", [128, 4, 128], BF16, 8)
            kr = self.ring(st, "sk", [128, 4, 128], BF16, 8)
            vr = self.ring(st, "sv", [128, 512], BF16, 8)
            ktr = self.ring(st, "skt", [128, 512], BF16, 6)
            qbr = self.ring(st, "sqb", [128, 4, 2, 128], BF16, 6)
            for qa, qb_ in zip(qbr.aps, qbr.bufs):
                fw.op("pool", lambda e: e.memset(qa, 0.0), writes=[qb_])
            var = self.ring(st, "sva", [128, 8, Wd], BF16, 6)
            Amr = self.ring(st, "sAm", [128, 8, 128], BF16, 6)
            t32r = self.ring(st, "st32", [128, 512], F32, 3)
            ybr = self.ring(st, "syb", [128, 512], BF16, 2)
            yTr = self.ring(st, "syT", [128, 4, 128], BF16, 2)
            str_ = self.ring(st, "sst", [128, 24], F32, 2)
            gtr = self.ring(st, "sgt", [128, 40], F32, 8)
            if ml:
                fw.dma("sp", ngb, I["ml_ng"].partition_broadcast(128), writes=[bng])
                gl = self.sb(st, "gl", [128, NTILE, 32]); bgl = Buf()
                fw.dma("sp", gl, S["gl"].rearrange("(c p) g -> p c g", p=128), writes=[bgl])
                skip = self.sb(st, "skip", [128, 4]); bsk = Buf()
                fw.dma("sp", skip, I["ml_skip"], writes=[bsk])
                xcr = self.ring(st, "sxc", [128, 4, 128], BF16, 8)
                szr = self.ring(st, "ssz", [128, 4, 128], F32, 8)
                y32r = self.ring(st, "sy32", [128, 4, 128], F32, 2)
            else:
                gr = self.ring(st, "sg", [128, 512], F32, 8)
                rt = self.sb(st, "rt", [128, 16]); brt = Buf()
                jc = self.sb(st, "jc", [128, 2]); bjc = Buf()
                tabs = self.sb(st, "tabs", [128, 2, 24]); btab = Buf()
                fw.dma("sp", rt, I["ret_logit"].partition_broadcast(128), writes=[brt])
                fw.dma("sp", jc, I["jcol"], writes=[bjc])
                fw.op("act", lambda e: e.activation(rt, rt, AF.Exp, scale=-1.0), reads=[brt], writes=[brt])
                fw.op("act", lambda e: e.activation(rt, rt, AF.Ln, bias=1.0), reads=[brt], writes=[brt])
                Ft = self.sb(st, "Ft", [128, 16]); bF = Buf()
                for d in range(2):
                    fw.op("dve", lambda e: e.tensor_scalar(Ft[:, d * 8:(d + 1) * 8], rt[:, d * 8:(d + 1) * 8], jc[:, d:d + 1], None, op0=ALU.mult), reads=[brt, bjc], writes=[bF])
                    fw.op("act", lambda e: e.activation(tabs[:, d, 0:8], Ft[:, d * 8:(d + 1) * 8], AF.Exp, scale=1.0), reads=[bF], writes=[btab])
                    fw.op("act", lambda e: e.activation(tabs[:, d, 8:16], Ft[:, d * 8:(d + 1) * 8], AF.Exp, scale=-1.0), reads=[bF], writes=[btab])
                    fw.op("act", lambda e: e.activation(tabs[:, d, 16:24], rt[:, d * 8:(d + 1) * 8], AF.Exp, scale=-128.0), reads=[brt], writes=[btab])
            psr = self.psr
            orders = [list(range(NTILE)), [1, 0] + list(range(NTILE - 1, 1, -1))]
            pos = [{c: i for i, c in enumerate(o)} for o in orders]
            last_dir = {c: (1 if pos[1][c] >= pos[0][c] else 0) for c in range(NTILE)}
            for d in range(2):
                fw.op("dve", lambda e: e.memset(Cn[d], 0.0), writes=[bC[d]])
                fw.op("dve", lambda e: e.memset(Cb[d], 0.0), writes=[bC[d]])

            def make_dir(d):
                TRI = self.UINC if d == 0 else self.LINC

                def stageL(c):
                    r0 = c * 128
                    qT, bq = qr.next(); kT, bk = kr.next(); vt, bv = vr.next()
                    fw.dma("sp", qT, qsrc[:, r0:r0 + 128].rearrange("(m p) t -> p m t", p=128), writes=[bq])
                    fw.dma("sp", kT, ksrc[:, r0:r0 + 128].rearrange("(m p) t -> p m t", p=128), writes=[bk])
                    fw.dma("sp", vt, vsrc[r0:r0 + 128, :], writes=[bv])
                    xcc = bxcc = szc = bszc = gt = bg = None
                    if last_dir[c] == d:
                        if ml:
                            xcc, bxcc = xcr.next(); szc, bszc = szr.next()
                            fw.dma("sp", xcc, S["xcT"][:, r0:r0 + 128].rearrange("(m p) t -> p m t", p=128), writes=[bxcc])
                            fw.dma("sp", szc, S["szT"][:, r0:r0 + 128].rearrange("(m p) t -> p m t", p=128), writes=[bszc])
                        else:
                            gt, bg = gr.next()
                            fw.dma("sp", gt, S["gD"][r0:r0 + 128, :], writes=[bg])
                    return locals()

                def stageA(c, L):
                    r0, qT, bq, kT, bk, vt, bv = (L[k] for k in ("r0", "qT", "bq", "kT", "bk", "vt", "bv"))
                    xcc, bxcc, szc, bszc, gt, bg = (L[k] for k in ("xcc", "bxcc", "szc", "bszc", "gt", "bg"))
                    if ml:
                        gtt, bgt = gtr.next()
                        pg, bpg = psr.next()
                        lfv = gl[:, c, d * 8:(d + 1) * 8]
                        fw.op("pe", lambda e: e.matmul(pg[:, 0:8], lhsT=cm[:, TRI, :], rhs=lfv, start=True, stop=True), reads=[bgl, self.bcm], writes=[bpg], signal=False)
                        fw.op("pe", lambda e: e.matmul(pg[:, 8:16], lhsT=cm[:, self.ONES, :], rhs=lfv, start=True, stop=True), reads=[bgl, self.bcm], writes=[bpg])
                        fw.op("dve", lambda e: e.tensor_tensor(gtt[:, 24:32], gl[:, c, 16 + d * 8:16 + (d + 1) * 8], pg[:, 0:8], op=ALU.subtract), reads=[bgl, bpg], writes=[bgt])
                        fw.op("act", lambda e: e.activation(gtt[:, 0:8], gtt[:, 24:32], AF.Exp), reads=[bgt], writes=[bgt])
                        fw.op("act", lambda e: e.activation(gtt[:, 8:16], pg[:, 0:8], AF.Exp, bias=-math.log(8.0)), reads=[bpg], writes=[bgt])
                        fw.op("act", lambda e: e.activation(gtt[:, 16:24], pg[:, 8:16], AF.Exp), reads=[bpg], writes=[bgt])
                        av, cv, dv = gtt[:, 0:8], gtt[:, 8:16], gtt[:, 16:24]
                    else:
                        gtt, bgt = tabs, btab
                        av, cv, dv = tabs[:, d, 0:8], tabs[:, d, 8:16], tabs[:, d, 16:24]
                    ps, bp = psr.next()
                    psb = ps.bitcast(BF16)
                    for m in range(4):
                        fw.op("pe", lambda e: e.transpose(psb[:, m * 128:(m + 1) * 128], kT[:, m, :], cmb[:, self.IDENT, :]), reads=[bk, self.bcm], writes=[bp], signal=(m == 3))
                    kt, bkt = ktr.next()
                    fw.op("act", lambda e: e.copy(kt, psb[:, 0:512]), reads=[bp], writes=[bkt])
                    qb, bqb = qbr.next()
                    for hh in range(2):
                        sl = slice(hh * 64, (hh + 1) * 64)
                        fw.op("act", lambda e: e.copy(qb[sl, :, hh, :], qT[sl, :, :]), reads=[bq], writes=[bqb])
                    va, bva = var.next()
                    fw.op("dve", lambda e: e.tensor_tensor(va[:, :, 0:64], vt.rearrange("p (h d) -> p h d", d=64), av.unsqueeze(2).to_broadcast([128, 8, 64]), op=ALU.mult),
                          reads=[bv, bgt], writes=[bva])
                    if ml:
                        fw.op("pool", lambda e: e.tensor_copy(va[:, :, 64:65], av.unsqueeze(2)), reads=[bgt], writes=[bva])
                    Am, bAm = Amr.next()
                    for b2 in range(2):
                        pa, bpa = psr.next()
                        for mm in range(2):
                            m = b2 * 2 + mm
                            fw.op("pe", lambda e: e.matmul(pa[:, mm * 256:(mm + 1) * 256], lhsT=kT[:, m, :], rhs=qb[:, m, :, :].rearrange("p h t -> p (h t)"), start=True, stop=True),
                                  reads=[bk, bqb], writes=[bpa], signal=(mm == 1))
                        fw.op("dve", lambda e: e.tensor_tensor(Am[:, b2 * 4:(b2 + 1) * 4, :], pa.rearrange("p (h t) -> p h t", h=4),
                                                                cm[:, TRI, :].unsqueeze(1).to_broadcast([128, 4, 128]), op=ALU.mult), reads=[bpa, self.bcm], writes=[bAm])
                    return locals()

                def stageB(c, L):
                    r0, qb, bqb, va, bva, Am, bAm, kt, bkt, gtt, bgt, av, cv, dv = (L[k] for k in ("r0", "qb", "bqb", "va", "bva", "Am", "bAm", "kt", "bkt", "gtt", "bgt", "av", "cv", "dv"))
                    xcc, bxcc, szc, bszc, gt, bg = (L[k] for k in ("xcc", "bxcc", "szc", "bszc", "gt", "bg"))
                    first = last_dir[c] != d
                    pos_ = []
                    for b2 in range(2):
                        po, bpo = psr.next()
                        for hq in range(4):
                            h = b2 * 4 + hq
                            m, hh = h // 2, h % 2
                            fw.op("pe", lambda e: e.matmul(po[:, hq * Wd:(hq + 1) * Wd], lhsT=Am[:, h, :], rhs=va[:, h, :], start=True, stop=False),
                                  reads=[bAm, bva], writes=[bpo], signal=False)
                            fw.op("pe", lambda e: e.matmul(po[:, hq * Wd:(hq + 1) * Wd], lhsT=qb[:, m, hh, :], rhs=Cb[d][:, m, :], start=False, stop=True),
                                  reads=[bqb, bC[d]], writes=[bpo], signal=(hq == 3))
                        pos_.append((po, bpo))
                    hdst = hacc[:, c, :]
                    tmpo, btmp = (hdst, bh[c]) if first else t32r.next()
                    if ml:
                        for b2 in range(2):
                            po, bpo = pos_[b2]
                            pv = po[:, 0:4 * Wd].rearrange("p (h w) -> p h w", w=Wd)
                            fw.op("dve", lambda e: e.tensor_tensor(gtt[:, 24 + b2 * 4:28 + b2 * 4].unsqueeze(2), pv[:, :, 64:65], cv[:, b2 * 4:(b2 + 1) * 4].unsqueeze(2), op=ALU.mult),
                                  reads=[bpo, bgt], writes=[bgt])
                        fw.op("dve", lambda e: e.scalar_tensor_tensor(gtt[:, 32:40], gtt[:, 24:32], -1.0, gtt[:, 24:32], op0=ALU.mult, op1=ALU.max), reads=[bgt], writes=[bgt])
                        fw.op("dve", lambda e: e.tensor_scalar(gtt[:, 24:32], gtt[:, 32:40], 1.0, None, op0=ALU.max), reads=[bgt], writes=[bgt])
                        fw.op("dve", lambda e: e.reciprocal(gtt[:, 32:40], gtt[:, 24:32]), reads=[bgt], writes=[bgt])
                        fw.op("dve", lambda e: e.tensor_tensor(gtt[:, 32:40], gtt[:, 32:40], cv, op=ALU.mult), reads=[bgt], writes=[bgt])
                        scv = gtt[:, 32:40]
                    else:
                        scv = cv
                    for b2 in range(2):
                        po, bpo = pos_[b2]
                        pv = po[:, 0:4 * Wd].rearrange("p (h w) -> p h w", w=Wd)
                        fw.op("dve", lambda e: e.tensor_tensor(tmpo[:, b2 * 256:(b2 + 1) * 256].rearrange("p (h d) -> p h d", d=64), pv[:, :, 0:64],
                                                                scv[:, b2 * 4:(b2 + 1) * 4].unsqueeze(2).to_broadcast([128, 4, 64]), op=ALU.mult),
                              reads=[bpo, bgt], writes=[btmp])
                    if not first:
                        fw.op("pool", lambda e: e.tensor_tensor(hdst, hdst, tmpo, op=ALU.add), reads=[btmp, bh[c]], writes=[bh[c]])
                    for b2 in range(2):
                        pu, bpu = psr.next()
                        for mm in range(2):
                            m = b2 * 2 + mm
                            fw.op("pe", lambda e: e.matmul(pu[:, mm * 2 * Wd:(mm + 1) * 2 * Wd], lhsT=kt[:, m * 128:(m + 1) * 128], rhs=va[:, 2 * m:2 * m + 2, :].rearrange("p h w -> p (h w)"), start=True, stop=True),
                                  reads=[bkt, bva], writes=[bpu], signal=(mm == 1))
                        puv = pu[:, 0:4 * Wd].rearrange("p (m h w) -> p m h w", m=2, h=2)
                        for hh in range(2):
                            sl = slice(hh * 64, (hh + 1) * 64)
                            fw.op("dve", lambda e: e.tensor_tensor(Cn[d][sl, b2 * 2:b2 * 2 + 2, :], Cn[d][sl, b2 * 2:b2 * 2 + 2, :], puv[sl, :, hh, :], op=ALU.add), reads=[bpu, bC[d]], writes=[bC[d]])
                    for hh in range(2):
                        sl = slice(hh * 64, (hh + 1) * 64)
                        dvv = dv[sl, hh:8:2].unsqueeze(2).to_broadcast([64, 4, Wd])
                        fw.op("pool", lambda e: e.tensor_tensor(Cn[d][sl, :, :], Cn[d][sl, :, :], dvv, op=ALU.mult), reads=[bC[d], bgt], writes=[bC[d]])
                    fw.op("act", lambda e: e.copy(Cb[d], Cn[d]), reads=[bC[d]], writes=[bC[d]])
                    if not first:
                        if ml:
                            def post(psb_, bp_, dst):
                                y32, by32 = y32r.next()
                                for m in range(4):
                                    fw.op("dve", lambda e: e.scalar_tensor_tensor(y32[:, m, :], xcc[:, m, :], skip[:, m:m + 1], psb_[:, m * 128:(m + 1) * 128], op0=ALU.mult, op1=ALU.add),
                                          reads=[bp_, bxcc, bsk], writes=[by32])
                                yTt, byT = yTr.next()
                                fw.op("pool", lambda e: e.tensor_tensor(yTt, y32, szc, op=ALU.mult), reads=[by32, bszc], writes=[byT])
                                fw.dma("sp", dst, yTt, reads=[byT])
                            self.finalize_tok(hdst, bh[c], ngb.unsqueeze(1).to_broadcast([128, 8, 64]), bng, t32r, str_, ybr, yTr, yv[:, :, r0:r0 + 128], post=post)
                        else:
                            self.finalize_tok(hdst, bh[c], gt, bg, t32r, str_, ybr, yTr, yv[:, :, r0:r0 + 128])

                return stageL, stageA, stageB

            self.scan_driver(orders, [make_dir(0), make_dir(1)])
            fw.barrier()

    IDENT, UINC, LINC, USTR, LSTR, ONES, BLK, BLK32 = range(8)

    def phase_ada(self):
        nc, fw, I = self.nc, self.fw, self.I
        self.modv = self.dscr("modv", [2, 2, 6, D])
        with ExitStack() as st:
            cc = self.sb(st, "cc", [128, 8, 2]); bcc = Buf()
            sc = self.sb(st, "sc", [128, 8, 2]); bsc = Buf()
            fw.dma("sp", cc, I["ccols"], writes=[bcc])
            fw.op("act", lambda e: e.activation(sc, cc, AF.Silu), reads=[bcc], writes=[bsc])
            wr = self.ring(st, "adw", [128, 8, 512], F32, 5)
            mod = self.sb(st, "mod", [2, 6 * D]); bmod = Buf()
            ab = self.sb(st, "adb", [2, 6 * D]); bab = Buf()
            ng = self.sb(st, "ng", [2, 4 * D]); bng = Buf()
            der = self.sb(st, "der", [2, 6 * D]); bder = Buf()
            for layer in range(2):
                fw.dma("sp", ab, I["ada_b"][layer:layer + 1, :].partition_broadcast(2), writes=[bab])
                fw.dma("sp", ng, I["norm_g"][layer:layer + 1].rearrange("o j d -> o (j d)").partition_broadcast(2), writes=[bng])
                wv = I["ada_w"][layer].rearrange("(kc p) n -> p kc n", p=128)
                for nb in range(12):
                    wt, bw = wr.next()
                    fw.dma("sp", wt, wv[:, :, nb * 512:(nb + 1) * 512], writes=[bw])
                    ps, bp = self.psr.next()
                    for kc in range(8):
                        fw.op("pe", lambda e: e.matmul(ps[0:2, :], lhsT=sc[:, kc, :], rhs=wt[:, kc, :], start=(kc == 0), stop=(kc == 7)),
                              reads=[bsc, bw], writes=[bp], signal=(kc == 7))
                    fw.op("dve", lambda e: e.tensor_tensor(mod[:, nb * 512:(nb + 1) * 512], ps[0:2, :], ab[:, nb * 512:(nb + 1) * 512], op=ALU.add),
                          reads=[bp, bab], writes=[bmod])
                m = lambda j: mod[:, j * D:(j + 1) * D]
                g = lambda j: ng[:, j * D:(j + 1) * D]
                dd = lambda j: der[:, j * D:(j + 1) * D]
                rw = dict(reads=[bmod, bng], writes=[bder])
                fw.op("dve", lambda e: e.scalar_tensor_tensor(dd(0), m(1), 1.0, g(0), op0=ALU.add, op1=ALU.mult), **rw)
                fw.op("dve", lambda e: e.tensor_copy(dd(1), m(0)), **rw)
                fw.op("dve", lambda e: e.tensor_tensor(dd(2), m(2), g(1), op=ALU.mult), **rw)
                fw.op("dve", lambda e: e.scalar_tensor_tensor(dd(3), m(4), 1.0, g(2), op0=ALU.add, op1=ALU.mult), **rw)
                fw.op("dve", lambda e: e.tensor_copy(dd(4), m(3)), **rw)
                fw.op("dve", lambda e: e.tensor_tensor(dd(5), m(5), g(3), op=ALU.mult), **rw)
                fw.dma("sp", self.modv[layer].rearrange("s j d -> s (j d)"), der, reads=[bder])
            fw.barrier()

    def load_bc(self, st, name, layer, stream, js):
        key = (id(st), name[:-1])
        if not hasattr(self, "_bc"):
            self._bc = {}
        if key not in self._bc:
            self._bc[key] = (self.sb(st, name, [128, len(js), D]), Buf())
        t, b = self._bc[key]
        for i, j in enumerate(js):
            self.fw.dma("sp", t[:, i, :], self.modv[layer, stream, j:j + 1, :].partition_broadcast(128), writes=[b])
        return t, b

    def rstd_col(self, ss, bss, out, tmp, n, scale):
        fw = self.fw
        fw.op("dve", lambda e: e.tensor_scalar(tmp, ss, scale, EPS, op0=ALU.mult, op1=ALU.add), reads=[bss], writes=[bss])
        fw.op("act", lambda e: e.activation(tmp, tmp, AF.Ln), reads=[bss], writes=[bss])
        fw.op("act", lambda e: e.activation(out, tmp, AF.Exp, scale=-0.5), reads=[bss], writes=[bss])

    def phase_inproj(self, layer):
        nc, fw, I = self.nc, self.fw, self.I
        cmb = self.cmb
        WC = W0C if layer == 0 else W1C
        wsrc = I["w_in0"] if layer == 0 else I["w_in1"]
        src_res = None if layer == 0 else self.res1
        S = {}
        if layer == 0:
            S["qAT"] = self.dscr("qAT", [256, NT]); S["kAT"] = self.dscr("kAT", [256, NT])
            S["kA"] = self.dscr("kA", [NT, 256]); S["vA"] = self.dscr("vA", [NT, 512], BF16)
            S["gA"] = self.dscr("gA", [NT, 512]); S["lgp"] = self.dscr("lgp", [NT, 512])
            S["qBT"] = self.dscr("qBT", [512, NT], BF16); S["kBT"] = self.dscr("kBT", [128, NT], BF16)
            S["vB"] = self.dscr("vB", [NT, 128], BF16)
        else:
            S["xmT"] = self.dscr("xmT", [512, NT], BF16); S["szT"] = self.dscr("szT", [512, NT])
            S["qDT"] = self.dscr("qDT", [512, NT], BF16); S["kDT"] = self.dscr("kDT", [512, NT], BF16)
            S["vD"] = self.dscr("vD", [NT, 512], BF16); S["gD"] = self.dscr("gD", [NT, 512])
        self.S.update(S)
        with ExitStack() as st:
            W = self.sb(st, "Win", [128, 8, WC], BF16); bWs = [Buf() for _ in range(8)]
            wv = wsrc.rearrange("(kc p) n -> p kc n", p=128)
            for kc in range(8):
                fw.dma("pool", W[:, kc, :], wv[:, kc, :], writes=[bWs[kc]])
            rope = self.sb(st, "rope", [128, 2, NT]); brope = Buf()
            fw.dma("sp", rope, I["rope"].rearrange("n p f -> p n f"), writes=[brope])
            small = self.sb(st, "small", [128, 16]); bsmall = Buf()
            ngbc = self.sb(st, "ngbc", [128, 64]); bng = Buf()
            if layer == 0:
                fw.dma("sp", small[:, 0:4], I["att_g"], writes=[bsmall])
                fw.dma("sp", ngbc, I["gla_ng"].partition_broadcast(128), writes=[bng])
                wg = self.sb(st, "wg", [17, 2, 256]); bwg = Buf()
                fw.dma("sp", wg, I["gla_wg"].rearrange("d k n -> k d n"), writes=[bwg])
                lra = [self.sb(st, "lra%d" % d, [32, 512]) for d in range(2)]
                blra = [Buf(), Buf()]
                for d in range(2):
                    fw.op("pool", lambda e: e.memset(lra[d], 1.0), writes=[blra[d]])
            else:
                fw.dma("sp", ngbc, I["ret_ng"].partition_broadcast(128), writes=[bng])
            xr = self.ring(st, "xt", [128, D], F32, 2)
            sqj = self.sb(st, "sqj", [128, D]); bsqj = Buf()
            xnr = self.ring(st, "xn", [128, D], F32, 2)
            ulr = self.ring(st, "ul", [128, D], BF16, 5)
            ulTr = self.ring(st, "ulT", [128, 8, 512], BF16, 2)
            str_ = self.ring(st, "stat", [128, 4], F32, 4)
            f32r = self.ring(st, "ev32", [128, 512], F32, 4)
            f32r2 = self.ring(st, "ev32b", [128, 512], F32, 4)
            bfr = self.ring(st, "evbf", [128, 512], BF16, 4)
            supers = [(0, 2)] + [(2 + 4 * i, 4) for i in range(8)]
            cur = {"stream": None, "bc": None}

            def pro_elem(t0, ntl):
                stream = 1 if t0 < 2 else 0
                if stream != cur["stream"]:
                    cur["bc"] = self.load_bc(st, "bc%d" % stream, layer, stream, [0, 1])
                    cur["stream"] = stream
                bc, bbc = cur["bc"]
                ulT, bulT = ulTr.next()
                uls = []
                for ti in range(ntl):
                    t = t0 + ti
                    xt, bx = xr.next()
                    if layer == 0:
                        srcap = I["ctx"][t * 128:(t + 1) * 128, :] if t < 2 else I["x"][(t - 2) * 128:(t - 1) * 128, :]
                    else:
                        srcap = src_res[t * 128:(t + 1) * 128, :]
                    fw.dma("sp", xt, srcap, writes=[bx])
                    stt, bst = str_.next()
                    fw.op("act", lambda e: e.activation(sqj, xt, AF.Square, accum_out=stt[:, 0:1]), reads=[bx], writes=[bsqj, bst])
                    self.rstd_col(stt[:, 0:1], bst, stt[:, 2:3], stt[:, 1:2], 1, 1.0 / D)
                    xn, bxn = xnr.next()
                    fw.op("dve", lambda e: e.scalar_tensor_tensor(xn, xt, stt[:, 2:3], bc[:, 0, :], op0=ALU.mult, op1=ALU.mult),
                          reads=[bx, bst, bbc], writes=[bxn])
                    ul, bul = ulr.next()
                    fw.op("pool", lambda e: e.tensor_tensor(ul, xn, bc[:, 1, :], op=ALU.add), reads=[bxn, bbc], writes=[bul])
                    uls.append((ul, bul))
                return {"ulT": ulT, "bulT": bulT, "uls": uls, "ntl": ntl, "done": 0}

            def pro_pe(stn, upto_ti):
                ulT, bulT = stn["ulT"], stn["bulT"]
                while stn["done"] < min(upto_ti, stn["ntl"]):
                    ti = stn["done"]
                    ul, bul = stn["uls"][ti]
                    ps, bp = self.psr.next()
                    psb = ps.bitcast(BF16)
                    for kc in range(8):
                        fw.op("pe", lambda e: e.transpose(psb[:, kc * 128:(kc + 1) * 128], ul[:, kc * 128:(kc + 1) * 128], cmb[:, self.IDENT, :]),
                              reads=[bul, self.bcm], writes=[bp], signal=(kc == 7))
                    fw.op("act", lambda e: e.copy(ulT[:, :, ti * 128:(ti + 1) * 128], psb.rearrange("p (k t) -> p k t", k=8)),
                          reads=[bp], writes=[bulT])
                    stn["done"] += 1

            nstate = pro_elem(*supers[0])
            pro_pe(nstate, 99)
            for si, (t0, ntl) in enumerate(supers):
                ulT, bulT = nstate["ulT"], nstate["bulT"]
                nstate = None
                N = ntl * 128
                c0 = t0 * 128

                def fm(col0, M):
                    ps, bp = self.psr.next()
                    for kc in range(8):
                        fw.op("pe", lambda e: e.matmul(ps[0:M, 0:N], lhsT=W[:, kc, col0:col0 + M], rhs=ulT[:, kc, 0:N], start=(kc == 0), stop=(kc == 7)),
                              reads=[bWs[kc], bulT], writes=[bp], signal=(kc == 7))
                    return ps, bp

                def tm(ti, col0, ncol):
                    ps, bp = self.psr.next()
                    for kc in range(8):
                        fw.op("pe", lambda e: e.matmul(ps[:, 0:ncol], lhsT=ulT[:, kc, ti * 128:(ti + 1) * 128], rhs=W[:, kc, col0:col0 + ncol], start=(kc == 0), stop=(kc == 7)),
                              reads=[bWs[kc], bulT], writes=[bp], signal=(kc == 7))
                    return ps, bp

                def rope_fm(col0, colp, gi, dst, scale, norm):
                    ps, bp = fm(col0, 128)
                    pp, bpp = fm(colp, 128)
                    t1, b1 = f32r.next()
                    t2, b2 = f32r2.next()
                    ob, bo = bfr.next()
                    cosv = rope[:, 0, c0:c0 + N]
                    sinv = rope[:, 1, c0:c0 + N]
                    if norm:
                        sq, bsq = bfr.next()
                        fw.op("act", lambda e: e.activation(sq[:, 0:N], ps[:, 0:N], AF.Square), reads=[bp], writes=[bsq])
                        ps2, bp2 = self.psr.next()
                        fw.op("pe", lambda e: e.matmul(ps2[:, 0:N], lhsT=cmb[:, self.BLK, :], rhs=sq[:, 0:N], start=True, stop=True),
                              reads=[bsq, self.bcm], writes=[bp2])
                        rs, brs = f32r.next()
                        fw.op("dve", lambda e: e.tensor_scalar(rs[:, 0:N], ps2[:, 0:N], 1.0 / 64, EPS, op0=ALU.mult, op1=ALU.add), reads=[bp2], writes=[brs])
                        fw.op("act", lambda e: e.activation(rs[:, 0:N], rs[:, 0:N], AF.Ln), reads=[brs], writes=[brs])
                        fw.op("act", lambda e: e.activation(rs[:, 0:N], rs[:, 0:N], AF.Exp, scale=-0.5), reads=[brs], writes=[brs])
                        fw.op("dve", lambda e: e.scalar_tensor_tensor(t1[:, 0:N], ps[:, 0:N], small[:, gi:gi + 1], cosv, op0=ALU.mult, op1=ALU.mult),
                              reads=[bp, bsmall, brope], writes=[b1])
                        fw.op("dve", lambda e: e.scalar_tensor_tensor(t2[:, 0:N], pp[:, 0:N], small[:, gi + 1:gi + 2], sinv, op0=ALU.mult, op1=ALU.mult),
                              reads=[bpp, bsmall, brope], writes=[b2])
                        fw.op("pool", lambda e: e.tensor_tensor(t1[:, 0:N], t1[:, 0:N], t2[:, 0:N], op=ALU.add), reads=[b1, b2], writes=[b1])
                        fw.op("pool", lambda e: e.tensor_tensor(ob[:, 0:N], t1[:, 0:N], rs[:, 0:N], op=ALU.mult), reads=[b1, brs], writes=[bo])
                    else:
                        fw.op("dve", lambda e: e.scalar_tensor_tensor(t1[:, 0:N], ps[:, 0:N], scale, cosv, op0=ALU.mult, op1=ALU.mult), reads=[bp, brope], writes=[b1])
                        fw.op("dve", lambda e: e.scalar_tensor_tensor(t2[:, 0:N], pp[:, 0:N], scale, sinv, op0=ALU.mult, op1=ALU.mult), reads=[bpp, brope], writes=[b2])
                        fw.op("pool", lambda e: e.tensor_tensor(ob[:, 0:N], t1[:, 0:N], t2[:, 0:N], op=ALU.add), reads=[b1, b2], writes=[bo])
                    fw.dma("sp", dst, ob[:, 0:N], reads=[bo])

                if layer == 0:
                    for j in range(2):
                        ps, bp = fm(0 + j * 128, 128)
                        t1, b1 = f32r.next()
                        fw.op("act", lambda e: e.activation(t1[:, 0:N], ps[:, 0:N], AF.Copy, scale=32 ** -0.5), reads=[bp], writes=[b1])
                        fw.dma("sp", S["qAT"][j * 128:(j + 1) * 128, c0:c0 + N], t1[:, 0:N], reads=[b1])
                        ps, bp = fm(256 + j * 128, 128)
                        t1, b1 = f32r.next()
                        fw.op("dve", lambda e: e.tensor_copy(t1[:, 0:N], ps[:, 0:N]), reads=[bp], writes=[b1])
                        fw.dma("sp", S["kAT"][j * 128:(j + 1) * 128, c0:c0 + N], t1[:, 0:N], reads=[b1])
                    for d in range(2):
                        ps, bp = fm(1536 + 16 * d, 16)
                        fw.op("dve", lambda e: e.tensor_copy(lra[d][0:16, 0:N], ps[0:16, 0:N]), reads=[bp], writes=[blra[d]])
                    for m in range(4):
                        rope_fm(1568 + m * 128, 2336 + m * 128, 0, S["qBT"][m * 128:(m + 1) * 128, c0:c0 + N], 1.0, True)
                    rope_fm(2080, 2336 + 512, 2, S["kBT"][:, c0:c0 + N], 1.0, True)
                    if si + 1 < len(supers):
                        nstate = pro_elem(*supers[si + 1])
                    for ti in range(ntl):
                        r0 = c0 + ti * 128
                        ps, bp = tm(ti, 256, 256)
                        t1, b1 = f32r.next()
                        fw.op("act", lambda e: e.copy(t1[:, 0:256], ps[:, 0:256]), reads=[bp], writes=[b1])
                        fw.dma("sp", S["kA"][r0:r0 + 128, :], t1[:, 0:256], reads=[b1])
                        ps, bp = tm(ti, 2208, 128)
                        ob, bo = bfr.next()
                        fw.op("dve", lambda e: e.tensor_copy(ob[:, 0:128], ps[:, 0:128]), reads=[bp], writes=[bo])
                        fw.dma("sp", S["vB"][r0:r0 + 128, :], ob[:, 0:128], reads=[bo])
                        ps, bp = tm(ti, 512, 512)
                        ob, bo = bfr.next()
                        fw.op("act", lambda e: e.copy(ob, ps), reads=[bp], writes=[bo])
                        fw.dma("sp", S["vA"][r0:r0 + 128, :], ob, reads=[bo])
                        ps, bp = tm(ti, 1024, 512)
                        t1, b1 = f32r.next()
                        fw.op("act", lambda e: e.activation(t1, ps, AF.Silu), reads=[bp], writes=[b1])
                        t2, b2 = f32r2.next()
                        fw.op("pool", lambda e: e.tensor_tensor(t2.rearrange("p (h d) -> p h d", d=64), t1.rearrange("p (h d) -> p h d", d=64),
                                                                 ngbc.unsqueeze(1).to_broadcast([128, 8, 64]), op=ALU.mult), reads=[b1, bng], writes=[b2])
                        fw.dma("sp", S["gA"][r0:r0 + 128, :], t2, reads=[b2])
                        ps, bp = self.psr.next()
                        for d in range(2):
                            fw.op("pe", lambda e: e.matmul(ps[:, d * 256:(d + 1) * 256], lhsT=lra[d][0:17, ti * 128:(ti + 1) * 128], rhs=wg[:, d, :], start=True, stop=True),
                                  reads=[blra[d], bwg], writes=[bp], signal=(d == 1))
                        t1, b1 = f32r.next()
                        fw.op("act", lambda e: e.activation(t1, ps, AF.Exp, scale=-1.0), reads=[bp], writes=[b1])
                        t2, b2 = f32r2.next()
                        fw.op("act", lambda e: e.activation(t2, t1, AF.Ln, bias=1.0), reads=[b1], writes=[b2])
                        fw.dma("sp", S["lgp"][r0:r0 + 128, :], t2, reads=[b2])
                        if nstate is not None:
                            pro_pe(nstate, ti + 1)
                    if nstate is not None:
                        pro_pe(nstate, 99)
                else:
                    for j in range(4):
                        ps, bp = fm(0 + j * 128, 128)
                        ob, bo = bfr.next()
                        fw.op("act", lambda e: e.copy(ob[:, 0:N], ps[:, 0:N]), reads=[bp], writes=[bo])
                        fw.dma("sp", S["xmT"][j * 128:(j + 1) * 128, c0:c0 + N], ob[:, 0:N], reads=[bo])
                        ps, bp = fm(512 + j * 128, 128)
                        t1, b1 = f32r.next()
                        fw.op("act", lambda e: e.activation(t1[:, 0:N], ps[:, 0:N], AF.Silu), reads=[bp], writes=[b1])
                        fw.dma("sp", S["szT"][j * 128:(j + 1) * 128, c0:c0 + N], t1[:, 0:N], reads=[b1])
                        rope_fm(1024 + j * 128, 3072 + j * 128, 0, S["qDT"][j * 128:(j + 1) * 128, c0:c0 + N], 0.125, False)
                        rope_fm(1536 + j * 128, 3072 + 512 + j * 128, 0, S["kDT"][j * 128:(j + 1) * 128, c0:c0 + N], 1.0, False)
                    if si + 1 < len(supers):
                        nstate = pro_elem(*supers[si + 1])
                    for ti in range(ntl):
                        r0 = c0 + ti * 128
                        ps, bp = tm(ti, 2048, 512)
                        ob, bo = bfr.next()
                        fw.op("act", lambda e: e.copy(ob, ps), reads=[bp], writes=[bo])
                        fw.dma("sp", S["vD"][r0:r0 + 128, :], ob, reads=[bo])
                        ps, bp = tm(ti, 2560, 512)
                        t1, b1 = f32r.next()
                        fw.op("act", lambda e: e.activation(t1, ps, AF.Silu), reads=[bp], writes=[b1])
                        t2, b2 = f32r2.next()
                        fw.op("pool", lambda e: e.tensor_tensor(t2.rearrange("p (h d) -> p h d", d=64), t1.rearrange("p (h d) -> p h d", d=64),
                                                                 ngbc.unsqueeze(1).to_broadcast([128, 8, 64]), op=ALU.mult), reads=[b1, bng], writes=[b2])
                        fw.dma("sp", S["gD"][r0:r0 + 128, :], t2, reads=[b2])
                        if nstate is not None:
                            pro_pe(nstate, ti + 1)
                    if nstate is not None:
                        pro_pe(nstate, 99)
            fw.barrier()


def _perm64():
    p = np.arange(64)
    return np.concatenate([p[16:32], p[0:16], p[48:64], p[32:48]])


def _rope_tables():
    axis_dim = 32
    inv = 10000.0 ** (-np.arange(0, axis_dim, 2, dtype=np.float32) / axis_dim)
    t = np.arange(NLAT)
    ang_r = (t // 64).astype(np.float32)[:, None] * inv[None, :]
    ang_c = (t % 64).astype(np.float32)[:, None] * inv[None, :]
    cos = np.concatenate([np.cos(ang_r), np.cos(ang_r), np.cos(ang_c), np.cos(ang_c)], axis=1)
    sin = np.concatenate([-np.sin(ang_r), np.sin(ang_r), -np.sin(ang_c), np.sin(ang_c)], axis=1)
    tab = np.zeros((2, 128, NT), np.float32)
    tab[0, :, :NCTX] = 1.0
    for hh in range(2):
        tab[0, hh * 64:(hh + 1) * 64, NCTX:] = cos.T
        tab[1, hh * 64:(hh + 1) * 64, NCTX:] = sin.T
    return tab


def _cmat():
    j = np.arange(128)[:, None]
    i = np.arange(128)[None, :]
    m = np.zeros((8, 128, 128), np.float32)
    m[0] = (j == i)
    m[1] = (j <= i)
    m[2] = (j >= i)
    m[3] = (j < i)
    m[4] = (j > i)
    m[5] = 1.0
    m[6] = ((j // 64) == (i // 64))
    m[7] = ((j // 32) == (i // 32))
    return m


def host_inputs(inp):
    f = lambda a: np.ascontiguousarray(np.asarray(a, dtype=np.float32))
    perm = _perm64()
    ab = f(inp["ab_w_in"][0])
    qB = ab[:, 1568:2080].reshape(D, 8, 64)[:, :, perm].reshape(D, 512)
    kB = ab[:, 2080:2208].reshape(D, 2, 64)[:, :, perm].reshape(D, 128)
    w_in0 = np.concatenate([ab, qB, kB], axis=1)
    cd = f(inp["cd_w_in"][0])
    qD = cd[:, 1024:1536].reshape(D, 8, 64)[:, :, perm].reshape(D, 512)
    kD = cd[:, 1536:2048].reshape(D, 8, 64)[:, :, perm].reshape(D, 512)
    w_in1 = np.concatenate([cd, qD, kD], axis=1)
    gla_wg = np.concatenate([f(inp["gla_w_gate"][0]), f(inp["gla_b_gate"][0])[:, None, :]], axis=1)
    aq = f(inp["att_qk_norm_g"][0])
    att_g = np.stack([np.tile(aq[0], 2), np.tile(aq[0][perm], 2), np.tile(aq[1], 2), np.tile(aq[1][perm], 2)], axis=1)
    cw = f(inp["ml_conv_w"][0])
    cb = f(inp["ml_conv_b"][0])
    ml_conv = np.stack([cw[0], cw[1], cw[2], cb], axis=1).reshape(4, 128, 4).transpose(1, 0, 2)
    wq = f(inp["ml_w_qkv"][0])
    ml_bd = np.zeros((3, 4, 128, 128), np.float32)
    for t in range(3):
        for h in range(8):
            m, hh = h // 2, h % 2
            ml_bd[t, m, hh * 64:(hh + 1) * 64, hh * 64:(hh + 1) * 64] = wq[t, h]
    mg = f(inp["ml_w_gate"][0])
    ml_wg = np.concatenate([mg[0], mg[1]], axis=1).reshape(12, 128, 32)
    ml_bg = f(inp["ml_b_gate"][0]).reshape(1, 32)
    ml_skip = f(inp["ml_skip"][0]).reshape(4, 128).T
    jj = np.arange(128, dtype=np.float32)
    jcol = np.stack([jj + 1, 128 - jj], axis=1)
    shared = {
        "ada_w": f(inp["ada_w"]), "ada_b": f(inp["ada_b"]), "norm_g": f(inp["norm_g"]), "w_out": f(inp["w_out"]),
        "mlp_w1": f(inp["mlp_w1"]), "mlp_w2": f(inp["mlp_w2"]), "w_in0": f(w_in0), "w_in1": f(w_in1),
        "gla_wg": f(gla_wg), "gla_ng": f(inp["gla_norm_g"]).reshape(1, 64), "att_g": f(att_g), "ml_conv": f(ml_conv),
        "ml_bd": ml_bd, "ml_wg": f(ml_wg), "ml_bg": ml_bg, "ml_ng": f(inp["ml_norm_g"]).reshape(1, 64), "ml_skip": f(ml_skip),
        "ret_logit": f(inp["ret_decay_logit"]).reshape(1, 16), "ret_ng": f(inp["ret_norm_g"]).reshape(1, 64),
        "cmat": _cmat(), "rope": _rope_tables(), "jcol": f(jcol),
    }
    maps = []
    x = np.asarray(inp["x"]); c = np.asarray(inp["c"]); ctx = np.asarray(inp["ctx"]); cc = np.asarray(inp["c_ctx"])
    for b in range(x.shape[0]):
        m = dict(shared)
        m["x"] = f(x[b]); m["ctx"] = f(ctx[b])
        m["ccols"] = f(np.stack([c[b].reshape(8, 128).T, cc.reshape(8, 128).T], axis=2))
        maps.append(m)
    return maps


_PROG = {}


def kernel(**inputs):
    maps = host_inputs(inputs)
    if "p" not in _PROG:
        p = Prog()
        p.build()
        _PROG["p"] = p
    p = _PROG["p"]
    res = run_bass_kernel_spmd(p.nc, maps, core_ids=list(range(8)))
    return np.stack([np.asarray(r["out"], dtype=np.float32) for r in res.results], axis=0)
```
